# Optimizing a Trainium2 kernel written in Bass

```python
import math
import jax
import jax.numpy as jnp
from jax import lax
import numpy as np

D_MODEL = 1024
BATCH = 16
SEQ = 2048
DEPTH = 4

GRID_W = 64
CTX_LEN = 256
N_MOD = 6

HEAD_DIM = 64
BRANCH_WIDTH = 512
N_BRANCH = 4

WA_HEADS = 8
WA_KV_HEADS = 2
WINDOW = 128
GA_HEADS = 8
GA_KV_HEADS = 2
Q_BLOCK = 128
ROPE_THETA = 10000.0

SSD_HEADS = 8
SSD_HEAD_DIM = 64
SSD_GROUPS = 2
SSD_STATE = 128
SSD_CONV = 3
SSD_CHUNK = 128
SSD_WIDTH = SSD_HEADS * SSD_HEAD_DIM
SSD_CONV_CH = SSD_WIDTH + 2 * SSD_GROUPS * SSD_STATE

S5_GROUP = 16
S5_WIDTH = 512
S5_GROUPS = S5_WIDTH // S5_GROUP
S5_STATE = 64

N_EXPERTS = 32
N_EXPERT_GROUPS = 8
TOP_K = 2
D_EXPERT = 512
MOE_BLOCK = 128

ALPHA = (2 * DEPTH) ** 0.25
BETA = (8 * DEPTH) ** -0.25
NORM_EPS = 1e-6
F32 = jnp.float32

IN_SIZES = (
    WA_HEADS * HEAD_DIM, WA_KV_HEADS * HEAD_DIM, WA_KV_HEADS * HEAD_DIM,
    GA_HEADS * HEAD_DIM, GA_KV_HEADS * HEAD_DIM, GA_KV_HEADS * HEAD_DIM,
    SSD_WIDTH, SSD_WIDTH, SSD_GROUPS * SSD_STATE, SSD_GROUPS * SSD_STATE,
    2 * SSD_HEADS,
    S5_WIDTH,
    N_BRANCH * D_MODEL,
)
D_IN = sum(IN_SIZES)

kernel_name = 'hybrid_prefix_dit_trunk'


def layer_norm(x):
    xf = x.astype(F32)
    mu = jnp.mean(xf, axis=-1, keepdims=True)
    var = jnp.mean(jnp.square(xf - mu), axis=-1, keepdims=True)
    return ((xf - mu) * lax.rsqrt(var + NORM_EPS)).astype(x.dtype)


def post_norm(v, g, b):
    return layer_norm(v) * g + b


def rms_norm(x, w):
    xf = x.astype(F32)
    y = xf * lax.rsqrt(jnp.mean(jnp.square(xf), axis=-1, keepdims=True) + NORM_EPS)
    return y.astype(x.dtype) * w


def split_heads(t, n_heads):
    return t.reshape(t.shape[:2] + (n_heads, HEAD_DIM))


def axial_rope_tables(n_tokens):
    rows = n_tokens // GRID_W
    row = jnp.repeat(jnp.arange(rows, dtype=F32), GRID_W)
    col = jnp.tile(jnp.arange(GRID_W, dtype=F32), rows)
    axis_dim = HEAD_DIM // 2
    inv_freq = ROPE_THETA ** (-jnp.arange(0, axis_dim, 2, dtype=F32) / axis_dim)
    ang_r = row[:, None] * inv_freq
    ang_c = col[:, None] * inv_freq
    ang = jnp.concatenate([ang_r, ang_r, ang_c, ang_c], axis=-1)
    return jnp.cos(ang), jnp.sin(ang)


def apply_axial_rope(t, cos, sin):
    axis_dim = HEAD_DIM // 2
    pair = axis_dim // 2
    tf = t.astype(F32)
    parts = []
    for a in range(2):
        seg = tf[..., a * axis_dim:(a + 1) * axis_dim]
        parts += [-seg[..., pair:], seg[..., :pair]]
    rotated = jnp.concatenate(parts, axis=-1)
    return (tf * cos[None, :, None, :] + rotated * sin[None, :, None, :]).astype(t.dtype)


def window_attention(q, k, v, k_ctx, v_ctx, sink):
    bsz, seq, n_heads, dh = q.shape
    n_kv = k.shape[2]
    grp = n_heads // n_kv
    span = Q_BLOCK + 2 * WINDOW
    pad = ((0, 0), (WINDOW, WINDOW), (0, 0), (0, 0))
    k_pad = jnp.pad(k, pad)
    v_pad = jnp.pad(v, pad)
    qg = q.reshape(bsz, seq, n_kv, grp, dh)
    sink_logit = jnp.broadcast_to(sink.astype(F32).reshape(1, n_kv, grp, 1, 1), (bsz, n_kv, grp, Q_BLOCK, 1))
    scale = dh ** -0.5
    offs_q = jnp.arange(Q_BLOCK)
    offs_k = jnp.arange(span) - WINDOW

    def block(i):
        start = i * Q_BLOCK
        qb = lax.dynamic_slice_in_dim(qg, start, Q_BLOCK, axis=1)
        kb = lax.dynamic_slice_in_dim(k_pad, start, span, axis=1)
        vb = lax.dynamic_slice_in_dim(v_pad, start, span, axis=1)
        q_pos = start + offs_q
        k_pos = start + offs_k
        valid = ((jnp.abs(q_pos[:, None] - k_pos[None, :]) <= WINDOW)
                 & (k_pos >= 0)[None, :] & (k_pos < seq)[None, :])
        s_loc = jnp.einsum('bqkgd,bskd->bkgqs', qb, kb).astype(F32) * scale
        s_loc = jnp.where(valid, s_loc, -jnp.inf)
        s_ctx = jnp.einsum('bqkgd,bckd->bkgqc', qb, k_ctx).astype(F32) * scale
        p = jax.nn.softmax(jnp.concatenate([s_loc, s_ctx, sink_logit], axis=-1), axis=-1).astype(v.dtype)
        return (jnp.einsum('bkgqs,bskd->bqkgd', p[..., :span], vb)
                + jnp.einsum('bkgqc,bckd->bqkgd', p[..., span:-1], v_ctx))

    out = lax.map(block, jnp.arange(seq // Q_BLOCK))
    return jnp.moveaxis(out, 0, 1).reshape(bsz, seq, n_heads * dh)


def dense_attention(q, k, v, k_ctx, v_ctx):
    bsz, seq, n_heads, dh = q.shape
    n_kv = k.shape[2]
    grp = n_heads // n_kv
    keys = jnp.concatenate([k, k_ctx], axis=1)
    vals = jnp.concatenate([v, v_ctx], axis=1)
    qg = q.reshape(bsz, seq, n_kv, grp, dh)
    scale = dh ** -0.5

    def block(i):
        qb = lax.dynamic_slice_in_dim(qg, i * Q_BLOCK, Q_BLOCK, axis=1)
        s = jnp.einsum('bqkgd,bskd->bkgqs', qb, keys).astype(F32) * scale
        p = jax.nn.softmax(s, axis=-1).astype(vals.dtype)
        return jnp.einsum('bkgqs,bskd->bqkgd', p, vals)

    out = lax.map(block, jnp.arange(seq // Q_BLOCK))
    return jnp.moveaxis(out, 0, 1).reshape(bsz, seq, n_heads * dh)


def context_attention(q, k, v, sink=None):
    bsz, n, n_heads, dh = q.shape
    n_kv = k.shape[2]
    grp = n_heads // n_kv
    qg = q.reshape(bsz, n, n_kv, grp, dh)
    s = jnp.einsum('bqkgd,bskd->bkgqs', qg, k).astype(F32) * dh ** -0.5
    if sink is not None:
        sink_logit = jnp.broadcast_to(sink.astype(F32).reshape(1, n_kv, grp, 1, 1), (bsz, n_kv, grp, n, 1))
        s = jnp.concatenate([s, sink_logit], axis=-1)
    p = jax.nn.softmax(s, axis=-1).astype(v.dtype)
    if sink is not None:
        p = p[..., :-1]
    return jnp.einsum('bkgqs,bskd->bqkgd', p, v).reshape(bsz, n, n_heads * dh)


def depthwise_conv(x, w, b):
    k = w.shape[0]
    y = lax.conv_general_dilated(x, w[:, None, :], window_strides=(1,), padding=[(k // 2, k // 2)],
                                 dimension_numbers=('NWC', 'WIO', 'NWC'), feature_group_count=x.shape[-1])
    return y + b


def segsum_exp(cs):
    t = cs.shape[-1]
    diff = cs[..., :, None] - cs[..., None, :]
    return jnp.exp(jnp.where(jnp.tril(jnp.ones((t, t), dtype=bool)), diff, -jnp.inf))


def ssd_scan(x, dt, a, bm, cm, init_state):
    bsz, seq, n_heads, p = x.shape
    g, n = bm.shape[2], bm.shape[3]
    j = n_heads // g
    nc = seq // SSD_CHUNK
    t = SSD_CHUNK
    xd = (x.astype(F32) * dt[..., None]).reshape(bsz, nc, t, g, j, p)
    da = (dt * a).reshape(bsz, nc, t, g, j).transpose(0, 3, 4, 1, 2)
    bc = bm.astype(F32).reshape(bsz, nc, t, g, n)
    cc = cm.astype(F32).reshape(bsz, nc, t, g, n)
    a_cs = jnp.cumsum(da, axis=-1)
    cb = jnp.einsum('bclgn,bcsgn->bgcls', cc, bc)
    y_diag = jnp.einsum('bgjcls,bcsgjp->bclgjp', cb[:, :, None] * segsum_exp(a_cs), xd)
    decay_to_end = jnp.exp(a_cs[..., -1:] - a_cs)
    states = jnp.einsum('bcsgn,bgjcs,bcsgjp->bcgjpn', bc, decay_to_end, xd)
    states = jnp.concatenate([init_state.astype(F32).reshape(bsz, 1, g, j, p, n), states], axis=1)
    chunk_cs = jnp.cumsum(jnp.pad(a_cs[..., -1], ((0, 0), (0, 0), (0, 0), (1, 0))), axis=-1)
    states = jnp.einsum('bgjzc,bcgjpn->bzgjpn', segsum_exp(chunk_cs), states)
    y_off = jnp.einsum('bclgn,bcgjpn,bgjcl->bclgjp', cc, states[:, :-1], jnp.exp(a_cs))
    y = (y_diag + y_off).reshape(bsz, seq, n_heads, p)
    return y, states[:, -1].reshape(bsz, n_heads, p, n)


def maybe_flip(t, rev):
    return jnp.flip(t, axis=1) if rev else t


def ssd_prepare(x, bm, cm, dt, conv_w, conv_b, dt_bias):
    xbc = jax.nn.silu(depthwise_conv(jnp.concatenate([x, bm, cm], axis=-1), conv_w, conv_b))
    x, bm, cm = jnp.split(xbc, [SSD_WIDTH, SSD_WIDTH + SSD_GROUPS * SSD_STATE], axis=-1)
    lead = x.shape[:2]
    return (x.reshape(lead + (SSD_HEADS, SSD_HEAD_DIM)),
            bm.reshape(lead + (SSD_GROUPS, SSD_STATE)),
            cm.reshape(lead + (SSD_GROUPS, SSD_STATE)),
            jax.nn.softplus(dt.astype(F32) + dt_bias.astype(F32)))


def ssd_gate_out(y, z, norm_w):
    g = y.reshape(z.shape[:2] + (SSD_WIDTH,)) * jax.nn.silu(z.astype(F32))
    return rms_norm(g, norm_w).astype(z.dtype)


def ssd_mixer(p_c, p_l, conv_w, conv_b, dt_bias, a_log, d_skip, norm_w, need_ctx):
    xc, bc, cc, dtc = ssd_prepare(p_c[0], p_c[2], p_c[3], p_c[4], conv_w, conv_b, dt_bias)
    xl, bl, cl, dtl = ssd_prepare(p_l[0], p_l[2], p_l[3], p_l[4], conv_w, conv_b, dt_bias)
    a = -jnp.exp(a_log.astype(F32))
    d = d_skip.astype(F32)[:, None]
    y_l = d * xl.astype(F32)
    y_c = d * xc.astype(F32) if need_ctx else None
    init = jnp.zeros((xc.shape[0], SSD_HEADS, SSD_HEAD_DIM, SSD_STATE), F32)
    for direction in range(2):
        rev = direction == 1
        hs = slice(direction * SSD_HEADS, (direction + 1) * SSD_HEADS)
        yc_d, s_ctx = ssd_scan(maybe_flip(xc, rev), maybe_flip(dtc[..., hs], rev), a[direction],
                               maybe_flip(bc, rev), maybe_flip(cc, rev), init)
        yl_d, _ = ssd_scan(maybe_flip(xl, rev), maybe_flip(dtl[..., hs], rev), a[direction],
                           maybe_flip(bl, rev), maybe_flip(cl, rev), s_ctx)
        y_l = y_l + maybe_flip(yl_d, rev)
        if need_ctx:
            y_c = y_c + maybe_flip(yc_d, rev)
    out_l = ssd_gate_out(y_l, p_l[1], norm_w)
    out_c = ssd_gate_out(y_c, p_c[1], norm_w) if need_ctx else None
    return out_c, out_l


def linear_recurrence_combine(left, right):
    a_l, b_l = left
    a_r, b_r = right
    return a_r * a_l, a_r * b_l + b_r


def s5_discretise(a_re, a_im, log_dt, b_cplx):
    a = lax.complex(jnp.minimum(a_re.astype(F32), -1e-4), a_im.astype(F32))
    dt = jnp.exp(log_dt.astype(F32))[:, None]
    a_bar = jnp.exp(a * dt)
    b_bar = ((a_bar - 1.0) / a)[..., None] * b_cplx
    return a_bar, b_bar


def s5_scan(bu, a_bar, init, reverse):
    n = bu.shape[1]
    a = jnp.broadcast_to(a_bar, (1, n) + a_bar.shape)
    a_cum, s = lax.associative_scan(linear_recurrence_combine, (a, bu), reverse=reverse, axis=1)
    if init is not None:
        s = s + a_cum * init[:, None]
    return s, (s[:, 0] if reverse else s[:, -1])


def s5_glu(y, w_glu, dtype):
    y = jax.nn.gelu(y.reshape(y.shape[:2] + (S5_WIDTH,))).astype(dtype)
    a, b = jnp.split(y @ w_glu, 2, axis=-1)
    return a * jax.nn.sigmoid(b)


def s5_mixer(u_c, u_l, a_re, a_im, log_dt, b_re, b_im, c_re, c_im, d_skip, w_glu, need_ctx):
    b_cplx = lax.complex(b_re.astype(F32), b_im.astype(F32))
    c_cplx = lax.complex(c_re.astype(F32), c_im.astype(F32))
    d_grp = d_skip.astype(F32).reshape(S5_GROUPS, S5_GROUP)
    uc = u_c.astype(F32).reshape(u_c.shape[:2] + (S5_GROUPS, S5_GROUP))
    ul = u_l.astype(F32).reshape(u_l.shape[:2] + (S5_GROUPS, S5_GROUP))
    y_l = d_grp * ul
    y_c = d_grp * uc if need_ctx else None
    for direction in range(2):
        rev = direction == 1
        a_bar, b_bar = s5_discretise(a_re[direction], a_im[direction], log_dt[direction], b_cplx)
        s_c, fin_c = s5_scan(jnp.einsum('blgs,gns->blgn', uc.astype(jnp.complex64), b_bar), a_bar, None, rev)
        s_l, _ = s5_scan(jnp.einsum('blgs,gns->blgn', ul.astype(jnp.complex64), b_bar), a_bar, fin_c, rev)
        y_l = y_l + jnp.real(jnp.einsum('blgn,gsn->blgs', s_l, c_cplx))
        if need_ctx:
            y_c = y_c + jnp.real(jnp.einsum('blgn,gsn->blgs', s_c, c_cplx))
    out_l = s5_glu(y_l, w_glu, u_l.dtype)
    out_c = s5_glu(y_c, w_glu, u_c.dtype) if need_ctx else None
    return out_c, out_l


def merge(branches, gate_logits, w_branch, w_out):
    ys = jnp.stack(branches, axis=2)
    g = jax.nn.sigmoid(gate_logits.reshape(gate_logits.shape[:2] + (N_BRANCH, D_MODEL)))
    m = jnp.einsum('blnw,nwd->blnd', ys, w_branch)
    return jnp.sum(g * m, axis=2) @ w_out


def token_mixer(h_c, h_l, cos, sin, w_in, wa_sink, ga_q_norm, ga_k_norm,
                ssd_conv_w, ssd_conv_b, ssd_dt_bias, ssd_a_log, ssd_d, ssd_norm_w,
                s5_a_re, s5_a_im, s5_log_dt, s5_b_re, s5_b_im, s5_c_re, s5_c_im, s5_d, s5_w_glu,
                w_branch, w_out, need_ctx):
    cuts = [int(v) for v in np.cumsum(IN_SIZES)[:-1]]
    (aq_c, ak_c, av_c, gq_c, gk_c, gv_c, sx_c, sz_c, sb_c, sc_c, sdt_c, su_c, gate_c) = jnp.split(h_c @ w_in, cuts, axis=-1)
    (aq_l, ak_l, av_l, gq_l, gk_l, gv_l, sx_l, sz_l, sb_l, sc_l, sdt_l, su_l, gate_l) = jnp.split(h_l @ w_in, cuts, axis=-1)

    ka_c = split_heads(ak_c, WA_KV_HEADS)
    va_c = split_heads(av_c, WA_KV_HEADS)
    ya_l = window_attention(apply_axial_rope(split_heads(aq_l, WA_HEADS), cos, sin),
                            apply_axial_rope(split_heads(ak_l, WA_KV_HEADS), cos, sin),
                            split_heads(av_l, WA_KV_HEADS), ka_c, va_c, wa_sink)
    kg_c = rms_norm(split_heads(gk_c, GA_KV_HEADS), ga_k_norm)
    vg_c = split_heads(gv_c, GA_KV_HEADS)
    yg_l = dense_attention(apply_axial_rope(rms_norm(split_heads(gq_l, GA_HEADS), ga_q_norm), cos, sin),
                           apply_axial_rope(rms_norm(split_heads(gk_l, GA_KV_HEADS), ga_k_norm), cos, sin),
                           split_heads(gv_l, GA_KV_HEADS), kg_c, vg_c)
    ys_c, ys_l = ssd_mixer((sx_c, sz_c, sb_c, sc_c, sdt_c), (sx_l, sz_l, sb_l, sc_l, sdt_l),
                           ssd_conv_w, ssd_conv_b, ssd_dt_bias, ssd_a_log, ssd_d, ssd_norm_w, need_ctx)
    y5_c, y5_l = s5_mixer(su_c, su_l, s5_a_re, s5_a_im, s5_log_dt, s5_b_re, s5_b_im,
                          s5_c_re, s5_c_im, s5_d, s5_w_glu, need_ctx)
    out_l = merge((ya_l, ys_l, yg_l, y5_l), gate_l, w_branch, w_out)
    if not need_ctx:
        return None, out_l
    ya_c = context_attention(split_heads(aq_c, WA_HEADS), ka_c, va_c, wa_sink)
    yg_c = context_attention(rms_norm(split_heads(gq_c, GA_HEADS), ga_q_norm), kg_c, vg_c)
    out_c = merge((ya_c, ys_c, yg_c, y5_c), gate_c, w_branch, w_out)
    return out_c, out_l


def expert_dispatch(h, expert_idx, gate_w, w_gate, w_up, w_down):
    n_tok, d = h.shape
    n_assign = n_tok * TOP_K
    n_blocks = -(-(n_assign + N_EXPERTS * (MOE_BLOCK - 1)) // MOE_BLOCK)
    cap = n_blocks * MOE_BLOCK
    flat_e = expert_idx.reshape(-1)
    flat_t = jnp.repeat(jnp.arange(n_tok, dtype=jnp.int32), TOP_K)
    flat_w = gate_w.reshape(-1)
    order = jnp.argsort(flat_e)
    e_s, t_s, w_s = flat_e[order], flat_t[order], flat_w[order]
    counts = jnp.bincount(flat_e, length=N_EXPERTS)
    padded = (counts + MOE_BLOCK - 1) // MOE_BLOCK * MOE_BLOCK
    first = jnp.cumsum(counts) - counts
    pad_end = jnp.cumsum(padded)
    dest = (pad_end - padded)[e_s] + jnp.arange(n_assign) - first[e_s]
    slot_tok = jnp.zeros((cap,), jnp.int32).at[dest].set(t_s)
    slot_w = jnp.zeros((cap,), h.dtype).at[dest].set(w_s.astype(h.dtype))
    block_expert = jnp.minimum(jnp.searchsorted(pad_end, jnp.arange(n_blocks) * MOE_BLOCK, side='right'),
                               N_EXPERTS - 1)
    x_blocks = h[slot_tok].reshape(n_blocks, MOE_BLOCK, d)

    def expert_block(args):
        xb, e = args
        return (jax.nn.silu(xb @ w_gate[e]) * (xb @ w_up[e])) @ w_down[e]

    y = lax.map(expert_block, (x_blocks, block_expert)).reshape(cap, d)
    return jnp.zeros_like(h).at[slot_tok].add(y * slot_w[:, None])


def moe(h, router_w, router_bias, w_gate, w_up, w_down):
    n_tok = h.shape[0]
    per_group = N_EXPERTS // N_EXPERT_GROUPS
    scores = jax.nn.sigmoid((h @ router_w).astype(F32))
    sel = (scores + router_bias.astype(F32)).reshape(n_tok, N_EXPERT_GROUPS, per_group)
    group_score = jnp.sum(lax.top_k(sel, 2)[0], axis=-1)
    grp = jnp.argmax(group_score, axis=-1).astype(jnp.int32)
    in_grp = jnp.take_along_axis(sel, grp[:, None, None], axis=1)[:, 0]
    local = lax.top_k(in_grp, TOP_K)[1]
    expert_idx = grp[:, None] * per_group + local
    w = jnp.take_along_axis(scores, expert_idx, axis=1)
    w = w / jnp.sum(w, axis=-1, keepdims=True)
    return expert_dispatch(h, expert_idx, w, w_gate, w_up, w_down)


def setup_inputs(seed: int = 0) -> dict:
    key = jax.random.key(seed)
    keys = jax.random.split(key, 40)
    counter = iter(range(40))

    def nrm(shape, scale):
        return jax.random.normal(keys[next(counter)], shape, F32) * scale

    def unif(shape, lo, hi):
        return jax.random.uniform(keys[next(counter)], shape, F32, lo, hi)

    L, D = DEPTH, D_MODEL
    inputs = {}
    inputs['x'] = nrm((BATCH, SEQ, D), 1.0)
    inputs['c'] = nrm((BATCH, D), 1.0)
    inputs['ctx'] = nrm((BATCH, CTX_LEN, D), 1.0)
    inputs['c_ctx'] = nrm((D,), 1.0)
    inputs['mod_w'] = nrm((L, D, N_MOD * D), 0.5 * D ** -0.5)
    inputs['mod_b'] = nrm((L, N_MOD * D), 0.02)
    inputs['w_in'] = nrm((L, D, D_IN), D ** -0.5)
    inputs['wa_sink'] = nrm((L, WA_HEADS), 0.5)
    inputs['ga_q_norm'] = 1.0 + nrm((L, HEAD_DIM), 0.02)
    inputs['ga_k_norm'] = 1.0 + nrm((L, HEAD_DIM), 0.02)
    inputs['ssd_conv_w'] = nrm((L, SSD_CONV, SSD_CONV_CH), SSD_CONV ** -0.5)
    inputs['ssd_conv_b'] = nrm((L, SSD_CONV_CH), 0.02)
    dt0 = jnp.exp(unif((L, 2 * SSD_HEADS), math.log(1e-3), math.log(1e-1)))
    inputs['ssd_dt_bias'] = dt0 + jnp.log(-jnp.expm1(-dt0))
    inputs['ssd_a_log'] = jnp.log(unif((L, 2, SSD_HEADS), 1.0, 16.0))
    inputs['ssd_d'] = 1.0 + nrm((L, SSD_HEADS), 0.1)
    inputs['ssd_norm_w'] = 1.0 + nrm((L, SSD_WIDTH), 0.02)
    inputs['s5_a_re'] = -0.5 + nrm((L, 2, S5_GROUPS, S5_STATE), 0.01)
    inputs['s5_a_im'] = jnp.pi * jnp.arange(S5_STATE, dtype=F32) + nrm((L, 2, S5_GROUPS, S5_STATE), 0.01)
    inputs['s5_log_dt'] = unif((L, 2, S5_GROUPS), math.log(1e-3), math.log(1e-1))
    inputs['s5_b_re'] = nrm((L, S5_GROUPS, S5_STATE, S5_GROUP), (2 * S5_GROUP) ** -0.5)
    inputs['s5_b_im'] = nrm((L, S5_GROUPS, S5_STATE, S5_GROUP), (2 * S5_GROUP) ** -0.5)
    inputs['s5_c_re'] = nrm((L, S5_GROUPS, S5_GROUP, S5_STATE), S5_STATE ** -0.5)
    inputs['s5_c_im'] = nrm((L, S5_GROUPS, S5_GROUP, S5_STATE), S5_STATE ** -0.5)
    inputs['s5_d'] = nrm((L, S5_WIDTH), 1.0)
    inputs['s5_w_glu'] = nrm((L, S5_WIDTH, 2 * S5_WIDTH), S5_WIDTH ** -0.5)
    inputs['w_branch'] = nrm((L, N_BRANCH, BRANCH_WIDTH, D), BETA * BRANCH_WIDTH ** -0.5)
    inputs['w_out'] = nrm((L, D, D), BETA * D ** -0.5)
    inputs['ln1_g'] = 1.0 + nrm((L, D), 0.02)
    inputs['ln1_b'] = nrm((L, D), 0.02)
    inputs['ln2_g'] = 1.0 + nrm((L, D), 0.02)
    inputs['ln2_b'] = nrm((L, D), 0.02)
    inputs['router_w'] = nrm((D, N_EXPERTS), D ** -0.5)
    inputs['router_bias'] = nrm((N_EXPERTS,), 0.01)
    inputs['moe_w_gate'] = nrm((L, N_EXPERTS, D, D_EXPERT), D ** -0.5)
    inputs['moe_w_up'] = nrm((L, N_EXPERTS, D, D_EXPERT), D ** -0.5)
    inputs['moe_w_down'] = nrm((L, N_EXPERTS, D_EXPERT, D), BETA * D_EXPERT ** -0.5)
    return inputs


def reference(x, c, ctx, c_ctx, mod_w, mod_b, w_in, wa_sink, ga_q_norm, ga_k_norm,
              ssd_conv_w, ssd_conv_b, ssd_dt_bias, ssd_a_log, ssd_d, ssd_norm_w,
              s5_a_re, s5_a_im, s5_log_dt, s5_b_re, s5_b_im, s5_c_re, s5_c_im, s5_d, s5_w_glu,
              w_branch, w_out, ln1_g, ln1_b, ln2_g, ln2_b, router_w, router_bias,
              moe_w_gate, moe_w_up, moe_w_down):
    bsz, seq, d = x.shape
    n_ctx = ctx.shape[1]
    cos, sin = axial_rope_tables(seq)
    cond_l = jax.nn.silu(c)
    cond_c = jax.nn.silu(c_ctx)
    xc = ctx
    for layer in range(DEPTH):
        need_ctx = layer < DEPTH - 1
        shift1, scale1, gate1, shift2, scale2, gate2 = jnp.split(
            (cond_l @ mod_w[layer] + mod_b[layer])[:, None, :], N_MOD, axis=-1)
        cshift1, cscale1, cgate1, cshift2, cscale2, cgate2 = jnp.split(
            cond_c @ mod_w[layer] + mod_b[layer], N_MOD, axis=-1)

        h_l = layer_norm(x) * (1 + scale1) + shift1
        h_c = layer_norm(xc) * (1 + cscale1) + cshift1
        mix_c, mix_l = token_mixer(h_c, h_l, cos, sin, w_in[layer], wa_sink[layer], ga_q_norm[layer], ga_k_norm[layer],
                                   ssd_conv_w[layer], ssd_conv_b[layer], ssd_dt_bias[layer], ssd_a_log[layer],
                                   ssd_d[layer], ssd_norm_w[layer],
                                   s5_a_re[layer], s5_a_im[layer], s5_log_dt[layer], s5_b_re[layer], s5_b_im[layer],
                                   s5_c_re[layer], s5_c_im[layer], s5_d[layer], s5_w_glu[layer],
                                   w_branch[layer], w_out[layer], need_ctx)
        x = post_norm(ALPHA * x + gate1 * mix_l, ln1_g[layer], ln1_b[layer])

        h_l = (layer_norm(x) * (1 + scale2) + shift2).reshape(bsz * seq, d)
        if need_ctx:
            xc = post_norm(ALPHA * xc + cgate1 * mix_c, ln1_g[layer], ln1_b[layer])
            h_c = (layer_norm(xc) * (1 + cscale2) + cshift2).reshape(bsz * n_ctx, d)
            ffn = moe(jnp.concatenate([h_c, h_l], axis=0), router_w, router_bias,
                      moe_w_gate[layer], moe_w_up[layer], moe_w_down[layer])
            xc = post_norm(ALPHA * xc + cgate2 * ffn[:bsz * n_ctx].reshape(bsz, n_ctx, d), ln2_g[layer], ln2_b[layer])
            ffn_l = ffn[bsz * n_ctx:]
        else:
            ffn_l = moe(h_l, router_w, router_bias, moe_w_gate[layer], moe_w_up[layer], moe_w_down[layer])
        x = post_norm(ALPHA * x + gate2 * ffn_l.reshape(bsz, seq, d), ln2_g[layer], ln2_b[layer])
    return x
```

```python
import contextlib
import math
import numpy as np
import ml_dtypes
import concourse.bass as bass
import concourse.mybir as mybir
from concourse.bass_utils import run_bass_kernel_spmd

F32 = mybir.dt.float32
BF16 = mybir.dt.bfloat16
I32 = mybir.dt.int32
AF = mybir.ActivationFunctionType
ALU = mybir.AluOpType
AX = mybir.AxisListType

SEM_LIM = 20000
ND = 10

D = 1024
CTX = 256
SEQ = 2048
NT = CTX + SEQ
NTILE = NT // 128
GRID_W = 64
EPS = 1e-6
D_IN = 7696
TCH = [(0, 512), (512, 512), (1024, 512), (1536, 512), (2048, 256)]
NEG = -30000.0


class TT:
    __slots__ = ("h", "name", "w", "r", "ps")

    def __init__(self, h, name, ps=False):
        self.h = h
        self.name = name
        self.w = None
        self.r = {}
        self.ps = ps

    def __getitem__(self, idx):
        return self.h[idx]


class KB:
    def __init__(self, nc):
        self.nc = nc
        self.es = contextlib.ExitStack()
        self.alloc_es = self.es
        self.engines = {"pe": nc.tensor, "dve": nc.vector, "act": nc.scalar,
                        "pool": nc.gpsimd, "sp": nc.sync}
        self.cur = {}
        self.known = {k: {} for k in self.engines}
        self.allsems = []
        self.dpool = {}
        self.dnext = {}
        self.nsem = 0
        self.uid = 0
        self.ninst = 0
        self.pe_sems = set()

    def _name(self, name):
        self.uid += 1
        return f"{name}_{self.uid}"

    def sb(self, name, shape, dtype=F32):
        n = self._name(name)
        h = self.alloc_es.enter_context(self.nc.sbuf_tensor(n, list(shape), dtype))
        return TT(h, n)

    def ps(self, name, shape, dtype=F32):
        n = self._name(name)
        h = self.alloc_es.enter_context(self.nc.psum_tensor(n, list(shape), dtype))
        return TT(h, n, ps=True)

    def dram(self, name, shape, dtype=F32, kind="Internal"):
        h = self.nc.dram_tensor(name, list(shape), dtype, kind=kind)
        return TT(h, name)

    @contextlib.contextmanager
    def scope(self):
        es = contextlib.ExitStack()
        old = self.alloc_es
        self.alloc_es = es
        try:
            yield
        finally:
            self.barrier()
            es.close()
            self.alloc_es = old

    def _newsem(self, name):
        self.nsem += 1
        s = self.es.enter_context(self.nc.semaphore(f"{name}_{self.nsem}"))
        rec = [s, 0]
        self.allsems.append(rec)
        return rec

    def _deps(self, reads, writes):
        d = {}

        def add(ev):
            rec, v = ev
            k = id(rec)
            if k not in d or d[k][1] < v:
                d[k] = (rec, v)
        for t in reads:
            if t.w is not None:
                add(t.w)
            if t.ps:
                for ev in t.r.values():
                    add(ev)
        for t in writes:
            if t.w is not None:
                add(t.w)
            for ev in t.r.values():
                add(ev)
        return d

    def _emit(self, e, fn, deps):
        eng = self.engines[e]
        kn = self.known[e]
        waits = []
        for k, (rec, v) in deps.items():
            if e == "pe" and k in self.pe_sems:
                continue
            if kn.get(k, 0) < v:
                waits.append((rec, v))
                kn[k] = v
        for rec, v in waits[:-1]:
            eng.wait_ge(rec[0], v)
            self.ninst += 1
        ins = fn(eng)
        self.ninst += 1
        if waits:
            rec, v = waits[-1]
            ins._wait_ge(rec[0], v)
        return ins

    def _record(self, ev, reads, writes):
        rec, v = ev
        for t in reads:
            t.r[id(rec)] = ev
        for t in writes:
            t.w = ev
            t.r = {}

    def op(self, e, fn, reads=(), writes=()):
        deps = self._deps(reads, writes)
        ins = self._emit(e, fn, deps)
        rec = self.cur.get(e)
        if rec is None or rec[1] >= SEM_LIM:
            rec = self.cur[e] = self._newsem("s" + e)
            if e == "pe":
                self.pe_sems.add(id(rec))
        rec[1] += 1
        ins.then_inc(rec[0], 1)
        self._record((rec, rec[1]), reads, writes)
        return ins

    def dma(self, q, out, in_, reads=(), writes=(), **kw):
        if q not in self.dpool:
            self.dpool[q] = [self._newsem("d" + q) for _ in range(ND)]
            self.dnext[q] = 0
        i = self.dnext[q]
        self.dnext[q] = i + 1
        rec = self.dpool[q][i % ND]
        if rec[1] >= SEM_LIM:
            rec = self.dpool[q][i % ND] = self._newsem("d" + q)
        deps = self._deps(reads, writes)
        if rec[1] > 0:
            k = id(rec)
            if k not in deps or deps[k][1] < rec[1]:
                deps[k] = (rec, rec[1])
        ins = self._emit(q, lambda eng: eng.dma_start(out=out, in_=in_, **kw), deps)
        rec[1] += 16
        ins.then_inc(rec[0], 16)
        self._record((rec, rec[1]), reads, writes)
        return ins

    def barrier(self):
        for e, eng in self.engines.items():
            kn = self.known[e]
            for rec in self.allsems:
                if rec[1] > 0 and kn.get(id(rec), 0) < rec[1]:
                    eng.wait_ge(rec[0], rec[1])
                    self.ninst += 1
                    kn[id(rec)] = rec[1]

    def close(self):
        self.barrier()
        self.es.close()

    def mm(self, out, lhsT, rhs, start, stop, R, W):
        return self.op("pe", lambda e: e.matmul(out, lhsT=lhsT, rhs=rhs, start=start, stop=stop), reads=R, writes=W)

    def mm32(self, out, lhsT, rhs, start, stop, R, W, junk, jt, identb):
        self.mm(out, lhsT, rhs, start, stop, R, W)
        if stop:
            self.op("pe", lambda e: e.matmul(junk, lhsT=identb[:, 0:32], rhs=identb[:, 0:1], start=True, stop=True),
                    reads=[], writes=list(W) + ([jt] if jt not in W else []))

    def tr(self, out, in_, ident, R, W):
        return self.op("pe", lambda e: e.transpose(out=out, in_=in_, identity=ident), reads=R, writes=W)

    def act(self, out, in_, func, R, W, **kw):
        return self.op("act", lambda e: e.activation(out=out, in_=in_, func=func, **kw), reads=R, writes=W)

    def ts(self, eng, out, in0, s1, s2, op0, op1, R, W):
        if op1 is None:
            return self.op(eng, lambda e: e.tensor_scalar(out=out, in0=in0, scalar1=s1, scalar2=None, op0=op0), reads=R, writes=W)
        return self.op(eng, lambda e: e.tensor_scalar(out=out, in0=in0, scalar1=s1, scalar2=s2, op0=op0, op1=op1), reads=R, writes=W)

    def tt(self, eng, out, in0, in1, op, R, W):
        return self.op(eng, lambda e: e.tensor_tensor(out=out, in0=in0, in1=in1, op=op), reads=R, writes=W)

    def stt(self, out, in0, scalar, in1, op0, op1, R, W):
        return self.op("dve", lambda e: e.scalar_tensor_tensor(out=out, in0=in0, scalar=scalar, in1=in1, op0=op0, op1=op1), reads=R, writes=W)

    def cp(self, eng, out, in_, R, W):
        if eng == "act":
            return self.act(out, in_, AF.Copy, R, W)
        return self.op(eng, lambda e: e.tensor_copy(out=out, in_=in_), reads=R, writes=W)


def host_consts():
    c = {}
    c["c_identb"] = np.eye(128).astype(ml_dtypes.bfloat16)
    c["c_identf"] = np.eye(128).astype(np.float32)
    rows = SEQ // GRID_W
    row = np.repeat(np.arange(rows, dtype=np.float32), GRID_W)
    col = np.tile(np.arange(GRID_W, dtype=np.float32), rows)
    axis_dim = 32
    inv_freq = (10000.0 ** (-np.arange(0, axis_dim, 2, dtype=np.float32) / axis_dim)).astype(np.float32)
    ang_r = row[:, None] * inv_freq
    ang_c = col[:, None] * inv_freq
    ang = np.concatenate([ang_r, ang_r, ang_c, ang_c], axis=-1)
    cos = np.cos(ang).T
    sin = np.sin(ang).T
    sign = np.where((np.arange(64) % 32) < 16, -1.0, 1.0)[:, None]
    tab = np.zeros((128, 2, NT), np.float32)
    tab[:, 0, :CTX] = 1.0
    for h in range(2):
        tab[h * 64:(h + 1) * 64, 0, CTX:] = cos
        tab[h * 64:(h + 1) * 64, 1, CTX:] = sin * sign
    c["c_rope"] = tab.astype(ml_dtypes.bfloat16)
    perm = np.zeros((128, 128), np.float32)
    for h in range(2):
        for m in range(64):
            src = m + 16 if (m % 32) < 16 else m - 16
            perm[h * 64 + src, h * 64 + m] = 1.0
    c["c_perm"] = perm.astype(ml_dtypes.bfloat16)
    on2 = np.zeros((128, 128), np.float32)
    on2[:64, :64] = 1.0 / 64
    on2[64:, 64:] = 1.0 / 64
    c["c_ones2"] = on2.astype(ml_dtypes.bfloat16)
    k = np.arange(128)[:, None]
    q = np.arange(128)[None, :]
    mL = np.where(k >= q, 0.0, NEG).astype(np.float32)
    mU = np.where(k <= q, 0.0, NEG).astype(np.float32)
    c["c_maskL"] = np.tile(mL, (1, 4)).astype(ml_dtypes.bfloat16)
    c["c_maskU"] = np.tile(mU, (1, 4)).astype(ml_dtypes.bfloat16)
    c["c_ones"] = np.ones((128, 128), ml_dtypes.bfloat16)
    c["c_triF"] = (k <= q).astype(np.float32)
    c["c_triB"] = (k >= q).astype(np.float32)
    c["c_nmF"] = np.where(k <= q, 0.0, NEG).astype(ml_dtypes.bfloat16)
    c["c_nmB"] = np.where(k >= q, 0.0, NEG).astype(ml_dtypes.bfloat16)
    return c


class G:
    pass


def build(NB=2, DEPTH=4, ALPHA=8 ** 0.25, debug=False, stages=None, use_moe=True):
    nc = bass.Bass("TRN2", target_bir_lowering=False)
    kb = KB(nc)
    g = G()
    g.kb, g.nc, g.NB, g.DEPTH, g.ALPHA, g.debug = kb, nc, NB, DEPTH, ALPHA, debug
    g.dbg = {}
    ins = {}

    def inp(name, shape, dt=F32):
        ins[name] = kb.dram(name, shape, dt, kind="ExternalInput")
        return ins[name]
    g.ins = ins
    inp("xin", [NB, NT, D])
    inp("cvec", [NB + 1, D])
    Ld = DEPTH
    inp("mod_w", [Ld, D, 6 * D]); inp("mod_b", [Ld, 6 * D]); inp("w_in", [Ld, D, D_IN])
    inp("wa_sink", [Ld, 8]); inp("ga_q_norm", [Ld, 64]); inp("ga_k_norm", [Ld, 64])
    inp("ssd_conv_w", [Ld, 3, 1024]); inp("ssd_conv_b", [Ld, 1024]); inp("ssd_dt_bias", [Ld, 16])
    inp("ssd_a_log", [Ld, 2, 8]); inp("ssd_d", [Ld, 8]); inp("ssd_norm_w", [Ld, 512])
    inp("s5_a_re", [Ld, 2, 32, 64]); inp("s5_a_im", [Ld, 2, 32, 64]); inp("s5_log_dt", [Ld, 2, 32])
    inp("s5_b_re", [Ld, 32, 64, 16]); inp("s5_b_im", [Ld, 32, 64, 16])
    inp("s5_c_re", [Ld, 32, 16, 64]); inp("s5_c_im", [Ld, 32, 16, 64])
    inp("s5_d", [Ld, 512]); inp("s5_w_glu", [Ld, 512, 1024])
    inp("w_branch", [Ld, 4, 512, D]); inp("w_out", [Ld, D, D])
    inp("ln1_g", [Ld, D]); inp("ln1_b", [Ld, D]); inp("ln2_g", [Ld, D]); inp("ln2_b", [Ld, D])
    inp("router_w", [D, 32]); inp("router_bias", [32])
    if use_moe:
        inp("moe_w_gate", [Ld, 32, D, 512]); inp("moe_w_up", [Ld, 32, D, 512]); inp("moe_w_down", [Ld, 32, 512, D])
    hc = host_consts()
    for k, v in hc.items():
        inp(k, list(v.shape), BF16 if v.dtype == ml_dtypes.bfloat16 else F32)
    g.out = kb.dram("out", [NB, SEQ, D], F32, kind="ExternalOutput")
    g.X1 = kb.dram("X1", [NB, NT, D], F32, kind="ExternalOutput" if debug else "Internal")
    g.X2 = kb.dram("X2", [NB, NT, D], F32, kind="ExternalOutput" if debug else "Internal")

    def cload(name, shape, dt):
        t = kb.sb(name, shape, dt)
        kb.dma("sp", t[:], ins[name][:], reads=[ins[name]], writes=[t])
        return t
    g.identb = cload("c_identb", [128, 128], BF16)
    g.identf = cload("c_identf", [128, 128], F32)
    g.perm = cload("c_perm", [128, 128], BF16)
    g.ones2 = cload("c_ones2", [128, 128], BF16)
    g.maskL = cload("c_maskL", [128, 512], BF16)
    g.maskU = cload("c_maskU", [128, 512], BF16)
    g.ones = cload("c_ones", [128, 128], BF16)
    g.triF = cload("c_triF", [128, 128], F32)
    g.triB = cload("c_triB", [128, 128], F32)
    g.nmF = cload("c_nmF", [128, 128], BF16)
    g.nmB = cload("c_nmB", [128, 128], BF16)

    run_all(g, stages)
    kb.close()
    return nc, g


def dbg_out(g, name, t, shape, dt=F32):
    if not g.debug:
        return
    kb = g.kb
    d = kb.dram("dbg_" + name, shape, dt, kind="ExternalOutput")
    kb.dma("sp", d[:], t[:], reads=[t], writes=[d])
    g.dbg[name] = d


def stage_mod(g, L):
    kb, NB = g.kb, g.NB
    ins = g.ins
    R = NB + 1
    modT = kb.sb("modT", [128, 48, R], F32)
    greps = [[kb.sb(f"grep{r}_{s}", [128, D], BF16) for s in range(2)] for r in range(R)]
    with kb.scope():
        condT = kb.sb("condT", [128, 8, R], F32)
        for r in range(R):
            kb.dma("sp", condT[:, :, r], ins["cvec"][r, :].rearrange("(k p) -> p k", p=128), reads=[ins["cvec"]], writes=[condT],
                   allow_slow_non_contiguous=True)
        kb.act(condT[:], condT[:], AF.Silu, [condT], [condT])
        mbT = kb.sb("mbT", [128, 48], F32)
        kb.dma("sp", mbT[:], ins["mod_b"][L, :].rearrange("(c p) -> p c", p=128), reads=[ins["mod_b"]], writes=[mbT],
               allow_slow_non_contiguous=True)
        mbrow = kb.sb("mbrow", [R, 6 * D], F32)
        kb.dma("sp", mbrow[:], ins["mod_b"][L:L + 1, :].broadcast_to([R, 6 * D]), reads=[ins["mod_b"]], writes=[mbrow])
        sel = kb.sb("sel", [R, R, 128], F32)
        kb.op("dve", lambda e: e.memset(sel[:], 0.0), writes=[sel])
        kb.op("dve", lambda e: e.tensor_copy(out=sel[:], in_=g.identf[0:R, 0:R].unsqueeze(2).broadcast_to([R, R, 128])),
              reads=[g.identf], writes=[sel])
        wt = [kb.sb(f"modw{i}", [128, 8, D], F32) for i in range(2)]
        pm = kb.ps("pm", [128, 512], F32)
        pr = kb.ps("pr", [128, 512], F32)
        pg = kb.ps("pg", [128, 512], F32)
        pj = kb.ps("pj", [128, 512], F32)
        rows = kb.sb("rows", [R, 512], F32)
        for s in range(6):
            w = wt[s % 2]
            kb.dma("sp" if s % 2 == 0 else "act", w[:], ins["mod_w"][L, :, s * D:(s + 1) * D].rearrange("(k p) c -> p k c", p=128),
                   reads=[ins["mod_w"]], writes=[w])
            for fc in range(8):
                for k in range(8):
                    kb.mm32(pm[:, fc * R:(fc + 1) * R], w[:, k, fc * 128:(fc + 1) * 128], condT[:, k, :], k == 0, k == 7, [w, condT], [pm],
                            pj[0:32, 0:1], pj, g.identb)
            for fc in range(8):
                ch = s * 8 + fc
                kb.ts("dve", modT[:, ch, :], pm[:, fc * R:(fc + 1) * R], mbT[:, ch:ch + 1], 1.0 if s in (1, 4) else 0.0,
                      ALU.add, ALU.add, [pm, mbT], [modT])
            if s in (2, 5):
                gi = 0 if s == 2 else 1
                for half in range(2):
                    for k in range(8):
                        kb.mm32(pr[0:R, :], condT[:, k, :], w[:, k, half * 512:(half + 1) * 512], k == 0, k == 7, [w, condT], [pr],
                                pj[0:32, 0:1], pj, g.identb)
                    kb.tt("dve", rows[:], pr[0:R, :], mbrow[:, s * D + half * 512: s * D + (half + 1) * 512], ALU.add, [pr, mbrow], [rows])
                    for r in range(R):
                        kb.mm32(pg[:], sel[:, r, :], rows[:], True, True, [sel, rows], [pg], pj[0:32, 0:1], pj, g.identb)
                        kb.cp("dve", greps[r][gi][:, half * 512:(half + 1) * 512], pg[:], [pg], [greps[r][gi]])
    g.modT = modT
    g.greps = greps


def stage_ln(g, b, Xsrc, chunkA, chunkB, hT, router=None):
    kb, NB = g.kb, g.NB
    with kb.scope():
        xts = [kb.sb(f"ln_x{i}", [128, D], F32) for i in range(2)]
        xns = [kb.sb(f"ln_xn{i}", [128, D], BF16) for i in range(2)]
        st = kb.sb("ln_st", [128, 2, 6], F32)
        mv = kb.sb("ln_mv", [128, 2], F32)
        rs = kb.sb("ln_rs", [128, 1], F32)
        tps = [kb.ps(f"ln_tp{i}", [128, D], BF16) for i in range(2)]
        for i in range(NTILE):
            col = NB if i < CTX // 128 else b
            xt, xn, tp = xts[i % 2], xns[i % 2], tps[i % 2]
            kb.dma("sp", xt[:], Xsrc[b, i * 128:(i + 1) * 128, :], reads=[Xsrc], writes=[xt])
            for hf in range(2):
                kb.op("dve", lambda e: e.bn_stats(out=st[:, hf, :], in_=xt[:, hf * 512:(hf + 1) * 512]), reads=[xt], writes=[st])
            kb.op("dve", lambda e: e.bn_aggr(out=mv[:], in_=st[:].rearrange("p a b -> p (a b)")), reads=[st], writes=[mv])
            kb.act(rs[:], mv[:, 1:2], AF.Sqrt, [mv], [rs], bias=g.epsc[:, 0:1])
            kb.op("dve", lambda e: e.reciprocal(out=rs[:], in_=rs[:]), reads=[rs], writes=[rs])
            kb.ts("dve", xn[:], xt[:], mv[:, 0:1], rs[:, 0:1], ALU.subtract, ALU.mult, [xt, mv, rs], [xn])
            for c in range(8):
                kb.tr(tp[:, c * 128:(c + 1) * 128], xn[:, c * 128:(c + 1) * 128], g.identb[:], [xn, g.identb], [tp])
            for c in range(8):
                o = hT[:, c, i * 128:(i + 1) * 128]
                a = g.modT[:, chunkA + c, col:col + 1]
                bb = g.modT[:, chunkB + c, col:col + 1]
                if c % 2 == 0:
                    kb.ts("dve", o, tp[:, c * 128:(c + 1) * 128], a, bb, ALU.mult, ALU.add, [tp, g.modT], [hT])
                else:
                    kb.act(o, tp[:, c * 128:(c + 1) * 128], AF.Identity, [tp, g.modT], [hT], scale=a, bias=bb)


def load_w(g, name, dst, src_ap, srcT, q="pool"):
    g.kb.dma(q, dst, src_ap, reads=[srcT], writes=[name])


def stage_attn(g, L, b, hT, YT, kind):
    kb, NB = g.kb, g.NB
    ins = g.ins
    w_in = ins["w_in"]
    c0 = 0 if kind == "a" else 768
    with kb.scope():
        Wq = kb.sb("Wq", [128, 8, 512], BF16)
        for i in range(4):
            for j in range(2):
                h = 4 * j + i
                kb.dma("pool", Wq[:, :, i * 128 + j * 64: i * 128 + (j + 1) * 64],
                       w_in[L, :, c0 + h * 64: c0 + (h + 1) * 64].rearrange("(k p) c -> p k c", p=128), reads=[w_in], writes=[Wq])
        Wk = kb.sb("Wk", [128, 8, 128], BF16)
        kb.dma("pool", Wk[:], w_in[L, :, c0 + 512: c0 + 640].rearrange("(k p) c -> p k c", p=128), reads=[w_in], writes=[Wk])
        Wv = kb.sb("Wv", [128, 8, 128], BF16)
        kb.dma("pool", Wv[:], w_in[L, :, c0 + 640: c0 + 768].rearrange("(k p) c -> p k c", p=128), reads=[w_in], writes=[Wv])
        QT = kb.sb("QT", [128, 4, NT], BF16)
        KT = kb.sb("KT", [128, NT], BF16)
        g.rope = kb.sb("rope", [128, 2, NT], BF16)
        kb.dma("sp", g.rope[:], ins["c_rope"][:], reads=[ins["c_rope"]], writes=[g.rope])
        V = kb.sb("V", [128, NTILE, 128], BF16)
        if kind == "g":
            nw = kb.sb("nw", [128, 2], F32)
            for hf in range(2):
                kb.dma("sp", nw[hf * 64:(hf + 1) * 64, 0:1], ins["ga_q_norm"][L, :].rearrange("(p o) -> p o", o=1), reads=[ins["ga_q_norm"]], writes=[nw])
                kb.dma("sp", nw[hf * 64:(hf + 1) * 64, 1:2], ins["ga_k_norm"][L, :].rearrange("(p o) -> p o", o=1), reads=[ins["ga_k_norm"]], writes=[nw])
        else:
            esink = kb.sb("esink", [128, 4], F32)
            for j in range(2):
                kb.dma("sp", esink[j * 64:(j + 1) * 64, :], ins["wa_sink"][L:L + 1, 4 * j:4 * j + 4].broadcast_to([64, 4]),
                       reads=[ins["wa_sink"]], writes=[esink])
            kb.act(esink[:], esink[:], AF.Exp, [esink], [esink])
        with kb.scope():
            pA = [kb.ps(f"pA{i}", [128, 512], F32) for i in range(2)]
            pB = [kb.ps(f"pB{i}", [128, 512], F32) for i in range(2)]
            pV = kb.ps("pV", [128, 512], F32)
            qa = [kb.sb(f"qa{i}", [128, 512], BF16) for i in range(2)]
            sq = [kb.sb(f"sq{i}", [128, 512], BF16) for i in range(2)]
            rstd = [kb.sb(f"rstd{i}", [128, 512], F32) for i in range(2)]
            t1 = [kb.sb(f"t1{i}", [128, 512], F32) for i in range(2)]
            t2 = [kb.sb(f"t2{i}", [128, 512], F32) for i in range(2)]
            n = 0
            for (t0, tn) in TCH:
                for m in range(5):
                    W = Wq if m < 4 else Wk
                    wc = m * 128 if m < 4 else 0
                    A, B = pA[n % 2], pB[n % 2]
                    Q, S, RS, T1, T2 = qa[n % 2], sq[n % 2], rstd[n % 2], t1[n % 2], t2[n % 2]
                    n += 1
                    for k in range(8):
                        kb.mm(A[:, :tn], W[:, k, wc:wc + 128], hT[:, k, t0:t0 + tn], k == 0, k == 7, [W, hT], [A])
                    dst = QT[:, m, t0:t0 + tn] if m < 4 else KT[:, t0:t0 + tn]
                    dstT = QT if m < 4 else KT
                    if kind == "g":
                        kb.act(S[:, :tn], A[:, :tn], AF.Square, [A], [S])
                        kb.mm(B[:, :tn], g.ones2[:], S[:, :tn], True, True, [g.ones2, S], [B])
                        kb.act(RS[:, :tn], B[:, :tn], AF.Sqrt, [B], [RS], bias=g.epsc[:, 0:1])
                        kb.op("dve", lambda e: e.reciprocal(out=RS[:, :tn], in_=RS[:, :tn]), reads=[RS], writes=[RS])
                        kb.stt(T1[:, :tn], A[:, :tn], nw[:, (0 if m < 4 else 1):(1 if m < 4 else 2)], RS[:, :tn], ALU.mult, ALU.mult, [A, nw, RS], [T1])
                        src = T1
                        kb.cp("act", Q[:, :tn], T1[:, :tn], [T1], [Q])
                    else:
                        src = A
                        kb.cp("act", Q[:, :tn], A[:, :tn], [A], [Q])
                    kb.mm(B[:, :tn], g.perm[:], Q[:, :tn], True, True, [g.perm, Q], [B])
                    kb.tt("dve", T2[:, :tn], B[:, :tn], g.rope[:, 1, t0:t0 + tn], ALU.mult, [B, g.rope], [T2])
                    kb.tt("pool" if src is T1 else "dve", T1[:, :tn], src[:, :tn], g.rope[:, 0, t0:t0 + tn], ALU.mult, [src, g.rope], [T1])
                    kb.tt("pool", dst, T1[:, :tn], T2[:, :tn], ALU.add, [T1, T2], [dstT])
            for i in range(NTILE):
                for k in range(8):
                    kb.mm(pV[:, 0:128], hT[:, k, i * 128:(i + 1) * 128], Wv[:, k, :], k == 0, k == 7, [hT, Wv], [pV])
                kb.cp("act", V[:, i, :], pV[:, 0:128], [pV], [V])
            if g.debug and b == 0 and L == 0:
                dbg_out(g, f"QT{kind}", QT, [128, 4, NT], BF16)
                dbg_out(g, f"KT{kind}", KT, [128, NT], BF16)
                dbg_out(g, f"V{kind}", V, [128, NTILE, 128], BF16)
        pS = [kb.ps(f"pS{i}", [128, 512], F32) for i in range(4)]
        pO = kb.ps("pO", [128, 512], F32)
        pD = kb.ps("pD", [128, 512], F32)
        P = [kb.sb(f"P{i}", [128, 512], BF16) for i in range(4)]
        rec = kb.sb("rec", [128, 512], F32)
        n = 0
        nctx = CTX // 128
        for qi in range(NTILE):
            if qi < nctx:
                keys = [(kt, None) for kt in range(nctx)]
            elif kind == "g":
                keys = [(kt, None) for kt in range(NTILE)]
            else:
                keys = [(kt, None) for kt in range(nctx)]
                if qi - 1 >= nctx:
                    keys.append((qi - 1, g.maskL))
                keys.append((qi, None))
                if qi + 1 < NTILE:
                    keys.append((qi + 1, g.maskU))
            for j in range(2):
                ps_ = slice(j * 64, (j + 1) * 64)
                qrhs = QT[ps_, :, qi * 128:(qi + 1) * 128]
                for ki, (kt, msk) in enumerate(keys):
                    S_, P_ = pS[n % 4], P[n % 4]
                    n += 1
                    kb.mm(S_[:], KT[ps_, kt * 128:(kt + 1) * 128], qrhs, True, msk is None, [KT, QT], [S_])
                    if msk is not None:
                        kb.mm(S_[:], g.identb[:], msk[:], False, True, [g.identb, msk], [S_])
                    kb.act(P_[:], S_[:], AF.Exp, [S_], [P_], scale=0.125)
                    kb.mm(pO[ps_, :], V[:, kt, ps_], P_[:], ki == 0, ki == len(keys) - 1, [V, P_], [pO])
                    kb.mm(pD[ps_, :], g.ones[:, 0:64], P_[:], ki == 0, ki == len(keys) - 1, [g.ones, P_], [pD])
            if kind == "a":
                kb.tt("dve", rec[:].rearrange("p (i t) -> p i t", i=4), pD[:].rearrange("p (i t) -> p i t", i=4),
                      esink[:].unsqueeze(2).broadcast_to([128, 4, 128]), ALU.add, [pD, esink], [rec])
                kb.op("dve", lambda e: e.reciprocal(out=rec[:], in_=rec[:]), reads=[rec], writes=[rec])
            else:
                kb.op("dve", lambda e: e.reciprocal(out=rec[:], in_=pD[:]), reads=[pD], writes=[rec])
            kb.tt("dve", YT[:, :, qi * 128:(qi + 1) * 128], pO[:].rearrange("p (i t) -> p i t", i=4),
                  rec[:].rearrange("p (i t) -> p i t", i=4), ALU.mult, [pO, rec], [YT])


def stage_ssd(g, L, b, hT, YsT):
    kb, NB = g.kb, g.NB
    ins = g.ins
    w_in = ins["w_in"]
    nctx = CTX // 128
    segs = [(0, CTX), (CTX, NT)]
    with kb.scope():
        bcT = kb.sb("bcT", [128, 4, NT], BF16)
        xbtok = kb.sb("xbtok", [128, NTILE, 768], BF16)
        zs = kb.sb("zs", [128, NTILE, 512], BF16)
        dt = kb.sb("dt", [128, NTILE, 16], F32)
        da = kb.sb("da", [128, NTILE, 16], F32)
        with kb.scope():
            Wc = kb.sb("Wc", [128, 8, 1024], BF16)
            kb.dma("pool", Wc[:, :, 0:512], w_in[L, :, 1536:2048].rearrange("(k p) c -> p k c", p=128), reads=[w_in], writes=[Wc])
            kb.dma("pool", Wc[:, :, 512:1024], w_in[L, :, 2560:3072].rearrange("(k p) c -> p k c", p=128), reads=[w_in], writes=[Wc])
            Wz = kb.sb("Wz", [128, 8, 512], BF16)
            kb.dma("pool", Wz[:], w_in[L, :, 2048:2560].rearrange("(k p) c -> p k c", p=128), reads=[w_in], writes=[Wz])
            Wdt = kb.sb("Wdt", [128, 8, 16], BF16)
            kb.dma("pool", Wdt[:], w_in[L, :, 3072:3088].rearrange("(k p) c -> p k c", p=128), reads=[w_in], writes=[Wdt])
            cw = kb.sb("cw", [128, 8, 3], F32)
            for k in range(3):
                kb.dma("sp", cw[:, :, k], ins["ssd_conv_w"][L, k, :].rearrange("(c p) -> p c", p=128), reads=[ins["ssd_conv_w"]], writes=[cw],
                       allow_slow_non_contiguous=True)
            cb = kb.sb("cb", [128, 8], F32)
            kb.dma("sp", cb[:], ins["ssd_conv_b"][L, :].rearrange("(c p) -> p c", p=128), reads=[ins["ssd_conv_b"]], writes=[cb],
                   allow_slow_non_contiguous=True)
            dtb = kb.sb("dtb", [128, 16], F32)
            kb.dma("sp", dtb[:], ins["ssd_dt_bias"][L:L + 1, :].broadcast_to([128, 16]), reads=[ins["ssd_dt_bias"]], writes=[dtb])
            negA = kb.sb("negA", [128, 16], F32)
            kb.dma("sp", negA[:], ins["ssd_a_log"][L:L + 1, :, :].rearrange("o a h -> o (a h)").broadcast_to([128, 16]),
                   reads=[ins["ssd_a_log"]], writes=[negA])
            kb.act(negA[:], negA[:], AF.Exp, [negA], [negA])
            kb.ts("dve", negA[:], negA[:], -1.0, None, ALU.mult, None, [negA], [negA])
            raw = kb.sb("raw", [128, NT], BF16)
            acc = kb.sb("acc", [128, NT], F32)
            xT = kb.sb("xT", [128, 4, NT], BF16)
            pp = [kb.ps(f"pp{i}", [128, 512], F32) for i in range(2)]
            pz = [kb.ps(f"pz{i}", [128, 512], F32) for i in range(2)]
            pt = [kb.ps(f"ptt{i}", [128, 768], BF16) for i in range(2)]
            n = 0
            for c in range(8):
                for (t0, tn) in TCH:
                    A = pp[n % 2]
                    n += 1
                    for k in range(8):
                        kb.mm(A[:, :tn], Wc[:, k, c * 128:(c + 1) * 128], hT[:, k, t0:t0 + tn], k == 0, k == 7, [Wc, hT], [A])
                    kb.cp("act", raw[:, t0:t0 + tn], A[:, :tn], [A], [raw])
                kb.ts("dve", acc[:], raw[:], cw[:, c, 1:2], cb[:, c:c + 1], ALU.mult, ALU.add, [raw, cw, cb], [acc])
                for (s0, s1) in segs:
                    kb.stt(acc[:, s0 + 1:s1], raw[:, s0:s1 - 1], cw[:, c, 0:1], acc[:, s0 + 1:s1], ALU.mult, ALU.add, [raw, cw, acc], [acc])
                    kb.stt(acc[:, s0:s1 - 1], raw[:, s0 + 1:s1], cw[:, c, 2:3], acc[:, s0:s1 - 1], ALU.mult, ALU.add, [raw, cw, acc], [acc])
                dst = xT[:, c, :] if c < 4 else bcT[:, c - 4, :]
                kb.act(dst, acc[:], AF.Silu, [acc], [xT if c < 4 else bcT])
            tmp16 = kb.sb("tmp16", [128, 16], F32)
            for i in range(NTILE):
                Z = pz[i % 2]
                tk = slice(i * 128, (i + 1) * 128)
                for k in range(8):
                    kb.mm(Z[:], hT[:, k, tk], Wz[:, k, :], k == 0, k == 7, [hT, Wz], [Z])
                kb.act(zs[:, i, :], Z[:], AF.Silu, [Z], [zs])
                Dp = pp[i % 2]
                for k in range(8):
                    kb.mm(Dp[:, 0:16], hT[:, k, tk], Wdt[:, k, :], k == 0, k == 7, [hT, Wdt], [Dp])
                kb.tt("dve", tmp16[:], Dp[:, 0:16], dtb[:], ALU.add, [Dp, dtb], [tmp16])
                kb.ts("dve", tmp16[:], tmp16[:], 30.0, None, ALU.min, None, [tmp16], [tmp16])
                kb.act(tmp16[:], tmp16[:], AF.Exp, [tmp16], [tmp16])
                kb.act(dt[:, i, :], tmp16[:], AF.Ln, [tmp16], [dt], bias=g.onec[:, 0:1])
                T = pt[i % 2]
                for c in range(6):
                    src = xT[:, c, tk] if c < 4 else bcT[:, c - 4, tk]
                    kb.tr(T[:, c * 128:(c + 1) * 128], src, g.identb[:], [xT if c < 4 else bcT, g.identb], [T])
                kb.cp("dve", xbtok[:, i, :], T[:], [T], [xbtok])
            kb.tt("dve", da[:], dt[:], negA[:].unsqueeze(1).broadcast_to([128, NTILE, 16]), ALU.mult, [dt, negA], [da])
        if g.debug and L == 0 and b == 0:
            dbg_out(g, "ssd_dt", dt, [128, NTILE, 16])
            dbg_out(g, "ssd_xbtok", xbtok, [128, NTILE, 768], BF16)
        import os
        PH = int(os.environ.get("SSD_PHASE", "3"))
        Yacc = kb.sb("Yacc", [128, NTILE, 512], F32)
        if PH < 2:
            return
        with kb.scope():
            selh = kb.sb("selh", [8, 8, 128], F32)
            kb.op("dve", lambda e: e.tensor_copy(out=selh[:], in_=g.identf[0:8, 0:8].unsqueeze(2).broadcast_to([8, 8, 128])),
                  reads=[g.identf], writes=[selh])
            pcs = kb.ps("pcs", [128, 512], F32)
            pR = kb.ps("pR", [128, 1024], F32)
            pCB = kb.ps("pCB", [128, 512], F32)
            pYd = kb.ps("pYd", [128, 512], F32)
            pYo = kb.ps("pYo", [128, 512], F32)
            pSn = kb.ps("pSn", [128, 512], F32)
            ncs = kb.sb("ncs", [128, 8], F32)
            csT = kb.sb("csT", [8, 128], F32)
            Lt = kb.sb("Lt", [128, 8, 128], F32)
            Gt = kb.sb("Gt", [128, 8, 128], BF16)
            ecol = kb.sb("ecol", [128, 8], F32)
            eend = kb.sb("eend", [128, 8], F32)
            wcol = kb.sb("wcol", [128, 8], F32)
            xd = kb.sb("xd", [128, 512], BF16)
            xdd = kb.sb("xdd", [128, 512], BF16)
            tmpy = kb.sb("tmpy", [128, 512], F32)
            S = kb.sb("S", [128, 512], F32)
            Sb = kb.sb("Sb", [128, 512], BF16)
            CUT = int(os.environ.get("SSD_CUT", "0"))
            for d in range(2):
                tri = g.triF if d == 0 else g.triB
                nmask = g.nmF if d == 0 else g.nmB
                e_idx = 127 if d == 0 else 0
                order = list(range(NTILE)) if d == 0 else ([1, 0] + list(range(NTILE - 1, nctx - 1, -1)))
                kb.op("dve", lambda e: e.memset(S[:], 0.0), writes=[S])
                kb.op("pool", lambda e: e.memset(Sb[:], 0.0), writes=[Sb])
                if CUT == 20 and d == 1:
                    return
                for ci in order:
                    if CUT == 21 and d == 1:
                        CUT = int(os.environ.get("SSD_CUT2", "13"))
                    tk = slice(ci * 128, (ci + 1) * 128)
                    dah = da[:, ci, d * 8:(d + 1) * 8]
                    dth = dt[:, ci, d * 8:(d + 1) * 8]
                    xs3 = xbtok[:, ci, 0:512].rearrange("p (h q) -> p h q", h=8)
                    kb.mm(pcs[:, 0:8], tri[:], dah, True, True, [tri, da], [pcs])
                    kb.mm32(pcs[0:8, 128:256], dah, tri[:], True, True, [tri, da], [pcs], pcs[0:32, 511:512], pcs, g.identb)
                    if CUT == 1:
                        dump = kb.sb("dump", [128, 256], F32)
                        kb.cp("dve", dump[:], pcs[:, 0:256], [pcs], [dump])
                        dbg_out(g, "pcs", dump, [128, 256])
                        return
                    kb.ts("dve", ncs[:], pcs[:, 0:8], -1.0, None, ALU.mult, None, [pcs], [ncs])
                    if CUT == 15:
                        return
                    kb.cp("dve", csT[:], pcs[0:8, 128:256], [pcs], [csT])
                    if CUT == 16:
                        return
                    kb.act(ecol[:], ncs[:], AF.Exp, [ncs], [ecol], scale=-1.0)
                    if CUT == 2:
                        return
                    for h in range(8):
                        kb.mm(pR[:, h * 128:(h + 1) * 128], selh[:, h, :], csT[:], True, False, [selh, csT], [pR])
                        kb.mm(pR[:, h * 128:(h + 1) * 128], g.identb[:], nmask[:], False, True, [g.identb, nmask], [pR])
                    if CUT == 3:
                        return
                    for gg in range(2):
                        kb.mm(pCB[:, gg * 128:(gg + 1) * 128], bcT[:, gg, tk], bcT[:, 2 + gg, tk], True, True, [bcT], [pCB])
                    if CUT == 4:
                        return
                    for h in range(8):
                        kb.act(Lt[:, h, :], pR[:, h * 128:(h + 1) * 128], AF.Exp, [pR, ncs], [Lt], bias=ncs[:, h:h + 1])
                    if CUT == 5:
                        return
                    kb.tt("dve", eend[:], Lt[:, :, e_idx], ecol[:], ALU.mult, [Lt, ecol], [eend])
                    if CUT == 6:
                        return
                    for gg in range(2):
                        kb.tt("dve", Gt[:, gg * 4:(gg + 1) * 4, :], Lt[:, gg * 4:(gg + 1) * 4, :],
                              pCB[:, gg * 128:(gg + 1) * 128].unsqueeze(1).broadcast_to([128, 4, 128]), ALU.mult, [Lt, pCB], [Gt])
                    if CUT == 7:
                        return
                    kb.tt("dve", wcol[:], dth, Lt[:, :, e_idx], ALU.mult, [dt, Lt], [wcol])
                    if CUT == 8:
                        return
                    kb.tt("dve", xd[:].rearrange("p (h q) -> p h q", h=8), xs3, dth.unsqueeze(2).broadcast_to([128, 8, 64]), ALU.mult, [xbtok, dt], [xd])
                    if CUT == 9:
                        return
                    kb.tt("pool", xdd[:].rearrange("p (h q) -> p h q", h=8), xs3, wcol[:].unsqueeze(2).broadcast_to([128, 8, 64]), ALU.mult, [xbtok, wcol], [xdd])
                    if CUT == 10:
                        return
                    for h in range(8):
                        kb.mm(pYd[:, h * 64:(h + 1) * 64], Gt[:, h, :], xd[:, h * 64:(h + 1) * 64], True, True, [Gt, xd], [pYd])
                    if CUT == 11:
                        return
                    for h in range(8):
                        kb.mm(pYo[:, h * 64:(h + 1) * 64], bcT[:, 2 + h // 4, tk], Sb[:, h * 64:(h + 1) * 64], True, True, [bcT, Sb], [pYo])
                    if CUT == 12:
                        return
                    for gg in range(2):
                        kb.mm(pSn[:, gg * 256:(gg + 1) * 256], xbtok[:, ci, 512 + gg * 128: 512 + (gg + 1) * 128], xdd[:, gg * 256:(gg + 1) * 256],
                              True, True, [xbtok, xdd], [pSn])
                    if CUT == 13:
                        return
                    kb.tt("dve", tmpy[:].rearrange("p (h q) -> p h q", h=8), pYo[:].rearrange("p (h q) -> p h q", h=8),
                          ecol[:].unsqueeze(2).broadcast_to([128, 8, 64]), ALU.mult, [pYo, ecol], [tmpy])
                    if d == 0:
                        kb.tt("dve", Yacc[:, ci, :], tmpy[:], pYd[:], ALU.add, [tmpy, pYd], [Yacc])
                    else:
                        kb.tt("dve", tmpy[:], tmpy[:], pYd[:], ALU.add, [tmpy, pYd], [tmpy])
                        kb.tt("pool", Yacc[:, ci, :], Yacc[:, ci, :], tmpy[:], ALU.add, [Yacc, tmpy], [Yacc])
                    if CUT == 14:
                        return
                    kb.tt("dve", S[:].rearrange("p (h q) -> p h q", h=8), S[:].rearrange("p (h q) -> p h q", h=8),
                          eend[:].unsqueeze(2).broadcast_to([128, 8, 64]), ALU.mult, [S, eend], [S])
                    kb.tt("dve", S[:], S[:], pSn[:], ALU.add, [S, pSn], [S])
                    kb.cp("act", Sb[:], S[:], [S], [Sb])
                    if CUT == 17:
                        return
                    if CUT == 18 and ci == order[2]:
                        return
                    if CUT == 19 and d == 1 and ci == order[0]:
                        return
        if PH < 3:
            return
        with kb.scope():
            Drep = kb.sb("Drep", [128, 8], F32)
            kb.dma("sp", Drep[:], ins["ssd_d"][L:L + 1, :].broadcast_to([128, 8]), reads=[ins["ssd_d"]], writes=[Drep])
            nwrep = kb.sb("nwrep", [128, 512], F32)
            kb.dma("sp", nwrep[:], ins["ssd_norm_w"][L:L + 1, :].broadcast_to([128, 512]), reads=[ins["ssd_norm_w"]], writes=[nwrep])
            ty = [kb.sb(f"ty{i}", [128, 512], F32) for i in range(2)]
            junk = kb.sb("junk", [128, 512], F32)
            ss = kb.sb("ss", [128, 1], F32)
            yb = [kb.sb(f"yb{i}", [128, 512], BF16) for i in range(2)]
            pt2 = [kb.ps(f"pt2{i}", [128, 512], BF16) for i in range(2)]
            for i in range(NTILE):
                Y, Yb, T = ty[i % 2], yb[i % 2], pt2[i % 2]
                kb.tt("dve", Y[:].rearrange("p (h q) -> p h q", h=8), xbtok[:, i, 0:512].rearrange("p (h q) -> p h q", h=8),
                      Drep[:].unsqueeze(2).broadcast_to([128, 8, 64]), ALU.mult, [xbtok, Drep], [Y])
                kb.tt("dve", Y[:], Y[:], Yacc[:, i, :], ALU.add, [Y, Yacc], [Y])
                kb.tt("dve", Y[:], Y[:], zs[:, i, :], ALU.mult, [Y, zs], [Y])
                kb.act(junk[:], Y[:], AF.Square, [Y], [junk, ss], accum_out=ss[:])
                kb.act(ss[:], ss[:], AF.Sqrt, [ss], [ss], scale=1.0 / 512, bias=g.epsc[:, 0:1])
                kb.op("dve", lambda e: e.reciprocal(out=ss[:], in_=ss[:]), reads=[ss], writes=[ss])
                kb.stt(Yb[:], Y[:], ss[:, 0:1], nwrep[:], ALU.mult, ALU.mult, [Y, ss, nwrep], [Yb])
                for c in range(4):
                    kb.tr(T[:, c * 128:(c + 1) * 128], Yb[:, c * 128:(c + 1) * 128], g.identb[:], [Yb, g.identb], [T])
                kb.cp("act", YsT[:, :, i * 128:(i + 1) * 128], T[:].rearrange("p (c t) -> p c t", c=4), [T], [YsT])


def run_all(g, stages=None):
    kb, NB = g.kb, g.NB
    stages = stages or ("ssd", "s5", "a", "g", "merge", "moe")
    g.epsc = kb.sb("epsc", [128, 1], F32)
    kb.op("dve", lambda e: e.memset(g.epsc[:], EPS), writes=[g.epsc])
    g.onec = kb.sb("onec", [128, 1], F32)
    kb.op("dve", lambda e: e.memset(g.onec[:], 1.0), writes=[g.onec])
    Xin = g.ins["xin"]
    for L in range(g.DEPTH):
        with kb.scope():
            stage_mod(g, L)
            for b in range(NB):
                with kb.scope():
                    dbg = g.debug and L == 0 and b == 0
                    hT = kb.sb("hT", [128, 8, NT], BF16)
                    stage_ln(g, b, Xin if L == 0 else g.X2, 8, 0, hT)
                    YsT = kb.sb("YsT", [128, 4, NT], BF16)
                    if "ssd" in stages:
                        stage_ssd(g, L, b, hT, YsT)
                        if dbg:
                            dbg_out(g, "YsT", YsT, [128, 4, NT], BF16)
                    Y5T = kb.sb("Y5T", [128, 4, NT], BF16)
                    if "s5" in stages:
                        stage_s5(g, L, b, hT, Y5T)
                        if dbg:
                            dbg_out(g, "Y5T", Y5T, [128, 4, NT], BF16)
                    YTa = kb.sb("YTa", [128, 4, NT], BF16)
                    YTg = kb.sb("YTg", [128, 4, NT], BF16)
                    if "a" in stages:
                        stage_attn(g, L, b, hT, YTa, "a")
                    if "g" in stages:
                        stage_attn(g, L, b, hT, YTg, "g")
                    if "merge" in stages:
                        stage_merge(g, L, b, hT, [YTa, YsT, YTg, Y5T], Xin if L == 0 else g.X2)
                if "moe" in stages:
                    stage_moe(g, L, b, L == g.DEPTH - 1 and not g.debug)


def s5_params(g, L):
    kb = g.kb
    ins = g.ins
    P = {}
    TWO_PI = 2 * math.pi
    P["BxT"] = kb.sb("BxT", [128, 2, 2, 16, 128], BF16)
    P["Cx"] = kb.sb("Cx", [128, 2, 16, 128], BF16)
    P["rr"] = kb.sb("rr", [128, 2, 16], F32)
    P["c1"] = kb.sb("c1", [128, 2, 16], F32)
    P["s1"] = kb.sb("s1", [128, 2, 16], F32)
    P["th"] = kb.sb("th", [128, 2, 16], F32)
    P["Dc"] = kb.sb("Dc", [128, 4], F32)
    kb.dma("sp", P["Dc"][:], ins["s5_d"][L, :].rearrange("(c p) -> p c", p=128), reads=[ins["s5_d"]], writes=[P["Dc"]],
           allow_slow_non_contiguous=True)
    with kb.scope():
        Are = kb.sb("Are", [128, 2, 16], F32)
        Aim = kb.sb("Aim", [128, 2, 16], F32)
        Ldt = kb.sb("Ldt", [128, 2, 16], F32)
        Bre = kb.sb("Bre", [128, 16, 16], F32)
        Bim = kb.sb("Bim", [128, 16, 16], F32)
        Cf = kb.sb("Cf", [128, 2, 16, 128], F32)
        kb.op("pool", lambda e: e.memset(Cf[:], 0.0), writes=[Cf])
        for gl in range(2):
            ph = slice(gl * 64, (gl + 1) * 64)
            for d in range(2):
                kb.dma("sp", Are[ph, d, :], ins["s5_a_re"][L, d, gl::2, :].rearrange("j n -> n j"), reads=[ins["s5_a_re"]], writes=[Are], allow_slow_non_contiguous=True)
                kb.dma("sp", Aim[ph, d, :], ins["s5_a_im"][L, d, gl::2, :].rearrange("j n -> n j"), reads=[ins["s5_a_im"]], writes=[Aim], allow_slow_non_contiguous=True)
                kb.dma("sp", Ldt[ph, d, :], ins["s5_log_dt"][L, d:d + 1, gl::2].broadcast_to([64, 16]), reads=[ins["s5_log_dt"]], writes=[Ldt], allow_slow_non_contiguous=True)
            kb.dma("sp", Bre[ph, :, :], ins["s5_b_re"][L, gl::2, :, :].rearrange("j n i -> n j i"), reads=[ins["s5_b_re"]], writes=[Bre], allow_slow_non_contiguous=True)
            kb.dma("act", Bim[ph, :, :], ins["s5_b_im"][L, gl::2, :, :].rearrange("j n i -> n j i"), reads=[ins["s5_b_im"]], writes=[Bim], allow_slow_non_contiguous=True)
            for jr in range(4):
                off = 32 * jr + 16 * gl
                for ri, nm in enumerate(("s5_c_re", "s5_c_im")):
                    for jq in range(4):
                        j = jq * 4 + jr
                        kb.dma("sp" if ri == 0 else "act", Cf[ph, ri, j, off:off + 16],
                               ins[nm][L, 2 * j + gl, :, :].rearrange("o n -> n o"), reads=[ins[nm]], writes=[Cf], allow_slow_non_contiguous=True)
        kb.cp("act", P["Cx"][:, 0], Cf[:, 0], [Cf], [P["Cx"]])
        kb.ts("dve", P["Cx"][:, 1], Cf[:, 1], -1.0, None, ALU.mult, None, [Cf], [P["Cx"]])
        dtt = kb.sb("dtt", [128, 2, 16], F32)
        kb.act(dtt[:], Ldt[:], AF.Exp, [Ldt], [dtt])
        kb.ts("dve", Are[:], Are[:], -1e-4, None, ALU.min, None, [Are], [Are])
        xre = kb.sb("xre", [128, 2, 16], F32)
        kb.tt("dve", xre[:], Are[:], dtt[:], ALU.mult, [Are, dtt], [xre])
        kb.tt("dve", P["th"][:], Aim[:], dtt[:], ALU.mult, [Aim, dtt], [P["th"]])
        kb.act(P["rr"][:], xre[:], AF.Exp, [xre], [P["rr"]])
        sc = kb.sb("sc", [128, 2, 2, 16], F32)
        s5_sincos(g, P["th"][:].rearrange("p d j -> p (d j)"), P["s1"][:].rearrange("p d j -> p (d j)"),
                  P["c1"][:].rearrange("p d j -> p (d j)"), [P["th"]], [P["s1"], P["c1"]], 32)
        nr = kb.sb("nr", [128, 2, 16], F32)
        ni = kb.sb("ni", [128, 2, 16], F32)
        kb.tt("dve", nr[:], P["rr"][:], P["c1"][:], ALU.mult, [P["rr"], P["c1"]], [nr])
        kb.ts("dve", nr[:], nr[:], -1.0, None, ALU.add, None, [nr], [nr])
        kb.tt("dve", ni[:], P["rr"][:], P["s1"][:], ALU.mult, [P["rr"], P["s1"]], [ni])
        den = kb.sb("den", [128, 2, 16], F32)
        t_ = kb.sb("t_", [128, 2, 16], F32)
        kb.tt("dve", den[:], Are[:], Are[:], ALU.mult, [Are], [den])
        kb.tt("dve", t_[:], Aim[:], Aim[:], ALU.mult, [Aim], [t_])
        kb.tt("dve", den[:], den[:], t_[:], ALU.add, [den, t_], [den])
        kb.op("dve", lambda e: e.reciprocal(out=den[:], in_=den[:]), reads=[den], writes=[den])
        kre = kb.sb("kre", [128, 2, 16], F32)
        kim = kb.sb("kim", [128, 2, 16], F32)
        kb.tt("dve", kre[:], nr[:], Are[:], ALU.mult, [nr, Are], [kre])
        kb.tt("dve", t_[:], ni[:], Aim[:], ALU.mult, [ni, Aim], [t_])
        kb.tt("dve", kre[:], kre[:], t_[:], ALU.add, [kre, t_], [kre])
        kb.tt("dve", kre[:], kre[:], den[:], ALU.mult, [kre, den], [kre])
        kb.tt("dve", kim[:], ni[:], Are[:], ALU.mult, [ni, Are], [kim])
        kb.tt("dve", t_[:], nr[:], Aim[:], ALU.mult, [nr, Aim], [t_])
        kb.tt("dve", kim[:], kim[:], t_[:], ALU.subtract, [kim, t_], [kim])
        kb.tt("dve", kim[:], kim[:], den[:], ALU.mult, [kim, den], [kim])
        Bx = kb.sb("Bx", [128, 16, 128], F32)
        tb1 = kb.sb("tb1", [128, 16, 16], F32)
        tb2 = kb.sb("tb2", [128, 16, 16], F32)
        ptb = [kb.ps(f"ptb{i}", [128, 512], F32) for i in range(2)]
        n = 0
        for d in range(2):
            for ri in range(2):
                X1, X2 = (Bre, Bim) if ri == 0 else (Bim, Bre)
                kb.tt("dve", tb1[:], X1[:], kre[:, d, :].unsqueeze(2).broadcast_to([128, 16, 16]), ALU.mult, [X1, kre], [tb1])
                kb.tt("dve", tb2[:], X2[:], kim[:, d, :].unsqueeze(2).broadcast_to([128, 16, 16]), ALU.mult, [X2, kim], [tb2])
                kb.tt("dve", tb1[:], tb1[:], tb2[:], ALU.subtract if ri == 0 else ALU.add, [tb1, tb2], [tb1])
                kb.op("pool", lambda e: e.memset(Bx[:], 0.0), writes=[Bx])
                for gl in range(2):
                    ph = slice(gl * 64, (gl + 1) * 64)
                    for jr in range(4):
                        off = 32 * jr + 16 * gl
                        kb.cp("dve", Bx[ph, jr::4, off:off + 16], tb1[ph, jr::4, :], [tb1], [Bx])
                for jq in range(4):
                    T = ptb[n % 2]
                    n += 1
                    for jj in range(4):
                        j = jq * 4 + jj
                        kb.tr(T[:, jj * 128:(jj + 1) * 128], Bx[:, j, :], g.identf[:], [Bx, g.identf], [T])
                    kb.cp("dve", P["BxT"][:, d, ri, jq * 4:(jq + 1) * 4, :], T[:].rearrange("p (a b) -> p a b", a=4), [T], [P["BxT"]])
    g.s5 = P


def s5_sincos(g, ang, s_out, c_out, R, W, n):
    kb = g.kb
    TWO_PI = 2 * math.pi
    with kb.scope():
        kf = kb.sb("kf", [128, n], F32)
        ki = kb.sb("ki", [128, n], I32)
        r = kb.sb("r", [128, n], F32)
        m = kb.sb("m", [128, n], F32)
        kb.ts("dve", kf[:], ang, 1.0 / TWO_PI, None, ALU.mult, None, R, [kf])
        kb.cp("dve", ki[:], kf[:], [kf], [ki])
        kb.cp("dve", kf[:], ki[:], [ki], [kf])
        kb.stt(r[:], kf[:], -TWO_PI, ang, ALU.mult, ALU.add, [kf] + list(R), [r])
        for shift, out in ((0.0, s_out), (math.pi / 2, c_out)):
            src = r
            if shift:
                kb.ts("dve", m[:], r[:], shift, None, ALU.add, None, [r], [m])
                src = m
            for _ in range(2):
                kb.ts("dve", kf[:], src[:], math.pi, -TWO_PI, ALU.is_gt, ALU.mult, [src], [kf])
                kb.tt("dve", m[:], src[:], kf[:], ALU.add, [src, kf], [m])
                src = m
                kb.ts("dve", kf[:], m[:], -math.pi, TWO_PI, ALU.is_lt, ALU.mult, [m], [kf])
                kb.tt("dve", m[:], m[:], kf[:], ALU.add, [m, kf], [m])
            kb.act(out, m[:], AF.Sin, [m], W)


def stage_s5(g, L, b, hT, Y5T):
    kb, NB = g.kb, g.NB
    ins = g.ins
    w_in = ins["w_in"]
    T = 256
    import os
    CUT = int(os.environ.get("S5_CUT", "0"))
    chunks = [(0, CTX)] + [(CTX + i * T, T) for i in range(SEQ // T)]
    with kb.scope():
        s5_params(g, L)
        P = g.s5
        uT = Y5T
        yacc = kb.sb("yacc", [128, 4, NT], F32)
        with kb.scope():
            Wu = kb.sb("Wu", [128, 8, 512], BF16)
            kb.dma("pool", Wu[:], w_in[L, :, 3088:3600].rearrange("(k p) c -> p k c", p=128), reads=[w_in], writes=[Wu])
            pu = [kb.ps(f"pu{i}", [128, 512], F32) for i in range(2)]
            n = 0
            for c in range(4):
                for (t0, tn) in TCH:
                    A = pu[n % 2]
                    n += 1
                    for k in range(8):
                        kb.mm(A[:, :tn], Wu[:, k, c * 128:(c + 1) * 128], hT[:, k, t0:t0 + tn], k == 0, k == 7, [Wu, hT], [A])
                    kb.cp("dve", uT[:, c, t0:t0 + tn], A[:, :tn], [A], [uT])
                    kb.ts("dve", yacc[:, c, t0:t0 + tn], A[:, :tn], P["Dc"][:, c:c + 1], None, ALU.mult, None, [A, P["Dc"]], [yacc])
        if CUT == 2:
            return
        with kb.scope():
            iot = kb.sb("iot", [128, T], F32)
            kb.op("pool", lambda e: e.iota(iot[:], pattern=[[1, T]], base=0, channel_multiplier=0, allow_small_or_imprecise_dtypes=True), writes=[iot])
            ang = kb.sb("ang", [128, T], F32)
            ctab = kb.sb("ctab", [128, T], F32)
            stab = kb.sb("stab", [128, T], F32)
            rrow = kb.sb("rrow", [128, T], F32)
            pb = [kb.ps(f"pbu{i}", [128, 512], F32) for i in range(4)]
            py = [kb.ps(f"py{i}", [128, 512], F32) for i in range(2)]
            v = [kb.sb(f"v{i}", [128, T], F32) for i in range(2)]
            tmp = [kb.sb(f"tmp{i}", [128, T], F32) for i in range(4)]
            sh = [kb.sb(f"sh{i}", [128, T], F32) for i in range(2)]
            sf = [kb.sb(f"sf{i}", [128, T], F32) for i in range(2)]
            sbf = [[kb.sb(f"sbf{i}_{k}", [128, T], BF16) for i in range(2)] for k in range(2)]
            init = kb.sb("init", [128, 2], F32)
            it = kb.sb("it", [128, 2], F32)
            n = 0
            for d in range(2):
                for j in range(16):
                    kc = j // 4
                    thc = P["th"][:, d, j:j + 1]
                    kb.ts("dve", ang[:], iot[:], thc, None, ALU.mult, None, [iot, P["th"]], [ang])
                    s5_sincos(g, ang[:], stab[:], ctab[:], [ang], [stab, ctab], T)
                    kb.ts("dve", rrow[:], iot[:], 0.0, P["rr"][:, d, j:j + 1], ALU.mult, ALU.add, [iot, P["rr"]], [rrow])
                    kb.op("dve", lambda e: e.memset(init[:], 0.0), writes=[init])
                    if CUT == 3:
                        return
                    order = chunks if d == 0 else [chunks[0]] + chunks[:0:-1]
                    for ci, (t0, tn) in enumerate(order):
                        def rv(ap_full):
                            a = ap_full[:, 0:tn]
                            return a if d == 0 else a[:, ::-1]
                        Bre_p, Bim_p = pb[(2 * n) % 4], pb[(2 * n + 1) % 4]
                        Y = py[n % 2]
                        SB = sbf[n % 2]
                        n += 1
                        kb.mm(Bre_p[:, :tn], P["BxT"][:, d, 0, j, :], uT[:, kc, t0:t0 + tn], True, True, [P["BxT"], uT], [Bre_p])
                        kb.mm(Bim_p[:, :tn], P["BxT"][:, d, 1, j, :], uT[:, kc, t0:t0 + tn], True, True, [P["BxT"], uT], [Bim_p])
                        c_, s_ = ctab[:, :tn], stab[:, :tn]
                        kb.tt("dve", tmp[0][:, :tn], rv(Bre_p), c_, ALU.mult, [Bre_p, ctab], [tmp[0]])
                        kb.tt("dve", tmp[1][:, :tn], rv(Bim_p), s_, ALU.mult, [Bim_p, stab], [tmp[1]])
                        kb.tt("dve", tmp[2][:, :tn], rv(Bim_p), c_, ALU.mult, [Bim_p, ctab], [tmp[2]])
                        kb.tt("dve", tmp[3][:, :tn], rv(Bre_p), s_, ALU.mult, [Bre_p, stab], [tmp[3]])
                        kb.tt("pool", v[0][:, :tn], tmp[0][:, :tn], tmp[1][:, :tn], ALU.add, [tmp[0], tmp[1]], [v[0]])
                        kb.tt("pool", v[1][:, :tn], tmp[2][:, :tn], tmp[3][:, :tn], ALU.subtract, [tmp[2], tmp[3]], [v[1]])
                        for ri in range(2):
                            kb.op("dve", lambda e: e.tensor_tensor_scan(out=sh[ri][:, :tn], data0=rrow[:, :tn], data1=v[ri][:, :tn],
                                                                          initial=init[:, ri:ri + 1], op0=ALU.mult, op1=ALU.add),
                                  reads=[rrow, v[ri], init], writes=[sh[ri]])
                        kb.tt("pool", tmp[0][:, :tn], sh[0][:, :tn], c_, ALU.mult, [sh[0], ctab], [tmp[0]])
                        kb.tt("pool", tmp[1][:, :tn], sh[1][:, :tn], s_, ALU.mult, [sh[1], stab], [tmp[1]])
                        kb.tt("pool", tmp[2][:, :tn], sh[0][:, :tn], s_, ALU.mult, [sh[0], stab], [tmp[2]])
                        kb.tt("pool", tmp[3][:, :tn], sh[1][:, :tn], c_, ALU.mult, [sh[1], ctab], [tmp[3]])
                        kb.tt("dve", sf[0][:, :tn], tmp[0][:, :tn], tmp[1][:, :tn], ALU.subtract, [tmp[0], tmp[1]], [sf[0]])
                        kb.tt("dve", sf[1][:, :tn], tmp[2][:, :tn], tmp[3][:, :tn], ALU.add, [tmp[2], tmp[3]], [sf[1]])
                        kb.ts("dve", it[:, 0:1], sf[0][:, tn - 1:tn], P["c1"][:, d, j:j + 1], None, ALU.mult, None, [sf[0], P["c1"]], [it])
                        kb.ts("dve", it[:, 1:2], sf[0][:, tn - 1:tn], P["s1"][:, d, j:j + 1], None, ALU.mult, None, [sf[0], P["s1"]], [it])
                        kb.stt(init[:, 0:1], sf[1][:, tn - 1:tn], P["s1"][:, d, j:j + 1], it[:, 0:1], ALU.mult, ALU.subtract, [sf[1], P["s1"], it], [init])
                        kb.ts("dve", init[:, 0:1], init[:, 0:1], -1.0, None, ALU.mult, None, [init], [init])
                        kb.stt(init[:, 1:2], sf[1][:, tn - 1:tn], P["c1"][:, d, j:j + 1], it[:, 1:2], ALU.mult, ALU.add, [sf[1], P["c1"], it], [init])
                        for ri in range(2):
                            dstv = SB[ri][:, :tn] if d == 0 else SB[ri][:, :tn][:, ::-1]
                            kb.cp("act", dstv, sf[ri][:, :tn], [sf[ri]], [SB[ri]])
                        kb.mm(Y[:, :tn], P["Cx"][:, 0, j, :], SB[0][:, :tn], True, False, [P["Cx"], SB[0]], [Y])
                        kb.mm(Y[:, :tn], P["Cx"][:, 1, j, :], SB[1][:, :tn], False, True, [P["Cx"], SB[1]], [Y])
                        kb.tt("dve", yacc[:, kc, t0:t0 + tn], yacc[:, kc, t0:t0 + tn], Y[:, :tn], ALU.add, [yacc, Y], [yacc])
                        if CUT == 4:
                            return
                    if CUT == 5:
                        return
                if CUT == 6:
                    return
        if g.debug and L == 0 and b == 0:
            dbg_out(g, "s5_yacc", yacc, [128, 4, NT])
        if CUT == 7:
            return
        with kb.scope():
            Wg = kb.sb("Wg", [128, 4, 1024], BF16)
            kb.dma("pool", Wg[:], ins["s5_w_glu"][L, :, :].rearrange("(k p) c -> p k c", p=128), reads=[ins["s5_w_glu"]], writes=[Wg])
            gy = kb.sb("gy", [128, 4, NT], BF16)
            x2 = kb.sb("x2", [128, NT], F32)
            for c in range(4):
                x = yacc[:, c, :]
                kb.act(x2[:], x, AF.Square, [yacc], [x2])
                kb.ts("dve", x2[:], x2[:], 0.044715 * math.sqrt(2 / math.pi), math.sqrt(2 / math.pi), ALU.mult, ALU.add, [x2], [x2])
                kb.tt("dve", x2[:], x2[:], x, ALU.mult, [x2, yacc], [x2])
                kb.act(x2[:], x2[:], AF.Tanh, [x2], [x2])
                kb.ts("dve", x2[:], x2[:], 1.0, 0.5, ALU.add, ALU.mult, [x2], [x2])
                kb.tt("dve", gy[:, c, :], x2[:], x, ALU.mult, [x2, yacc], [gy])
            pa = [kb.ps(f"pa{i}", [128, 512], F32) for i in range(2)]
            pbb = [kb.ps(f"pbb{i}", [128, 512], F32) for i in range(2)]
            sg = [kb.sb(f"sg{i}", [128, 512], F32) for i in range(2)]
            n = 0
            for c in range(4):
                for (t0, tn) in TCH:
                    A, B, SG = pa[n % 2], pbb[n % 2], sg[n % 2]
                    n += 1
                    for k in range(4):
                        kb.mm(A[:, :tn], Wg[:, k, c * 128:(c + 1) * 128], gy[:, k, t0:t0 + tn], k == 0, k == 3, [Wg, gy], [A])
                    for k in range(4):
                        kb.mm(B[:, :tn], Wg[:, k, 512 + c * 128:512 + (c + 1) * 128], gy[:, k, t0:t0 + tn], k == 0, k == 3, [Wg, gy], [B])
                    kb.act(SG[:, :tn], B[:, :tn], AF.Sigmoid, [B], [SG])
                    kb.tt("dve", Y5T[:, c, t0:t0 + tn], A[:, :tn], SG[:, :tn], ALU.mult, [A, SG], [Y5T])


def load_lnrep(g, L, names):
    kb, ins = g.kb, g.ins
    g.lnrep = {}
    for nm in names:
        t = kb.sb(nm + "rep", [128, D], F32)
        kb.dma("sp", t[:], ins[nm][L:L + 1, :].broadcast_to([128, D]), reads=[ins[nm]], writes=[t])
        g.lnrep[nm] = t


def post_norm_tile(g, pool, mix_ap, mixT, xt, grep, gname, bname, outT, out_ap):
    kb = g.kb
    t, st, mv, rs = pool["t"], pool["st"], pool["mv"], pool["rs"]
    kb.tt("dve", t[:], mix_ap, grep[:], ALU.mult, [mixT, grep], [t])
    kb.stt(t[:], xt[:], float(g.ALPHA), t[:], ALU.mult, ALU.add, [xt, t], [t])
    for hf in range(2):
        kb.op("dve", lambda e: e.bn_stats(out=st[:, hf, :], in_=t[:, hf * 512:(hf + 1) * 512]), reads=[t], writes=[st])
    kb.op("dve", lambda e: e.bn_aggr(out=mv[:], in_=st[:].rearrange("p a b -> p (a b)")), reads=[st], writes=[mv])
    kb.act(rs[:], mv[:, 1:2], AF.Sqrt, [mv], [rs], bias=g.epsc[:, 0:1])
    kb.op("dve", lambda e: e.reciprocal(out=rs[:], in_=rs[:]), reads=[rs], writes=[rs])
    kb.ts("dve", t[:], t[:], mv[:, 0:1], rs[:, 0:1], ALU.subtract, ALU.mult, [t, mv, rs], [t])
    kb.tt("pool", t[:], t[:], g.lnrep[gname][:], ALU.mult, [t, g.lnrep[gname]], [t])
    kb.tt("pool", t[:], t[:], g.lnrep[bname][:], ALU.add, [t, g.lnrep[bname]], [t])
    kb.dma("sp", out_ap, t[:], reads=[t], writes=[outT])


def stage_merge(g, L, b, hT, Ys, Xsrc):
    kb, NB = g.kb, g.NB
    ins = g.ins
    w_in = ins["w_in"]
    nctx = CTX // 128
    with kb.scope():
        zT = kb.sb("zT", [128, 8, NT], BF16)
        with kb.scope():
            Wg = [kb.sb(f"Wgm{i}", [128, 8, 4, 128], BF16) for i in range(2)]
            Wb = [kb.sb(f"Wbm{i}", [128, 4, 4, 128], BF16) for i in range(2)]
            zf = kb.sb("zf", [128, 512], F32)
            sg = [kb.sb(f"msg{i}", [128, 512], F32) for i in range(2)]
            pG = [kb.ps(f"pG{i}", [128, 512], F32) for i in range(2)]
            pM = [kb.ps(f"pM{i}", [128, 512], F32) for i in range(2)]
            n = 0
            for fc in range(8):
                WG, WB = Wg[fc % 2], Wb[fc % 2]
                for br in range(4):
                    c0 = 3600 + br * 1024 + fc * 128
                    kb.dma("pool", WG[:, :, br, :], w_in[L, :, c0:c0 + 128].rearrange("(k p) c -> p k c", p=128), reads=[w_in], writes=[WG])
                    wsrc = ins["w_branch"]
                    if br in (0, 2):
                        for i in range(4):
                            for j in range(2):
                                h = 4 * j + i
                                kb.dma("pool", WB[j * 64:(j + 1) * 64, br, i, :], wsrc[L, br, h * 64:(h + 1) * 64, fc * 128:(fc + 1) * 128],
                                       reads=[wsrc], writes=[WB])
                    else:
                        kb.dma("pool", WB[:, br, :, :], wsrc[L, br, :, fc * 128:(fc + 1) * 128].rearrange("(k p) c -> p k c", p=128),
                               reads=[wsrc], writes=[WB])
                for (t0, tn) in TCH:
                    for br in range(4):
                        G_, M_, S_ = pG[n % 2], pM[n % 2], sg[n % 2]
                        n += 1
                        for k in range(8):
                            kb.mm(G_[:, :tn], WG[:, k, br, :], hT[:, k, t0:t0 + tn], k == 0, k == 7, [WG, hT], [G_])
                        for k in range(4):
                            kb.mm(M_[:, :tn], WB[:, br, k, :], Ys[br][:, k, t0:t0 + tn], k == 0, k == 3, [WB, Ys[br]], [M_])
                        kb.act(S_[:, :tn], G_[:, :tn], AF.Sigmoid, [G_], [S_])
                        if br == 0:
                            kb.tt("dve", zf[:, :tn], M_[:, :tn], S_[:, :tn], ALU.mult, [M_, S_], [zf])
                        else:
                            kb.tt("dve", S_[:, :tn], M_[:, :tn], S_[:, :tn], ALU.mult, [M_, S_], [S_])
                            if br < 3:
                                kb.tt("pool", zf[:, :tn], zf[:, :tn], S_[:, :tn], ALU.add, [zf, S_], [zf])
                            else:
                                kb.tt("pool", zT[:, fc, t0:t0 + tn], zf[:, :tn], S_[:, :tn], ALU.add, [zf, S_], [zT])
        with kb.scope():
            load_lnrep(g, L, ("ln1_g", "ln1_b"))
            Wo = kb.sb("Wo", [128, 8, D], BF16)
            for hf in range(2):
                kb.dma("pool", Wo[:, :, hf * 512:(hf + 1) * 512], ins["w_out"][L, :, hf * 512:(hf + 1) * 512].rearrange("(k p) c -> p k c", p=128),
                       reads=[ins["w_out"]], writes=[Wo])
            pX = [kb.ps(f"pX{i}", [128, D], F32) for i in range(2)]
            xts = [kb.sb(f"mxt{i}", [128, D], F32) for i in range(2)]
            pool = {"t": kb.sb("pn_t", [128, D], F32), "st": kb.sb("pn_st", [128, 2, 6], F32),
                    "mv": kb.sb("pn_mv", [128, 2], F32), "rs": kb.sb("pn_rs", [128, 1], F32)}
            for i in range(NTILE):
                PX, xt = pX[i % 2], xts[i % 2]
                kb.dma("act", xt[:], Xsrc[b, i * 128:(i + 1) * 128, :], reads=[Xsrc], writes=[xt])
                for hf in range(2):
                    for k in range(8):
                        kb.mm(PX[:, hf * 512:(hf + 1) * 512], zT[:, k, i * 128:(i + 1) * 128], Wo[:, k, hf * 512:(hf + 1) * 512], k == 0, k == 7, [zT, Wo], [PX])
                grep = g.greps[NB if i < nctx else b][0]
                post_norm_tile(g, pool, PX[:], PX, xt, grep, "ln1_g", "ln1_b", g.X1, g.X1[b, i * 128:(i + 1) * 128, :])


def stage_moe(g, L, b, last):
    kb, NB = g.kb, g.NB
    ins = g.ins
    nctx = CTX // 128
    t_start = CTX if last else 0
    tiles = list(range(t_start // 128, NTILE))
    chunks = [(t0, min(512, NT - t0)) for t0 in range(t_start, NT, 512)]
    with kb.scope():
        h2T = kb.sb("h2T", [128, 8, NT], BF16)
        stage_ln(g, b, g.X1, 32, 24, h2T)
        Yacc = kb.sb("Yacc2", [128, NTILE, D], F32)
        Wt = kb.sb("Wt", [128, NTILE, 32], F32)
        with kb.scope():
            rw = kb.sb("rw", [128, 8, 32], BF16)
            kb.dma("pool", rw[:], ins["router_w"][:, :].rearrange("(k p) c -> p k c", p=128), reads=[ins["router_w"]], writes=[rw])
            rb = kb.sb("rb", [128, 32], F32)
            kb.dma("sp", rb[:], ins["router_bias"][:].rearrange("(o c) -> o c", o=1).broadcast_to([128, 32]), reads=[ins["router_bias"]], writes=[rb])
            pr_ = [kb.ps(f"prt{i}", [128, 512], F32) for i in range(2)]
            sc = kb.sb("sc", [128, 32], F32)
            sel = kb.sb("sel_", [128, 32], F32)
            eq = kb.sb("eq", [128, 32], F32)
            m1 = kb.sb("m1", [128, 8], F32)
            m2 = kb.sb("m2", [128, 8], F32)
            gs = kb.sb("gs", [128, 8], F32)
            gm = kb.sb("gm", [128, 1], F32)
            v3 = lambda t: t[:].rearrange("p (a b) -> p a b", a=8)
            bc = lambda t: t[:].unsqueeze(2).broadcast_to([128, 8, 4])
            for i in tiles:
                Pp = pr_[i % 2]
                for k in range(8):
                    kb.mm(Pp[:, 0:32], h2T[:, k, i * 128:(i + 1) * 128], rw[:, k, :], k == 0, k == 7, [h2T, rw], [Pp])
                kb.act(sc[:], Pp[:, 0:32], AF.Sigmoid, [Pp], [sc])
                kb.tt("dve", sel[:], sc[:], rb[:], ALU.add, [sc, rb], [sel])
                kb.op("dve", lambda e: e.tensor_reduce(out=m1[:], in_=v3(sel), axis=AX.X, op=ALU.max), reads=[sel], writes=[m1])
                kb.tt("dve", v3(eq), v3(sel), bc(m1), ALU.is_equal, [sel, m1], [eq])
                kb.stt(eq[:], eq[:], -1000.0, sel[:], ALU.mult, ALU.add, [eq, sel], [eq])
                kb.op("dve", lambda e: e.tensor_reduce(out=m2[:], in_=v3(eq), axis=AX.X, op=ALU.max), reads=[eq], writes=[m2])
                kb.tt("dve", gs[:], m1[:], m2[:], ALU.add, [m1, m2], [gs])
                kb.op("dve", lambda e: e.tensor_reduce(out=gm[:], in_=gs[:], axis=AX.X, op=ALU.max), reads=[gs], writes=[gm])
                kb.ts("dve", gs[:], gs[:], gm[:, 0:1], None, ALU.is_equal, None, [gs, gm], [gs])
                kb.tt("dve", v3(eq), v3(sel), bc(m2), ALU.is_ge, [sel, m2], [eq])
                kb.tt("dve", v3(eq), v3(eq), bc(gs), ALU.mult, [eq, gs], [eq])
                kb.tt("dve", eq[:], eq[:], sc[:], ALU.mult, [eq, sc], [eq])
                kb.op("dve", lambda e: e.tensor_reduce(out=gm[:], in_=eq[:], axis=AX.X, op=ALU.add), reads=[eq], writes=[gm])
                kb.op("dve", lambda e: e.reciprocal(out=gm[:], in_=gm[:]), reads=[gm], writes=[gm])
                kb.ts("dve", Wt[:, i, :], eq[:], gm[:, 0:1], None, ALU.mult, None, [eq, gm], [Wt])
        if g.debug and L == 0 and b == 0:
            dbg_out(g, "Wt", Wt, [128, NTILE, 32])
        with kb.scope():
            Wgu = [kb.sb(f"Wgu{i}", [128, 2, 8, 512], BF16) for i in range(2)]
            Wd = [kb.sb(f"Wd{i}", [128, 4, D], BF16) for i in range(2)]
            AT = [kb.sb(f"AT{i}", [128, 4, 512], BF16) for i in range(2)]
            sgm = [kb.sb(f"sgm{i}", [128, 512], BF16) for i in range(2)]
            pGm = [kb.ps(f"pGm{i}", [128, 512], F32) for i in range(2)]
            pUm = [kb.ps(f"pUm{i}", [128, 512], F32) for i in range(2)]
            pY = [kb.ps(f"pY{i}", [128, D], F32) for i in range(2)]
            n = 0
            ny = 0
            na = 0
            for e_ in range(32):
                WGU, WD = Wgu[e_ % 2], Wd[e_ % 2]
                kb.dma("pool", WGU[:, 0], ins["moe_w_gate"][L, e_, :, :].rearrange("(k p) c -> p k c", p=128), reads=[ins["moe_w_gate"]], writes=[WGU])
                kb.dma("pool", WGU[:, 1], ins["moe_w_up"][L, e_, :, :].rearrange("(k p) c -> p k c", p=128), reads=[ins["moe_w_up"]], writes=[WGU])
                for hf in range(2):
                    kb.dma("pool", WD[:, :, hf * 512:(hf + 1) * 512], ins["moe_w_down"][L, e_, :, hf * 512:(hf + 1) * 512].rearrange("(k p) c -> p k c", p=128),
                           reads=[ins["moe_w_down"]], writes=[WD])
                for (t0, tn) in chunks:
                    A_ = AT[na % 2]
                    na += 1
                    for fcx in range(4):
                        G_, U_, S_ = pGm[n % 2], pUm[n % 2], sgm[n % 2]
                        n += 1
                        for k in range(8):
                            kb.mm(G_[:, :tn], WGU[:, 0, k, fcx * 128:(fcx + 1) * 128], h2T[:, k, t0:t0 + tn], k == 0, k == 7, [WGU, h2T], [G_])
                        for k in range(8):
                            kb.mm(U_[:, :tn], WGU[:, 1, k, fcx * 128:(fcx + 1) * 128], h2T[:, k, t0:t0 + tn], k == 0, k == 7, [WGU, h2T], [U_])
                        kb.act(S_[:, :tn], G_[:, :tn], AF.Silu, [G_], [S_])
                        kb.tt("dve", A_[:, fcx, :tn], U_[:, :tn], S_[:, :tn], ALU.mult, [U_, S_], [A_])
                    for ti in range(tn // 128):
                        i = t0 // 128 + ti
                        Y_ = pY[ny % 2]
                        ny += 1
                        for hf in range(2):
                            for k in range(4):
                                kb.mm(Y_[:, hf * 512:(hf + 1) * 512], A_[:, k, ti * 128:(ti + 1) * 128], WD[:, k, hf * 512:(hf + 1) * 512], k == 0, k == 3, [A_, WD], [Y_])
                        if e_ == 0:
                            kb.ts("dve", Yacc[:, i, :], Y_[:], Wt[:, i, e_:e_ + 1], None, ALU.mult, None, [Y_, Wt], [Yacc])
                        else:
                            kb.stt(Yacc[:, i, :], Y_[:], Wt[:, i, e_:e_ + 1], Yacc[:, i, :], ALU.mult, ALU.add, [Y_, Wt, Yacc], [Yacc])
        if g.debug and L == 0 and b == 0:
            dbg_out(g, "ffn", Yacc, [128, NTILE, D])
        with kb.scope():
            load_lnrep(g, L, ("ln2_g", "ln2_b"))
            xts = [kb.sb(f"fxt{i}", [128, D], F32) for i in range(2)]
            pool = {"t": kb.sb("pn2_t", [128, D], F32), "st": kb.sb("pn2_st", [128, 2, 6], F32),
                    "mv": kb.sb("pn2_mv", [128, 2], F32), "rs": kb.sb("pn2_rs", [128, 1], F32)}
            for i in tiles:
                xt = xts[i % 2]
                kb.dma("act", xt[:], g.X1[b, i * 128:(i + 1) * 128, :], reads=[g.X1], writes=[xt])
                grep = g.greps[NB if i < nctx else b][1]
                if last:
                    oT, oap = g.out, g.out[b, (i - nctx) * 128:(i - nctx + 1) * 128, :]
                else:
                    oT, oap = g.X2, g.X2[b, i * 128:(i + 1) * 128, :]
                post_norm_tile(g, pool, Yacc[:, i, :], Yacc, xt, grep, "ln2_g", "ln2_b", oT, oap)


WEIGHT_NAMES = ["mod_w", "mod_b", "w_in", "wa_sink", "ga_q_norm", "ga_k_norm", "ssd_conv_w", "ssd_conv_b", "ssd_dt_bias",
                "ssd_a_log", "ssd_d", "ssd_norm_w", "s5_a_re", "s5_a_im", "s5_log_dt", "s5_b_re", "s5_b_im", "s5_c_re",
                "s5_c_im", "s5_d", "s5_w_glu", "w_branch", "w_out", "ln1_g", "ln1_b", "ln2_g", "ln2_b", "router_w",
                "router_bias", "moe_w_gate", "moe_w_up", "moe_w_down"]


def kernel(**inputs):
    NB = 2
    n_cores = 8
    nc, g = build(NB=NB, DEPTH=4, debug=False)
    consts = host_consts()
    x = np.asarray(inputs["x"], dtype=np.float32)
    ctx = np.asarray(inputs["ctx"], dtype=np.float32)
    c = np.asarray(inputs["c"], dtype=np.float32)
    c_ctx = np.asarray(inputs["c_ctx"], dtype=np.float32)
    weights = {k: np.ascontiguousarray(np.asarray(inputs[k], dtype=np.float32)) for k in WEIGHT_NAMES}
    in_maps = []
    for core in range(n_cores):
        bs = slice(core * NB, (core + 1) * NB)
        m = {}
        m["xin"] = np.ascontiguousarray(np.concatenate([ctx[bs], x[bs]], axis=1))
        m["cvec"] = np.ascontiguousarray(np.concatenate([c[bs], c_ctx[None, :]], axis=0))
        m.update(weights)
        m.update(consts)
        in_maps.append(m)
    res = run_bass_kernel_spmd(nc, in_maps, core_ids=list(range(n_cores)))
    out = np.concatenate([np.asarray(res.results[i]["out"]) for i in range(n_cores)], axis=0)
    return out.astype(np.float32)
```

```python
import contextlib
import math
import numpy as np
import ml_dtypes
import concourse.bass as bass
import concourse.mybir as mybir
from concourse.bass_utils import run_bass_kernel_spmd

F32 = mybir.dt.float32
BF16 = mybir.dt.bfloat16
I32 = mybir.dt.int32
AF = mybir.ActivationFunctionType
ALU = mybir.AluOpType
AX = mybir.AxisListType

SEM_LIM = 20000
ND = 10

D = 1024
CTX = 256
SEQ = 2048
NT = CTX + SEQ
NTILE = NT // 128
GRID_W = 64
EPS = 1e-6
D_IN = 7696
TCH = [(0, 512), (512, 512), (1024, 512), (1536, 512), (2048, 256)]
NEG = -30000.0


class TT:
    __slots__ = ("h", "name", "w", "r", "ps")

    def __init__(self, h, name, ps=False):
        self.h = h
        self.name = name
        self.w = None
        self.r = {}
        self.ps = ps

    def __getitem__(self, idx):
        return self.h[idx]


class KB:
    def __init__(self, nc):
        self.nc = nc
        self.es = contextlib.ExitStack()
        self.alloc_es = self.es
        self.engines = {"pe": nc.tensor, "dve": nc.vector, "act": nc.scalar,
                        "pool": nc.gpsimd, "sp": nc.sync}
        self.cur = {}
        self.known = {k: {} for k in self.engines}
        self.allsems = []
        self.dpool = {}
        self.dnext = {}
        self.nsem = 0
        self.uid = 0
        self.ninst = 0
        self.pe_sems = set()

    def _name(self, name):
        self.uid += 1
        return f"{name}_{self.uid}"

    def sb(self, name, shape, dtype=F32):
        n = self._name(name)
        h = self.alloc_es.enter_context(self.nc.sbuf_tensor(n, list(shape), dtype))
        return TT(h, n)

    def ps(self, name, shape, dtype=F32):
        n = self._name(name)
        h = self.alloc_es.enter_context(self.nc.psum_tensor(n, list(shape), dtype))
        return TT(h, n, ps=True)

    def dram(self, name, shape, dtype=F32, kind="Internal"):
        h = self.nc.dram_tensor(name, list(shape), dtype, kind=kind)
        return TT(h, name)

    @contextlib.contextmanager
    def scope(self):
        es = contextlib.ExitStack()
        old = self.alloc_es
        self.alloc_es = es
        try:
            yield
        finally:
            self.barrier()
            es.close()
            self.alloc_es = old

    def _newsem(self, name):
        self.nsem += 1
        s = self.es.enter_context(self.nc.semaphore(f"{name}_{self.nsem}"))
        rec = [s, 0]
        self.allsems.append(rec)
        return rec

    def _deps(self, reads, writes):
        d = {}

        def add(ev):
            rec, v = ev
            k = id(rec)
            if k not in d or d[k][1] < v:
                d[k] = (rec, v)
        for t in reads:
            if t.w is not None:
                add(t.w)
            if t.ps:
                for ev in t.r.values():
                    add(ev)
        for t in writes:
            if t.w is not None:
                add(t.w)
            for ev in t.r.values():
                add(ev)
        return d

    def _emit(self, e, fn, deps):
        eng = self.engines[e]
        kn = self.known[e]
        waits = []
        for k, (rec, v) in deps.items():
            if e == "pe" and k in self.pe_sems:
                continue
            if kn.get(k, 0) < v:
                waits.append((rec, v))
                kn[k] = v
        for rec, v in waits[:-1]:
            eng.wait_ge(rec[0], v)
            self.ninst += 1
        ins = fn(eng)
        self.ninst += 1
        if waits:
            rec, v = waits[-1]
            ins._wait_ge(rec[0], v)
        return ins

    def _record(self, ev, reads, writes):
        rec, v = ev
        for t in reads:
            t.r[id(rec)] = ev
        for t in writes:
            t.w = ev
            t.r = {}

    def op(self, e, fn, reads=(), writes=()):
        deps = self._deps(reads, writes)
        ins = self._emit(e, fn, deps)
        rec = self.cur.get(e)
        if rec is None or rec[1] >= SEM_LIM:
            rec = self.cur[e] = self._newsem("s" + e)
            if e == "pe":
                self.pe_sems.add(id(rec))
        rec[1] += 1
        ins.then_inc(rec[0], 1)
        self._record((rec, rec[1]), reads, writes)
        return ins

    def dma(self, q, out, in_, reads=(), writes=(), **kw):
        if q not in self.dpool:
            self.dpool[q] = [self._newsem("d" + q) for _ in range(ND)]
            self.dnext[q] = 0
        i = self.dnext[q]
        self.dnext[q] = i + 1
        rec = self.dpool[q][i % ND]
        if rec[1] >= SEM_LIM:
            rec = self.dpool[q][i % ND] = self._newsem("d" + q)
        deps = self._deps(reads, writes)
        if rec[1] > 0:
            k = id(rec)
            if k not in deps or deps[k][1] < rec[1]:
                deps[k] = (rec, rec[1])
        ins = self._emit(q, lambda eng: eng.dma_start(out=out, in_=in_, **kw), deps)
        rec[1] += 16
        ins.then_inc(rec[0], 16)
        self._record((rec, rec[1]), reads, writes)
        return ins

    def barrier(self):
        for e, eng in self.engines.items():
            kn = self.known[e]
            for rec in self.allsems:
                if rec[1] > 0 and kn.get(id(rec), 0) < rec[1]:
                    eng.wait_ge(rec[0], rec[1])
                    self.ninst += 1
                    kn[id(rec)] = rec[1]

    def close(self):
        self.barrier()
        self.es.close()

    def mm(self, out, lhsT, rhs, start, stop, R, W):
        return self.op("pe", lambda e: e.matmul(out, lhsT=lhsT, rhs=rhs, start=start, stop=stop), reads=R, writes=W)

    def mm32(self, out, lhsT, rhs, start, stop, R, W, junk, jt, identb):
        self.mm(out, lhsT, rhs, start, stop, R, W)
        if stop:
            self.op("pe", lambda e: e.matmul(junk, lhsT=identb[:, 0:32], rhs=identb[:, 0:1], start=True, stop=True),
                    reads=[], writes=list(W) + ([jt] if jt not in W else []))

    def tr(self, out, in_, ident, R, W):
        return self.op("pe", lambda e: e.transpose(out=out, in_=in_, identity=ident), reads=R, writes=W)

    def act(self, out, in_, func, R, W, **kw):
        return self.op("act", lambda e: e.activation(out=out, in_=in_, func=func, **kw), reads=R, writes=W)

    def ts(self, eng, out, in0, s1, s2, op0, op1, R, W):
        if op1 is None:
            return self.op(eng, lambda e: e.tensor_scalar(out=out, in0=in0, scalar1=s1, scalar2=None, op0=op0), reads=R, writes=W)
        return self.op(eng, lambda e: e.tensor_scalar(out=out, in0=in0, scalar1=s1, scalar2=s2, op0=op0, op1=op1), reads=R, writes=W)

    def tt(self, eng, out, in0, in1, op, R, W):
        return self.op(eng, lambda e: e.tensor_tensor(out=out, in0=in0, in1=in1, op=op), reads=R, writes=W)

    def stt(self, out, in0, scalar, in1, op0, op1, R, W):
        return self.op("dve", lambda e: e.scalar_tensor_tensor(out=out, in0=in0, scalar=scalar, in1=in1, op0=op0, op1=op1), reads=R, writes=W)

    def cp(self, eng, out, in_, R, W):
        if eng == "act":
            return self.act(out, in_, AF.Copy, R, W)
        return self.op(eng, lambda e: e.tensor_copy(out=out, in_=in_), reads=R, writes=W)


def host_consts():
    c = {}
    c["c_identb"] = np.eye(128).astype(ml_dtypes.bfloat16)
    c["c_identf"] = np.eye(128).astype(np.float32)
    rows = SEQ // GRID_W
    row = np.repeat(np.arange(rows, dtype=np.float32), GRID_W)
    col = np.tile(np.arange(GRID_W, dtype=np.float32), rows)
    axis_dim = 32
    inv_freq = (10000.0 ** (-np.arange(0, axis_dim, 2, dtype=np.float32) / axis_dim)).astype(np.float32)
    ang_r = row[:, None] * inv_freq
    ang_c = col[:, None] * inv_freq
    ang = np.concatenate([ang_r, ang_r, ang_c, ang_c], axis=-1)
    cos = np.cos(ang).T
    sin = np.sin(ang).T
    sign = np.where((np.arange(64) % 32) < 16, -1.0, 1.0)[:, None]
    tab = np.zeros((128, 2, NT), np.float32)
    tab[:, 0, :CTX] = 1.0
    for h in range(2):
        tab[h * 64:(h + 1) * 64, 0, CTX:] = cos
        tab[h * 64:(h + 1) * 64, 1, CTX:] = sin * sign
    c["c_rope"] = tab.astype(ml_dtypes.bfloat16)
    perm = np.zeros((128, 128), np.float32)
    for h in range(2):
        for m in range(64):
            src = m + 16 if (m % 32) < 16 else m - 16
            perm[h * 64 + src, h * 64 + m] = 1.0
    c["c_perm"] = perm.astype(ml_dtypes.bfloat16)
    on2 = np.zeros((128, 128), np.float32)
    on2[:64, :64] = 1.0 / 64
    on2[64:, 64:] = 1.0 / 64
    c["c_ones2"] = on2.astype(ml_dtypes.bfloat16)
    k = np.arange(128)[:, None]
    q = np.arange(128)[None, :]
    mL = np.where(k >= q, 0.0, NEG).astype(np.float32)
    mU = np.where(k <= q, 0.0, NEG).astype(np.float32)
    c["c_maskL"] = np.tile(mL, (1, 4)).astype(ml_dtypes.bfloat16)
    c["c_maskU"] = np.tile(mU, (1, 4)).astype(ml_dtypes.bfloat16)
    c["c_ones"] = np.ones((128, 128), ml_dtypes.bfloat16)
    c["c_triF"] = (k <= q).astype(np.float32)
    c["c_triB"] = (k >= q).astype(np.float32)
    c["c_nmF"] = np.where(k <= q, 0.0, NEG).astype(ml_dtypes.bfloat16)
    c["c_nmB"] = np.where(k >= q, 0.0, NEG).astype(ml_dtypes.bfloat16)
    return c


class G:
    pass


def build(NB=2, DEPTH=4, ALPHA=8 ** 0.25, debug=False, stages=None, use_moe=True):
    nc = bass.Bass("TRN2", target_bir_lowering=False)
    kb = KB(nc)
    g = G()
    g.kb, g.nc, g.NB, g.DEPTH, g.ALPHA, g.debug = kb, nc, NB, DEPTH, ALPHA, debug
    g.dbg = {}
    ins = {}

    def inp(name, shape, dt=F32):
        ins[name] = kb.dram(name, shape, dt, kind="ExternalInput")
        return ins[name]
    g.ins = ins
    inp("xin", [NB, NT, D])
    inp("cvec", [NB + 1, D])
    Ld = DEPTH
    inp("mod_w", [Ld, D, 6 * D]); inp("mod_b", [Ld, 6 * D]); inp("w_in", [Ld, D, D_IN])
    inp("wa_sink", [Ld, 8]); inp("ga_q_norm", [Ld, 64]); inp("ga_k_norm", [Ld, 64])
    inp("ssd_conv_w", [Ld, 3, 1024]); inp("ssd_conv_b", [Ld, 1024]); inp("ssd_dt_bias", [Ld, 16])
    inp("ssd_a_log", [Ld, 2, 8]); inp("ssd_d", [Ld, 8]); inp("ssd_norm_w", [Ld, 512])
    inp("s5_a_re", [Ld, 2, 32, 64]); inp("s5_a_im", [Ld, 2, 32, 64]); inp("s5_log_dt", [Ld, 2, 32])
    inp("s5_b_re", [Ld, 32, 64, 16]); inp("s5_b_im", [Ld, 32, 64, 16])
    inp("s5_c_re", [Ld, 32, 16, 64]); inp("s5_c_im", [Ld, 32, 16, 64])
    inp("s5_d", [Ld, 512]); inp("s5_w_glu", [Ld, 512, 1024])
    inp("w_branch", [Ld, 4, 512, D]); inp("w_out", [Ld, D, D])
    inp("ln1_g", [Ld, D]); inp("ln1_b", [Ld, D]); inp("ln2_g", [Ld, D]); inp("ln2_b", [Ld, D])
    inp("router_w", [D, 32]); inp("router_bias", [32])
    if use_moe:
        inp("moe_w_gate", [Ld, 32, D, 512]); inp("moe_w_up", [Ld, 32, D, 512]); inp("moe_w_down", [Ld, 32, 512, D])
    hc = host_consts()
    for k, v in hc.items():
        inp(k, list(v.shape), BF16 if v.dtype == ml_dtypes.bfloat16 else F32)
    g.out = kb.dram("out", [NB, SEQ, D], F32, kind="ExternalOutput")
    g.X1 = kb.dram("X1", [NB, NT, D], F32, kind="ExternalOutput" if debug else "Internal")
    g.X2 = kb.dram("X2", [NB, NT, D], F32, kind="ExternalOutput" if debug else "Internal")

    def cload(name, shape, dt):
        t = kb.sb(name, shape, dt)
        kb.dma("sp", t[:], ins[name][:], reads=[ins[name]], writes=[t])
        return t
    g.identb = cload("c_identb", [128, 128], BF16)
    g.identf = cload("c_identf", [128, 128], F32)
    g.perm = cload("c_perm", [128, 128], BF16)
    g.ones2 = cload("c_ones2", [128, 128], BF16)
    g.maskL = cload("c_maskL", [128, 512], BF16)
    g.maskU = cload("c_maskU", [128, 512], BF16)
    g.ones = cload("c_ones", [128, 128], BF16)
    g.triF = cload("c_triF", [128, 128], F32)
    g.triB = cload("c_triB", [128, 128], F32)
    g.nmF = cload("c_nmF", [128, 128], BF16)
    g.nmB = cload("c_nmB", [128, 128], BF16)

    run_all(g, stages)
    kb.close()
    return nc, g


def dbg_out(g, name, t, shape, dt=F32):
    if not g.debug:
        return
    kb = g.kb
    d = kb.dram("dbg_" + name, shape, dt, kind="ExternalOutput")
    kb.dma("sp", d[:], t[:], reads=[t], writes=[d])
    g.dbg[name] = d


def stage_mod(g, L):
    kb, NB = g.kb, g.NB
    ins = g.ins
    R = NB + 1
    modT = kb.sb("modT", [128, 48, R], F32)
    greps = [[kb.sb(f"grep{r}_{s}", [128, D], BF16) for s in range(2)] for r in range(R)]
    with kb.scope():
        condT = kb.sb("condT", [128, 8, R], F32)
        for r in range(R):
            kb.dma("sp", condT[:, :, r], ins["cvec"][r, :].rearrange("(k p) -> p k", p=128), reads=[ins["cvec"]], writes=[condT],
                   allow_slow_non_contiguous=True)
        kb.act(condT[:], condT[:], AF.Silu, [condT], [condT])
        mbT = kb.sb("mbT", [128, 48], F32)
        kb.dma("sp", mbT[:], ins["mod_b"][L, :].rearrange("(c p) -> p c", p=128), reads=[ins["mod_b"]], writes=[mbT],
               allow_slow_non_contiguous=True)
        mbrow = kb.sb("mbrow", [R, 6 * D], F32)
        kb.dma("sp", mbrow[:], ins["mod_b"][L:L + 1, :].broadcast_to([R, 6 * D]), reads=[ins["mod_b"]], writes=[mbrow])
        sel = kb.sb("sel", [R, R, 128], F32)
        kb.op("dve", lambda e: e.memset(sel[:], 0.0), writes=[sel])
        kb.op("dve", lambda e: e.tensor_copy(out=sel[:], in_=g.identf[0:R, 0:R].unsqueeze(2).broadcast_to([R, R, 128])),
              reads=[g.identf], writes=[sel])
        wt = [kb.sb(f"modw{i}", [128, 8, D], F32) for i in range(2)]
        pm = kb.ps("pm", [128, 512], F32)
        pr = kb.ps("pr", [128, 512], F32)
        pg = kb.ps("pg", [128, 512], F32)
        pj = kb.ps("pj", [128, 512], F32)
        rows = kb.sb("rows", [R, 512], F32)
        for s in range(6):
            w = wt[s % 2]
            kb.dma("sp" if s % 2 == 0 else "act", w[:], ins["mod_w"][L, :, s * D:(s + 1) * D].rearrange("(k p) c -> p k c", p=128),
                   reads=[ins["mod_w"]], writes=[w])
            for fc in range(8):
                for k in range(8):
                    kb.mm32(pm[:, fc * R:(fc + 1) * R], w[:, k, fc * 128:(fc + 1) * 128], condT[:, k, :], k == 0, k == 7, [w, condT], [pm],
                            pj[0:32, 0:1], pj, g.identb)
            for fc in range(8):
                ch = s * 8 + fc
                kb.ts("dve", modT[:, ch, :], pm[:, fc * R:(fc + 1) * R], mbT[:, ch:ch + 1], 1.0 if s in (1, 4) else 0.0,
                      ALU.add, ALU.add, [pm, mbT], [modT])
            if s in (2, 5):
                gi = 0 if s == 2 else 1
                for half in range(2):
                    for k in range(8):
                        kb.mm32(pr[0:R, :], condT[:, k, :], w[:, k, half * 512:(half + 1) * 512], k == 0, k == 7, [w, condT], [pr],
                                pj[0:32, 0:1], pj, g.identb)
                    kb.tt("dve", rows[:], pr[0:R, :], mbrow[:, s * D + half * 512: s * D + (half + 1) * 512], ALU.add, [pr, mbrow], [rows])
                    for r in range(R):
                        kb.mm32(pg[:], sel[:, r, :], rows[:], True, True, [sel, rows], [pg], pj[0:32, 0:1], pj, g.identb)
                        kb.cp("dve", greps[r][gi][:, half * 512:(half + 1) * 512], pg[:], [pg], [greps[r][gi]])
    g.modT = modT
    g.greps = greps


def stage_ln(g, b, Xsrc, chunkA, chunkB, hT, router=None):
    kb, NB = g.kb, g.NB
    with kb.scope():
        xts = [kb.sb(f"ln_x{i}", [128, D], F32) for i in range(2)]
        xns = [kb.sb(f"ln_xn{i}", [128, D], BF16) for i in range(2)]
        st = kb.sb("ln_st", [128, 2, 6], F32)
        mv = kb.sb("ln_mv", [128, 2], F32)
        rs = kb.sb("ln_rs", [128, 1], F32)
        tps = [kb.ps(f"ln_tp{i}", [128, D], BF16) for i in range(2)]
        for i in range(NTILE):
            col = NB if i < CTX // 128 else b
            xt, xn, tp = xts[i % 2], xns[i % 2], tps[i % 2]
            kb.dma("sp", xt[:], Xsrc[b, i * 128:(i + 1) * 128, :], reads=[Xsrc], writes=[xt])
            for hf in range(2):
                kb.op("dve", lambda e: e.bn_stats(out=st[:, hf, :], in_=xt[:, hf * 512:(hf + 1) * 512]), reads=[xt], writes=[st])
            kb.op("dve", lambda e: e.bn_aggr(out=mv[:], in_=st[:].rearrange("p a b -> p (a b)")), reads=[st], writes=[mv])
            kb.act(rs[:], mv[:, 1:2], AF.Sqrt, [mv], [rs], bias=g.epsc[:, 0:1])
            kb.op("dve", lambda e: e.reciprocal(out=rs[:], in_=rs[:]), reads=[rs], writes=[rs])
            kb.ts("dve", xn[:], xt[:], mv[:, 0:1], rs[:, 0:1], ALU.subtract, ALU.mult, [xt, mv, rs], [xn])
            for c in range(8):
                kb.tr(tp[:, c * 128:(c + 1) * 128], xn[:, c * 128:(c + 1) * 128], g.identb[:], [xn, g.identb], [tp])
            for c in range(8):
                o = hT[:, c, i * 128:(i + 1) * 128]
                a = g.modT[:, chunkA + c, col:col + 1]
                bb = g.modT[:, chunkB + c, col:col + 1]
                if c % 2 == 0:
                    kb.ts("dve", o, tp[:, c * 128:(c + 1) * 128], a, bb, ALU.mult, ALU.add, [tp, g.modT], [hT])
                else:
                    kb.act(o, tp[:, c * 128:(c + 1) * 128], AF.Identity, [tp, g.modT], [hT], scale=a, bias=bb)


def load_w(g, name, dst, src_ap, srcT, q="pool"):
    g.kb.dma(q, dst, src_ap, reads=[srcT], writes=[name])


def stage_attn(g, L, b, hT, YT, kind):
    kb, NB = g.kb, g.NB
    ins = g.ins
    w_in = ins["w_in"]
    c0 = 0 if kind == "a" else 768
    with kb.scope():
        Wq = kb.sb("Wq", [128, 8, 512], BF16)
        for i in range(4):
            for j in range(2):
                h = 4 * j + i
                kb.dma("pool", Wq[:, :, i * 128 + j * 64: i * 128 + (j + 1) * 64],
                       w_in[L, :, c0 + h * 64: c0 + (h + 1) * 64].rearrange("(k p) c -> p k c", p=128), reads=[w_in], writes=[Wq])
        Wk = kb.sb("Wk", [128, 8, 128], BF16)
        kb.dma("pool", Wk[:], w_in[L, :, c0 + 512: c0 + 640].rearrange("(k p) c -> p k c", p=128), reads=[w_in], writes=[Wk])
        Wv = kb.sb("Wv", [128, 8, 128], BF16)
        kb.dma("pool", Wv[:], w_in[L, :, c0 + 640: c0 + 768].rearrange("(k p) c -> p k c", p=128), reads=[w_in], writes=[Wv])
        QT = kb.sb("QT", [128, 4, NT], BF16)
        KT = kb.sb("KT", [128, NT], BF16)
        g.rope = kb.sb("rope", [128, 2, NT], BF16)
        kb.dma("sp", g.rope[:], ins["c_rope"][:], reads=[ins["c_rope"]], writes=[g.rope])
        V = kb.sb("V", [128, NTILE, 128], BF16)
        if kind == "g":
            nw = kb.sb("nw", [128, 2], F32)
            for hf in range(2):
                kb.dma("sp", nw[hf * 64:(hf + 1) * 64, 0:1], ins["ga_q_norm"][L, :].rearrange("(p o) -> p o", o=1), reads=[ins["ga_q_norm"]], writes=[nw])
                kb.dma("sp", nw[hf * 64:(hf + 1) * 64, 1:2], ins["ga_k_norm"][L, :].rearrange("(p o) -> p o", o=1), reads=[ins["ga_k_norm"]], writes=[nw])
        else:
            esink = kb.sb("esink", [128, 4], F32)
            for j in range(2):
                kb.dma("sp", esink[j * 64:(j + 1) * 64, :], ins["wa_sink"][L:L + 1, 4 * j:4 * j + 4].broadcast_to([64, 4]),
                       reads=[ins["wa_sink"]], writes=[esink])
            kb.act(esink[:], esink[:], AF.Exp, [esink], [esink])
        with kb.scope():
            pA = [kb.ps(f"pA{i}", [128, 512], F32) for i in range(2)]
            pB = [kb.ps(f"pB{i}", [128, 512], F32) for i in range(2)]
            pV = kb.ps("pV", [128, 512], F32)
            qa = [kb.sb(f"qa{i}", [128, 512], BF16) for i in range(2)]
            sq = [kb.sb(f"sq{i}", [128, 512], BF16) for i in range(2)]
            rstd = [kb.sb(f"rstd{i}", [128, 512], F32) for i in range(2)]
            t1 = [kb.sb(f"t1{i}", [128, 512], F32) for i in range(2)]
            t2 = [kb.sb(f"t2{i}", [128, 512], F32) for i in range(2)]
            units = [(t0, tn, m) for (t0, tn) in TCH for m in range(5)]

            def bufs(u):
                return pA[u % 2], pB[u % 2], qa[u % 2], sq[u % 2], rstd[u % 2], t1[u % 2], t2[u % 2]

            def ph1(u):
                t0, tn, m = units[u]
                W = Wq if m < 4 else Wk
                wc = m * 128 if m < 4 else 0
                A, B, Q, S, RS, T1, T2 = bufs(u)
                for k in range(8):
                    kb.mm(A[:, :tn], W[:, k, wc:wc + 128], hT[:, k, t0:t0 + tn], k == 0, k == 7, [W, hT], [A])
                if kind == "g":
                    kb.act(S[:, :tn], A[:, :tn], AF.Square, [A], [S])
                else:
                    kb.cp("act", Q[:, :tn], A[:, :tn], [A], [Q])

            def ph2(u):
                if kind != "g":
                    return
                t0, tn, m = units[u]
                A, B, Q, S, RS, T1, T2 = bufs(u)
                kb.mm(B[:, :tn], g.ones2[:], S[:, :tn], True, True, [g.ones2, S], [B])
                kb.act(RS[:, :tn], B[:, :tn], AF.Sqrt, [B], [RS], bias=g.epsc[:, 0:1])
                kb.op("dve", lambda e: e.reciprocal(out=RS[:, :tn], in_=RS[:, :tn]), reads=[RS], writes=[RS])
                kb.stt(T1[:, :tn], A[:, :tn], nw[:, (0 if m < 4 else 1):(1 if m < 4 else 2)], RS[:, :tn], ALU.mult, ALU.mult, [A, nw, RS], [T1])
                kb.cp("act", Q[:, :tn], T1[:, :tn], [T1], [Q])

            def ph3(u):
                t0, tn, m = units[u]
                A, B, Q, S, RS, T1, T2 = bufs(u)
                dst = QT[:, m, t0:t0 + tn] if m < 4 else KT[:, t0:t0 + tn]
                dstT = QT if m < 4 else KT
                src = T1 if kind == "g" else A
                kb.mm(B[:, :tn], g.perm[:], Q[:, :tn], True, True, [g.perm, Q], [B])
                kb.tt("dve", T2[:, :tn], B[:, :tn], g.rope[:, 1, t0:t0 + tn], ALU.mult, [B, g.rope], [T2])
                kb.tt("pool" if src is T1 else "dve", T1[:, :tn], src[:, :tn], g.rope[:, 0, t0:t0 + tn], ALU.mult, [src, g.rope], [T1])
                kb.tt("pool", dst, T1[:, :tn], T2[:, :tn], ALU.add, [T1, T2], [dstT])

            for u in range(len(units) + 1):
                if u < len(units):
                    ph1(u)
                if u - 1 >= 0:
                    ph2(u - 1)
                    ph3(u - 1)
            for i in range(NTILE):
                for k in range(8):
                    kb.mm(pV[:, 0:128], hT[:, k, i * 128:(i + 1) * 128], Wv[:, k, :], k == 0, k == 7, [hT, Wv], [pV])
                kb.cp("act", V[:, i, :], pV[:, 0:128], [pV], [V])
            if g.debug and b == 0 and L == 0:
                dbg_out(g, f"QT{kind}", QT, [128, 4, NT], BF16)
                dbg_out(g, f"KT{kind}", KT, [128, NT], BF16)
                dbg_out(g, f"V{kind}", V, [128, NTILE, 128], BF16)
        pS = [kb.ps(f"pS{i}", [128, 512], F32) for i in range(4)]
        pO = kb.ps("pO", [128, 512], F32)
        pD = kb.ps("pD", [128, 512], F32)
        P = [kb.sb(f"P{i}", [128, 512], BF16) for i in range(4)]
        rec = kb.sb("rec", [128, 512], F32)
        nctx = CTX // 128
        items = []
        for qi in range(NTILE):
            if qi < nctx:
                keys = [(kt, None) for kt in range(nctx)]
            elif kind == "g":
                keys = [(kt, None) for kt in range(NTILE)]
            else:
                keys = [(kt, None) for kt in range(nctx)]
                if qi - 1 >= nctx:
                    keys.append((qi - 1, g.maskL))
                keys.append((qi, None))
                if qi + 1 < NTILE:
                    keys.append((qi + 1, g.maskU))
            for j in range(2):
                for ki, (kt, msk) in enumerate(keys):
                    items.append((qi, j, ki, len(keys), kt, msk))
        LA = 2

        def s_phase(t):
            qi, j, ki, nk, kt, msk = items[t]
            ps_ = slice(j * 64, (j + 1) * 64)
            S_, P_ = pS[t % 4], P[t % 4]
            kb.mm(S_[:], KT[ps_, kt * 128:(kt + 1) * 128], QT[ps_, :, qi * 128:(qi + 1) * 128], True, msk is None, [KT, QT], [S_])
            if msk is not None:
                kb.mm(S_[:], g.identb[:], msk[:], False, True, [g.identb, msk], [S_])
            kb.act(P_[:], S_[:], AF.Exp, [S_], [P_], scale=0.125)

        def pv_phase(t):
            qi, j, ki, nk, kt, msk = items[t]
            ps_ = slice(j * 64, (j + 1) * 64)
            P_ = P[t % 4]
            kb.mm(pO[ps_, :], V[:, kt, ps_], P_[:], ki == 0, ki == nk - 1, [V, P_], [pO])
            kb.mm(pD[ps_, :], g.ones[:, 0:64], P_[:], ki == 0, ki == nk - 1, [g.ones, P_], [pD])
            if j == 1 and ki == nk - 1:
                if kind == "a":
                    kb.tt("dve", rec[:].rearrange("p (i t) -> p i t", i=4), pD[:].rearrange("p (i t) -> p i t", i=4),
                          esink[:].unsqueeze(2).broadcast_to([128, 4, 128]), ALU.add, [pD, esink], [rec])
                    kb.op("dve", lambda e: e.reciprocal(out=rec[:], in_=rec[:]), reads=[rec], writes=[rec])
                else:
                    kb.op("dve", lambda e: e.reciprocal(out=rec[:], in_=pD[:]), reads=[pD], writes=[rec])
                kb.tt("dve", YT[:, :, qi * 128:(qi + 1) * 128], pO[:].rearrange("p (i t) -> p i t", i=4),
                      rec[:].rearrange("p (i t) -> p i t", i=4), ALU.mult, [pO, rec], [YT])

        for t in range(len(items) + LA):
            if t < len(items):
                s_phase(t)
            if t - LA >= 0:
                pv_phase(t - LA)


def stage_ssd(g, L, b, hT, YsT):
    kb, NB = g.kb, g.NB
    ins = g.ins
    w_in = ins["w_in"]
    nctx = CTX // 128
    segs = [(0, CTX), (CTX, NT)]
    with kb.scope():
        bcT = kb.sb("bcT", [128, 4, NT], BF16)
        xbtok = kb.sb("xbtok", [128, NTILE, 768], BF16)
        zs = kb.sb("zs", [128, NTILE, 512], BF16)
        dt = kb.sb("dt", [128, NTILE, 16], F32)
        da = kb.sb("da", [128, NTILE, 16], F32)
        with kb.scope():
            Wc = kb.sb("Wc", [128, 8, 1024], BF16)
            kb.dma("pool", Wc[:, :, 0:512], w_in[L, :, 1536:2048].rearrange("(k p) c -> p k c", p=128), reads=[w_in], writes=[Wc])
            kb.dma("pool", Wc[:, :, 512:1024], w_in[L, :, 2560:3072].rearrange("(k p) c -> p k c", p=128), reads=[w_in], writes=[Wc])
            Wz = kb.sb("Wz", [128, 8, 512], BF16)
            kb.dma("pool", Wz[:], w_in[L, :, 2048:2560].rearrange("(k p) c -> p k c", p=128), reads=[w_in], writes=[Wz])
            Wdt = kb.sb("Wdt", [128, 8, 16], BF16)
            kb.dma("pool", Wdt[:], w_in[L, :, 3072:3088].rearrange("(k p) c -> p k c", p=128), reads=[w_in], writes=[Wdt])
            cw = kb.sb("cw", [128, 8, 3], F32)
            for k in range(3):
                kb.dma("sp", cw[:, :, k], ins["ssd_conv_w"][L, k, :].rearrange("(c p) -> p c", p=128), reads=[ins["ssd_conv_w"]], writes=[cw],
                       allow_slow_non_contiguous=True)
            cb = kb.sb("cb", [128, 8], F32)
            kb.dma("sp", cb[:], ins["ssd_conv_b"][L, :].rearrange("(c p) -> p c", p=128), reads=[ins["ssd_conv_b"]], writes=[cb],
                   allow_slow_non_contiguous=True)
            dtb = kb.sb("dtb", [128, 16], F32)
            kb.dma("sp", dtb[:], ins["ssd_dt_bias"][L:L + 1, :].broadcast_to([128, 16]), reads=[ins["ssd_dt_bias"]], writes=[dtb])
            negA = kb.sb("negA", [128, 16], F32)
            kb.dma("sp", negA[:], ins["ssd_a_log"][L:L + 1, :, :].rearrange("o a h -> o (a h)").broadcast_to([128, 16]),
                   reads=[ins["ssd_a_log"]], writes=[negA])
            kb.act(negA[:], negA[:], AF.Exp, [negA], [negA])
            kb.ts("dve", negA[:], negA[:], -1.0, None, ALU.mult, None, [negA], [negA])
            raw = kb.sb("raw", [128, NT], BF16)
            acc = kb.sb("acc", [128, NT], F32)
            xT = kb.sb("xT", [128, 4, NT], BF16)
            pp = [kb.ps(f"pp{i}", [128, 512], F32) for i in range(2)]
            pz = [kb.ps(f"pz{i}", [128, 512], F32) for i in range(2)]
            pt = [kb.ps(f"ptt{i}", [128, 768], BF16) for i in range(2)]
            n = 0
            for c in range(8):
                for (t0, tn) in TCH:
                    A = pp[n % 2]
                    n += 1
                    for k in range(8):
                        kb.mm(A[:, :tn], Wc[:, k, c * 128:(c + 1) * 128], hT[:, k, t0:t0 + tn], k == 0, k == 7, [Wc, hT], [A])
                    kb.cp("act", raw[:, t0:t0 + tn], A[:, :tn], [A], [raw])
                kb.ts("dve", acc[:], raw[:], cw[:, c, 1:2], cb[:, c:c + 1], ALU.mult, ALU.add, [raw, cw, cb], [acc])
                for (s0, s1) in segs:
                    kb.stt(acc[:, s0 + 1:s1], raw[:, s0:s1 - 1], cw[:, c, 0:1], acc[:, s0 + 1:s1], ALU.mult, ALU.add, [raw, cw, acc], [acc])
                    kb.stt(acc[:, s0:s1 - 1], raw[:, s0 + 1:s1], cw[:, c, 2:3], acc[:, s0:s1 - 1], ALU.mult, ALU.add, [raw, cw, acc], [acc])
                dst = xT[:, c, :] if c < 4 else bcT[:, c - 4, :]
                kb.act(dst, acc[:], AF.Silu, [acc], [xT if c < 4 else bcT])
            tmp16 = kb.sb("tmp16", [128, 16], F32)
            for i in range(NTILE):
                Z = pz[i % 2]
                tk = slice(i * 128, (i + 1) * 128)
                for k in range(8):
                    kb.mm(Z[:], hT[:, k, tk], Wz[:, k, :], k == 0, k == 7, [hT, Wz], [Z])
                kb.act(zs[:, i, :], Z[:], AF.Silu, [Z], [zs])
                Dp = pp[i % 2]
                for k in range(8):
                    kb.mm(Dp[:, 0:16], hT[:, k, tk], Wdt[:, k, :], k == 0, k == 7, [hT, Wdt], [Dp])
                kb.tt("dve", tmp16[:], Dp[:, 0:16], dtb[:], ALU.add, [Dp, dtb], [tmp16])
                kb.ts("dve", tmp16[:], tmp16[:], 30.0, None, ALU.min, None, [tmp16], [tmp16])
                kb.act(tmp16[:], tmp16[:], AF.Exp, [tmp16], [tmp16])
                kb.act(dt[:, i, :], tmp16[:], AF.Ln, [tmp16], [dt], bias=g.onec[:, 0:1])
                T = pt[i % 2]
                for c in range(6):
                    src = xT[:, c, tk] if c < 4 else bcT[:, c - 4, tk]
                    kb.tr(T[:, c * 128:(c + 1) * 128], src, g.identb[:], [xT if c < 4 else bcT, g.identb], [T])
                kb.cp("dve", xbtok[:, i, :], T[:], [T], [xbtok])
            kb.tt("dve", da[:], dt[:], negA[:].unsqueeze(1).broadcast_to([128, NTILE, 16]), ALU.mult, [dt, negA], [da])
        if g.debug and L == 0 and b == 0:
            dbg_out(g, "ssd_dt", dt, [128, NTILE, 16])
            dbg_out(g, "ssd_xbtok", xbtok, [128, NTILE, 768], BF16)
        import os
        PH = int(os.environ.get("SSD_PHASE", "3"))
        Yacc = kb.sb("Yacc", [128, NTILE, 512], F32)
        if PH < 2:
            return
        with kb.scope():
            selh = kb.sb("selh", [8, 8, 128], F32)
            kb.op("dve", lambda e: e.tensor_copy(out=selh[:], in_=g.identf[0:8, 0:8].unsqueeze(2).broadcast_to([8, 8, 128])),
                  reads=[g.identf], writes=[selh])
            pcs = kb.ps("pcs", [128, 512], F32)
            pR = kb.ps("pR", [128, 1024], F32)
            pCB = kb.ps("pCB", [128, 512], F32)
            pYd = kb.ps("pYd", [128, 512], F32)
            pYo = kb.ps("pYo", [128, 512], F32)
            pSn = kb.ps("pSn", [128, 512], F32)
            ncs = kb.sb("ncs", [128, 8], F32)
            csT = kb.sb("csT", [8, 128], F32)
            Lt = kb.sb("Lt", [128, 8, 128], F32)
            Gt = kb.sb("Gt", [128, 8, 128], BF16)
            ecol = kb.sb("ecol", [128, 8], F32)
            eend = kb.sb("eend", [128, 8], F32)
            wcol = kb.sb("wcol", [128, 8], F32)
            xd = kb.sb("xd", [128, 512], BF16)
            xdd = kb.sb("xdd", [128, 512], BF16)
            tmpy = kb.sb("tmpy", [128, 512], F32)
            S = kb.sb("S", [128, 512], F32)
            Sb = kb.sb("Sb", [128, 512], BF16)
            CUT = int(os.environ.get("SSD_CUT", "0"))
            for d in range(2):
                tri = g.triF if d == 0 else g.triB
                nmask = g.nmF if d == 0 else g.nmB
                e_idx = 127 if d == 0 else 0
                order = list(range(NTILE)) if d == 0 else ([1, 0] + list(range(NTILE - 1, nctx - 1, -1)))
                kb.op("dve", lambda e: e.memset(S[:], 0.0), writes=[S])
                kb.op("pool", lambda e: e.memset(Sb[:], 0.0), writes=[Sb])
                if CUT == 20 and d == 1:
                    return
                for ci in order:
                    if CUT == 21 and d == 1:
                        CUT = int(os.environ.get("SSD_CUT2", "13"))
                    tk = slice(ci * 128, (ci + 1) * 128)
                    dah = da[:, ci, d * 8:(d + 1) * 8]
                    dth = dt[:, ci, d * 8:(d + 1) * 8]
                    xs3 = xbtok[:, ci, 0:512].rearrange("p (h q) -> p h q", h=8)
                    kb.mm(pcs[:, 0:8], tri[:], dah, True, True, [tri, da], [pcs])
                    kb.mm32(pcs[0:8, 128:256], dah, tri[:], True, True, [tri, da], [pcs], pcs[0:32, 511:512], pcs, g.identb)
                    if CUT == 1:
                        dump = kb.sb("dump", [128, 256], F32)
                        kb.cp("dve", dump[:], pcs[:, 0:256], [pcs], [dump])
                        dbg_out(g, "pcs", dump, [128, 256])
                        return
                    kb.ts("dve", ncs[:], pcs[:, 0:8], -1.0, None, ALU.mult, None, [pcs], [ncs])
                    if CUT == 15:
                        return
                    kb.cp("dve", csT[:], pcs[0:8, 128:256], [pcs], [csT])
                    if CUT == 16:
                        return
                    kb.act(ecol[:], ncs[:], AF.Exp, [ncs], [ecol], scale=-1.0)
                    if CUT == 2:
                        return
                    for h in range(8):
                        kb.mm(pR[:, h * 128:(h + 1) * 128], selh[:, h, :], csT[:], True, False, [selh, csT], [pR])
                        kb.mm(pR[:, h * 128:(h + 1) * 128], g.identb[:], nmask[:], False, True, [g.identb, nmask], [pR])
                    if CUT == 3:
                        return
                    for gg in range(2):
                        kb.mm(pCB[:, gg * 128:(gg + 1) * 128], bcT[:, gg, tk], bcT[:, 2 + gg, tk], True, True, [bcT], [pCB])
                    if CUT == 4:
                        return
                    for h in range(8):
                        kb.act(Lt[:, h, :], pR[:, h * 128:(h + 1) * 128], AF.Exp, [pR, ncs], [Lt], bias=ncs[:, h:h + 1])
                    if CUT == 5:
                        return
                    kb.tt("dve", eend[:], Lt[:, :, e_idx], ecol[:], ALU.mult, [Lt, ecol], [eend])
                    if CUT == 6:
                        return
                    for gg in range(2):
                        kb.tt("dve", Gt[:, gg * 4:(gg + 1) * 4, :], Lt[:, gg * 4:(gg + 1) * 4, :],
                              pCB[:, gg * 128:(gg + 1) * 128].unsqueeze(1).broadcast_to([128, 4, 128]), ALU.mult, [Lt, pCB], [Gt])
                    if CUT == 7:
                        return
                    kb.tt("dve", wcol[:], dth, Lt[:, :, e_idx], ALU.mult, [dt, Lt], [wcol])
                    if CUT == 8:
                        return
                    kb.tt("dve", xd[:].rearrange("p (h q) -> p h q", h=8), xs3, dth.unsqueeze(2).broadcast_to([128, 8, 64]), ALU.mult, [xbtok, dt], [xd])
                    if CUT == 9:
                        return
                    kb.tt("pool", xdd[:].rearrange("p (h q) -> p h q", h=8), xs3, wcol[:].unsqueeze(2).broadcast_to([128, 8, 64]), ALU.mult, [xbtok, wcol], [xdd])
                    if CUT == 10:
                        return
                    for h in range(8):
                        kb.mm(pYd[:, h * 64:(h + 1) * 64], Gt[:, h, :], xd[:, h * 64:(h + 1) * 64], True, True, [Gt, xd], [pYd])
                    if CUT == 11:
                        return
                    for h in range(8):
                        kb.mm(pYo[:, h * 64:(h + 1) * 64], bcT[:, 2 + h // 4, tk], Sb[:, h * 64:(h + 1) * 64], True, True, [bcT, Sb], [pYo])
                    if CUT == 12:
                        return
                    for gg in range(2):
                        kb.mm(pSn[:, gg * 256:(gg + 1) * 256], xbtok[:, ci, 512 + gg * 128: 512 + (gg + 1) * 128], xdd[:, gg * 256:(gg + 1) * 256],
                              True, True, [xbtok, xdd], [pSn])
                    if CUT == 13:
                        return
                    kb.tt("dve", tmpy[:].rearrange("p (h q) -> p h q", h=8), pYo[:].rearrange("p (h q) -> p h q", h=8),
                          ecol[:].unsqueeze(2).broadcast_to([128, 8, 64]), ALU.mult, [pYo, ecol], [tmpy])
                    if d == 0:
                        kb.tt("dve", Yacc[:, ci, :], tmpy[:], pYd[:], ALU.add, [tmpy, pYd], [Yacc])
                    else:
                        kb.tt("dve", tmpy[:], tmpy[:], pYd[:], ALU.add, [tmpy, pYd], [tmpy])
                        kb.tt("pool", Yacc[:, ci, :], Yacc[:, ci, :], tmpy[:], ALU.add, [Yacc, tmpy], [Yacc])
                    if CUT == 14:
                        return
                    kb.tt("dve", S[:].rearrange("p (h q) -> p h q", h=8), S[:].rearrange("p (h q) -> p h q", h=8),
                          eend[:].unsqueeze(2).broadcast_to([128, 8, 64]), ALU.mult, [S, eend], [S])
                    kb.tt("dve", S[:], S[:], pSn[:], ALU.add, [S, pSn], [S])
                    kb.cp("act", Sb[:], S[:], [S], [Sb])
                    if CUT == 17:
                        return
                    if CUT == 18 and ci == order[2]:
                        return
                    if CUT == 19 and d == 1 and ci == order[0]:
                        return
        if PH < 3:
            return
        with kb.scope():
            Drep = kb.sb("Drep", [128, 8], F32)
            kb.dma("sp", Drep[:], ins["ssd_d"][L:L + 1, :].broadcast_to([128, 8]), reads=[ins["ssd_d"]], writes=[Drep])
            nwrep = kb.sb("nwrep", [128, 512], F32)
            kb.dma("sp", nwrep[:], ins["ssd_norm_w"][L:L + 1, :].broadcast_to([128, 512]), reads=[ins["ssd_norm_w"]], writes=[nwrep])
            ty = [kb.sb(f"ty{i}", [128, 512], F32) for i in range(2)]
            junk = kb.sb("junk", [128, 512], F32)
            ss = kb.sb("ss", [128, 1], F32)
            yb = [kb.sb(f"yb{i}", [128, 512], BF16) for i in range(2)]
            pt2 = [kb.ps(f"pt2{i}", [128, 512], BF16) for i in range(2)]
            for i in range(NTILE):
                Y, Yb, T = ty[i % 2], yb[i % 2], pt2[i % 2]
                kb.tt("dve", Y[:].rearrange("p (h q) -> p h q", h=8), xbtok[:, i, 0:512].rearrange("p (h q) -> p h q", h=8),
                      Drep[:].unsqueeze(2).broadcast_to([128, 8, 64]), ALU.mult, [xbtok, Drep], [Y])
                kb.tt("dve", Y[:], Y[:], Yacc[:, i, :], ALU.add, [Y, Yacc], [Y])
                kb.tt("dve", Y[:], Y[:], zs[:, i, :], ALU.mult, [Y, zs], [Y])
                kb.act(junk[:], Y[:], AF.Square, [Y], [junk, ss], accum_out=ss[:])
                kb.act(ss[:], ss[:], AF.Sqrt, [ss], [ss], scale=1.0 / 512, bias=g.epsc[:, 0:1])
                kb.op("dve", lambda e: e.reciprocal(out=ss[:], in_=ss[:]), reads=[ss], writes=[ss])
                kb.stt(Yb[:], Y[:], ss[:, 0:1], nwrep[:], ALU.mult, ALU.mult, [Y, ss, nwrep], [Yb])
                for c in range(4):
                    kb.tr(T[:, c * 128:(c + 1) * 128], Yb[:, c * 128:(c + 1) * 128], g.identb[:], [Yb, g.identb], [T])
                kb.cp("act", YsT[:, :, i * 128:(i + 1) * 128], T[:].rearrange("p (c t) -> p c t", c=4), [T], [YsT])


def run_all(g, stages=None):
    kb, NB = g.kb, g.NB
    stages = stages or ("ssd", "s5", "a", "g", "merge", "moe")
    g.epsc = kb.sb("epsc", [128, 1], F32)
    kb.op("dve", lambda e: e.memset(g.epsc[:], EPS), writes=[g.epsc])
    g.onec = kb.sb("onec", [128, 1], F32)
    kb.op("dve", lambda e: e.memset(g.onec[:], 1.0), writes=[g.onec])
    Xin = g.ins["xin"]
    for L in range(g.DEPTH):
        with kb.scope():
            stage_mod(g, L)
            for b in range(NB):
                with kb.scope():
                    dbg = g.debug and L == 0 and b == 0
                    hT = kb.sb("hT", [128, 8, NT], BF16)
                    stage_ln(g, b, Xin if L == 0 else g.X2, 8, 0, hT)
                    YsT = kb.sb("YsT", [128, 4, NT], BF16)
                    if "ssd" in stages:
                        stage_ssd(g, L, b, hT, YsT)
                        if dbg:
                            dbg_out(g, "YsT", YsT, [128, 4, NT], BF16)
                    Y5T = kb.sb("Y5T", [128, 4, NT], BF16)
                    if "s5" in stages:
                        stage_s5(g, L, b, hT, Y5T)
                        if dbg:
                            dbg_out(g, "Y5T", Y5T, [128, 4, NT], BF16)
                    YTa = kb.sb("YTa", [128, 4, NT], BF16)
                    YTg = kb.sb("YTg", [128, 4, NT], BF16)
                    if "a" in stages:
                        stage_attn(g, L, b, hT, YTa, "a")
                    if "g" in stages:
                        stage_attn(g, L, b, hT, YTg, "g")
                    if dbg and "a" in stages and "g" in stages:
                        dbg_out(g, "YTa", YTa, [128, 4, NT], BF16)
                        dbg_out(g, "YTg", YTg, [128, 4, NT], BF16)
                    if "merge" in stages:
                        stage_merge(g, L, b, hT, [YTa, YsT, YTg, Y5T], Xin if L == 0 else g.X2)
                if "moe" in stages:
                    stage_moe(g, L, b, L == g.DEPTH - 1 and not g.debug)


def s5_params(g, L):
    kb = g.kb
    ins = g.ins
    P = {}
    TWO_PI = 2 * math.pi
    P["BxT"] = kb.sb("BxT", [128, 2, 2, 16, 128], BF16)
    P["Cx"] = kb.sb("Cx", [128, 2, 16, 128], BF16)
    P["rr"] = kb.sb("rr", [128, 2, 16], F32)
    P["c1"] = kb.sb("c1", [128, 2, 16], F32)
    P["s1"] = kb.sb("s1", [128, 2, 16], F32)
    P["th"] = kb.sb("th", [128, 2, 16], F32)
    P["Dc"] = kb.sb("Dc", [128, 4], F32)
    kb.dma("sp", P["Dc"][:], ins["s5_d"][L, :].rearrange("(c p) -> p c", p=128), reads=[ins["s5_d"]], writes=[P["Dc"]],
           allow_slow_non_contiguous=True)
    with kb.scope():
        Are = kb.sb("Are", [128, 2, 16], F32)
        Aim = kb.sb("Aim", [128, 2, 16], F32)
        Ldt = kb.sb("Ldt", [128, 2, 16], F32)
        Bre = kb.sb("Bre", [128, 16, 16], F32)
        Bim = kb.sb("Bim", [128, 16, 16], F32)
        Cf = kb.sb("Cf", [128, 2, 16, 128], F32)
        kb.op("pool", lambda e: e.memset(Cf[:], 0.0), writes=[Cf])
        for gl in range(2):
            ph = slice(gl * 64, (gl + 1) * 64)
            for d in range(2):
                kb.dma("sp", Are[ph, d, :], ins["s5_a_re"][L, d, gl::2, :].rearrange("j n -> n j"), reads=[ins["s5_a_re"]], writes=[Are], allow_slow_non_contiguous=True)
                kb.dma("sp", Aim[ph, d, :], ins["s5_a_im"][L, d, gl::2, :].rearrange("j n -> n j"), reads=[ins["s5_a_im"]], writes=[Aim], allow_slow_non_contiguous=True)
                kb.dma("sp", Ldt[ph, d, :], ins["s5_log_dt"][L, d:d + 1, gl::2].broadcast_to([64, 16]), reads=[ins["s5_log_dt"]], writes=[Ldt], allow_slow_non_contiguous=True)
            kb.dma("sp", Bre[ph, :, :], ins["s5_b_re"][L, gl::2, :, :].rearrange("j n i -> n j i"), reads=[ins["s5_b_re"]], writes=[Bre], allow_slow_non_contiguous=True)
            kb.dma("act", Bim[ph, :, :], ins["s5_b_im"][L, gl::2, :, :].rearrange("j n i -> n j i"), reads=[ins["s5_b_im"]], writes=[Bim], allow_slow_non_contiguous=True)
            for jr in range(4):
                off = 32 * jr + 16 * gl
                for ri, nm in enumerate(("s5_c_re", "s5_c_im")):
                    for jq in range(4):
                        j = jq * 4 + jr
                        kb.dma("sp" if ri == 0 else "act", Cf[ph, ri, j, off:off + 16],
                               ins[nm][L, 2 * j + gl, :, :].rearrange("o n -> n o"), reads=[ins[nm]], writes=[Cf], allow_slow_non_contiguous=True)
        kb.cp("act", P["Cx"][:, 0], Cf[:, 0], [Cf], [P["Cx"]])
        kb.ts("dve", P["Cx"][:, 1], Cf[:, 1], -1.0, None, ALU.mult, None, [Cf], [P["Cx"]])
        dtt = kb.sb("dtt", [128, 2, 16], F32)
        kb.act(dtt[:], Ldt[:], AF.Exp, [Ldt], [dtt])
        kb.ts("dve", Are[:], Are[:], -1e-4, None, ALU.min, None, [Are], [Are])
        xre = kb.sb("xre", [128, 2, 16], F32)
        kb.tt("dve", xre[:], Are[:], dtt[:], ALU.mult, [Are, dtt], [xre])
        kb.tt("dve", P["th"][:], Aim[:], dtt[:], ALU.mult, [Aim, dtt], [P["th"]])
        kb.act(P["rr"][:], xre[:], AF.Exp, [xre], [P["rr"]])
        sc = kb.sb("sc", [128, 2, 2, 16], F32)
        s5_sincos(g, P["th"][:].rearrange("p d j -> p (d j)"), P["s1"][:].rearrange("p d j -> p (d j)"),
                  P["c1"][:].rearrange("p d j -> p (d j)"), [P["th"]], [P["s1"], P["c1"]], 32)
        nr = kb.sb("nr", [128, 2, 16], F32)
        ni = kb.sb("ni", [128, 2, 16], F32)
        kb.tt("dve", nr[:], P["rr"][:], P["c1"][:], ALU.mult, [P["rr"], P["c1"]], [nr])
        kb.ts("dve", nr[:], nr[:], -1.0, None, ALU.add, None, [nr], [nr])
        kb.tt("dve", ni[:], P["rr"][:], P["s1"][:], ALU.mult, [P["rr"], P["s1"]], [ni])
        den = kb.sb("den", [128, 2, 16], F32)
        t_ = kb.sb("t_", [128, 2, 16], F32)
        kb.tt("dve", den[:], Are[:], Are[:], ALU.mult, [Are], [den])
        kb.tt("dve", t_[:], Aim[:], Aim[:], ALU.mult, [Aim], [t_])
        kb.tt("dve", den[:], den[:], t_[:], ALU.add, [den, t_], [den])
        kb.op("dve", lambda e: e.reciprocal(out=den[:], in_=den[:]), reads=[den], writes=[den])
        kre = kb.sb("kre", [128, 2, 16], F32)
        kim = kb.sb("kim", [128, 2, 16], F32)
        kb.tt("dve", kre[:], nr[:], Are[:], ALU.mult, [nr, Are], [kre])
        kb.tt("dve", t_[:], ni[:], Aim[:], ALU.mult, [ni, Aim], [t_])
        kb.tt("dve", kre[:], kre[:], t_[:], ALU.add, [kre, t_], [kre])
        kb.tt("dve", kre[:], kre[:], den[:], ALU.mult, [kre, den], [kre])
        kb.tt("dve", kim[:], ni[:], Are[:], ALU.mult, [ni, Are], [kim])
        kb.tt("dve", t_[:], nr[:], Aim[:], ALU.mult, [nr, Aim], [t_])
        kb.tt("dve", kim[:], kim[:], t_[:], ALU.subtract, [kim, t_], [kim])
        kb.tt("dve", kim[:], kim[:], den[:], ALU.mult, [kim, den], [kim])
        Bx = kb.sb("Bx", [128, 16, 128], F32)
        tb1 = kb.sb("tb1", [128, 16, 16], F32)
        tb2 = kb.sb("tb2", [128, 16, 16], F32)
        ptb = [kb.ps(f"ptb{i}", [128, 512], F32) for i in range(2)]
        n = 0
        for d in range(2):
            for ri in range(2):
                X1, X2 = (Bre, Bim) if ri == 0 else (Bim, Bre)
                kb.tt("dve", tb1[:], X1[:], kre[:, d, :].unsqueeze(2).broadcast_to([128, 16, 16]), ALU.mult, [X1, kre], [tb1])
                kb.tt("dve", tb2[:], X2[:], kim[:, d, :].unsqueeze(2).broadcast_to([128, 16, 16]), ALU.mult, [X2, kim], [tb2])
                kb.tt("dve", tb1[:], tb1[:], tb2[:], ALU.subtract if ri == 0 else ALU.add, [tb1, tb2], [tb1])
                kb.op("pool", lambda e: e.memset(Bx[:], 0.0), writes=[Bx])
                for gl in range(2):
                    ph = slice(gl * 64, (gl + 1) * 64)
                    for jr in range(4):
                        off = 32 * jr + 16 * gl
                        kb.cp("dve", Bx[ph, jr::4, off:off + 16], tb1[ph, jr::4, :], [tb1], [Bx])
                for jq in range(4):
                    T = ptb[n % 2]
                    n += 1
                    for jj in range(4):
                        j = jq * 4 + jj
                        kb.tr(T[:, jj * 128:(jj + 1) * 128], Bx[:, j, :], g.identf[:], [Bx, g.identf], [T])
                    kb.cp("dve", P["BxT"][:, d, ri, jq * 4:(jq + 1) * 4, :], T[:].rearrange("p (a b) -> p a b", a=4), [T], [P["BxT"]])
    g.s5 = P


def s5_sincos(g, ang, s_out, c_out, R, W, n):
    kb = g.kb
    TWO_PI = 2 * math.pi
    with kb.scope():
        kf = kb.sb("kf", [128, n], F32)
        ki = kb.sb("ki", [128, n], I32)
        r = kb.sb("r", [128, n], F32)
        m = kb.sb("m", [128, n], F32)
        kb.ts("dve", kf[:], ang, 1.0 / TWO_PI, None, ALU.mult, None, R, [kf])
        kb.cp("dve", ki[:], kf[:], [kf], [ki])
        kb.cp("dve", kf[:], ki[:], [ki], [kf])
        kb.stt(r[:], kf[:], -TWO_PI, ang, ALU.mult, ALU.add, [kf] + list(R), [r])
        for shift, out in ((0.0, s_out), (math.pi / 2, c_out)):
            src = r
            if shift:
                kb.ts("dve", m[:], r[:], shift, None, ALU.add, None, [r], [m])
                src = m
            for _ in range(2):
                kb.ts("dve", kf[:], src[:], math.pi, -TWO_PI, ALU.is_gt, ALU.mult, [src], [kf])
                kb.tt("dve", m[:], src[:], kf[:], ALU.add, [src, kf], [m])
                src = m
                kb.ts("dve", kf[:], m[:], -math.pi, TWO_PI, ALU.is_lt, ALU.mult, [m], [kf])
                kb.tt("dve", m[:], m[:], kf[:], ALU.add, [m, kf], [m])
            kb.act(out, m[:], AF.Sin, [m], W)


def stage_s5(g, L, b, hT, Y5T):
    kb, NB = g.kb, g.NB
    ins = g.ins
    w_in = ins["w_in"]
    T = 256
    import os
    CUT = int(os.environ.get("S5_CUT", "0"))
    chunks = [(0, CTX)] + [(CTX + i * T, T) for i in range(SEQ // T)]
    with kb.scope():
        s5_params(g, L)
        P = g.s5
        uT = Y5T
        yacc = kb.sb("yacc", [128, 4, NT], F32)
        with kb.scope():
            Wu = kb.sb("Wu", [128, 8, 512], BF16)
            kb.dma("pool", Wu[:], w_in[L, :, 3088:3600].rearrange("(k p) c -> p k c", p=128), reads=[w_in], writes=[Wu])
            pu = [kb.ps(f"pu{i}", [128, 512], F32) for i in range(2)]
            n = 0
            for c in range(4):
                for (t0, tn) in TCH:
                    A = pu[n % 2]
                    n += 1
                    for k in range(8):
                        kb.mm(A[:, :tn], Wu[:, k, c * 128:(c + 1) * 128], hT[:, k, t0:t0 + tn], k == 0, k == 7, [Wu, hT], [A])
                    kb.cp("dve", uT[:, c, t0:t0 + tn], A[:, :tn], [A], [uT])
                    kb.ts("dve", yacc[:, c, t0:t0 + tn], A[:, :tn], P["Dc"][:, c:c + 1], None, ALU.mult, None, [A, P["Dc"]], [yacc])
        if CUT == 2:
            return
        with kb.scope():
            iot = kb.sb("iot", [128, T], F32)
            kb.op("pool", lambda e: e.iota(iot[:], pattern=[[1, T]], base=0, channel_multiplier=0, allow_small_or_imprecise_dtypes=True), writes=[iot])
            ang = kb.sb("ang", [128, T], F32)
            ctab = kb.sb("ctab", [128, T], F32)
            stab = kb.sb("stab", [128, T], F32)
            rrow = kb.sb("rrow", [128, T], F32)
            pb = [kb.ps(f"pbu{i}", [128, 512], F32) for i in range(4)]
            py = [kb.ps(f"py{i}", [128, 512], F32) for i in range(2)]
            v2 = [[kb.sb(f"v{i}_{q}", [128, T], F32) for i in range(2)] for q in range(2)]
            tmp2 = [[kb.sb(f"tmp{i}_{q}", [128, T], F32) for i in range(4)] for q in range(2)]
            sh2 = [[kb.sb(f"sh{i}_{q}", [128, T], F32) for i in range(2)] for q in range(2)]
            sf2 = [[kb.sb(f"sf{i}_{q}", [128, T], F32) for i in range(2)] for q in range(2)]
            ET = kb.sb("ET", [128, 2], F32)
            et = kb.sb("et", [128, 4], F32)
            sbf = [[kb.sb(f"sbf{i}_{k}", [128, T], BF16) for i in range(2)] for k in range(2)]
            init = kb.sb("init", [128, 2], F32)
            it = kb.sb("it", [128, 2], F32)
            n = 0
            for d in range(2):
                for j in range(16):
                    kc = j // 4
                    thc = P["th"][:, d, j:j + 1]
                    kb.ts("dve", ang[:], iot[:], thc, None, ALU.mult, None, [iot, P["th"]], [ang])
                    s5_sincos(g, ang[:], stab[:], ctab[:], [ang], [stab, ctab], T)
                    kb.ts("dve", rrow[:], iot[:], 0.0, P["rr"][:, d, j:j + 1], ALU.mult, ALU.add, [iot, P["rr"]], [rrow])
                    kb.op("dve", lambda e: e.memset(init[:], 0.0), writes=[init])
                    c1c, s1c = P["c1"][:, d, j:j + 1], P["s1"][:, d, j:j + 1]
                    kb.ts("dve", et[:, 0:1], ctab[:, T - 1:T], c1c, None, ALU.mult, None, [ctab, P["c1"]], [et])
                    kb.ts("dve", et[:, 1:2], stab[:, T - 1:T], s1c, None, ALU.mult, None, [stab, P["s1"]], [et])
                    kb.ts("dve", et[:, 2:3], stab[:, T - 1:T], c1c, None, ALU.mult, None, [stab, P["c1"]], [et])
                    kb.ts("dve", et[:, 3:4], ctab[:, T - 1:T], s1c, None, ALU.mult, None, [ctab, P["s1"]], [et])
                    kb.tt("dve", ET[:, 0:1], et[:, 0:1], et[:, 1:2], ALU.subtract, [et], [ET])
                    kb.tt("dve", ET[:, 1:2], et[:, 2:3], et[:, 3:4], ALU.add, [et], [ET])
                    if CUT == 3:
                        return
                    order = chunks if d == 0 else [chunks[0]] + chunks[:0:-1]
                    for ci, (t0, tn) in enumerate(order):
                        def rv(ap_full):
                            a = ap_full[:, 0:tn]
                            return a if d == 0 else a[:, ::-1]
                        assert tn == T
                        v, tmp, sh, sf = v2[n % 2], tmp2[n % 2], sh2[n % 2], sf2[n % 2]
                        Bre_p, Bim_p = pb[(2 * n) % 4], pb[(2 * n + 1) % 4]
                        Y = py[n % 2]
                        SB = sbf[n % 2]
                        n += 1
                        kb.mm(Bre_p[:, :tn], P["BxT"][:, d, 0, j, :], uT[:, kc, t0:t0 + tn], True, True, [P["BxT"], uT], [Bre_p])
                        kb.mm(Bim_p[:, :tn], P["BxT"][:, d, 1, j, :], uT[:, kc, t0:t0 + tn], True, True, [P["BxT"], uT], [Bim_p])
                        c_, s_ = ctab[:, :tn], stab[:, :tn]
                        kb.tt("dve", tmp[0][:, :tn], rv(Bre_p), c_, ALU.mult, [Bre_p, ctab], [tmp[0]])
                        kb.tt("dve", tmp[1][:, :tn], rv(Bim_p), s_, ALU.mult, [Bim_p, stab], [tmp[1]])
                        kb.tt("dve", tmp[2][:, :tn], rv(Bim_p), c_, ALU.mult, [Bim_p, ctab], [tmp[2]])
                        kb.tt("dve", tmp[3][:, :tn], rv(Bre_p), s_, ALU.mult, [Bre_p, stab], [tmp[3]])
                        kb.tt("pool", v[0][:, :tn], tmp[0][:, :tn], tmp[1][:, :tn], ALU.add, [tmp[0], tmp[1]], [v[0]])
                        kb.tt("pool", v[1][:, :tn], tmp[2][:, :tn], tmp[3][:, :tn], ALU.subtract, [tmp[2], tmp[3]], [v[1]])
                        for ri in range(2):
                            kb.op("dve", lambda e: e.tensor_tensor_scan(out=sh[ri][:, :tn], data0=rrow[:, :tn], data1=v[ri][:, :tn],
                                                                          initial=init[:, ri:ri + 1], op0=ALU.mult, op1=ALU.add),
                                  reads=[rrow, v[ri], init], writes=[sh[ri]])
                        kb.ts("dve", it[:, 0:1], sh[0][:, tn - 1:tn], ET[:, 0:1], None, ALU.mult, None, [sh[0], ET], [it])
                        kb.ts("dve", it[:, 1:2], sh[0][:, tn - 1:tn], ET[:, 1:2], None, ALU.mult, None, [sh[0], ET], [it])
                        kb.stt(init[:, 0:1], sh[1][:, tn - 1:tn], ET[:, 1:2], it[:, 0:1], ALU.mult, ALU.subtract, [sh[1], ET, it], [init])
                        kb.ts("dve", init[:, 0:1], init[:, 0:1], -1.0, None, ALU.mult, None, [init], [init])
                        kb.stt(init[:, 1:2], sh[1][:, tn - 1:tn], ET[:, 0:1], it[:, 1:2], ALU.mult, ALU.add, [sh[1], ET, it], [init])
                        kb.tt("pool", tmp[0][:, :tn], sh[0][:, :tn], c_, ALU.mult, [sh[0], ctab], [tmp[0]])
                        kb.tt("pool", tmp[1][:, :tn], sh[1][:, :tn], s_, ALU.mult, [sh[1], stab], [tmp[1]])
                        kb.tt("pool", tmp[2][:, :tn], sh[0][:, :tn], s_, ALU.mult, [sh[0], stab], [tmp[2]])
                        kb.tt("pool", tmp[3][:, :tn], sh[1][:, :tn], c_, ALU.mult, [sh[1], ctab], [tmp[3]])
                        kb.tt("dve", sf[0][:, :tn], tmp[0][:, :tn], tmp[1][:, :tn], ALU.subtract, [tmp[0], tmp[1]], [sf[0]])
                        kb.tt("dve", sf[1][:, :tn], tmp[2][:, :tn], tmp[3][:, :tn], ALU.add, [tmp[2], tmp[3]], [sf[1]])
                        for ri in range(2):
                            dstv = SB[ri][:, :tn] if d == 0 else SB[ri][:, :tn][:, ::-1]
                            kb.cp("act", dstv, sf[ri][:, :tn], [sf[ri]], [SB[ri]])
                        kb.mm(Y[:, :tn], P["Cx"][:, 0, j, :], SB[0][:, :tn], True, False, [P["Cx"], SB[0]], [Y])
                        kb.mm(Y[:, :tn], P["Cx"][:, 1, j, :], SB[1][:, :tn], False, True, [P["Cx"], SB[1]], [Y])
                        kb.tt("dve", yacc[:, kc, t0:t0 + tn], yacc[:, kc, t0:t0 + tn], Y[:, :tn], ALU.add, [yacc, Y], [yacc])
                        if CUT == 4:
                            return
                    if CUT == 5:
                        return
                if CUT == 6:
                    return
        if g.debug and L == 0 and b == 0:
            dbg_out(g, "s5_yacc", yacc, [128, 4, NT])
        if CUT == 7:
            return
        with kb.scope():
            Wg = kb.sb("Wg", [128, 4, 1024], BF16)
            kb.dma("pool", Wg[:], ins["s5_w_glu"][L, :, :].rearrange("(k p) c -> p k c", p=128), reads=[ins["s5_w_glu"]], writes=[Wg])
            gy = kb.sb("gy", [128, 4, NT], BF16)
            x2 = kb.sb("x2", [128, NT], F32)
            for c in range(4):
                x = yacc[:, c, :]
                kb.act(x2[:], x, AF.Square, [yacc], [x2])
                kb.ts("dve", x2[:], x2[:], 0.044715 * math.sqrt(2 / math.pi), math.sqrt(2 / math.pi), ALU.mult, ALU.add, [x2], [x2])
                kb.tt("dve", x2[:], x2[:], x, ALU.mult, [x2, yacc], [x2])
                kb.act(x2[:], x2[:], AF.Tanh, [x2], [x2])
                kb.ts("dve", x2[:], x2[:], 1.0, 0.5, ALU.add, ALU.mult, [x2], [x2])
                kb.tt("dve", gy[:, c, :], x2[:], x, ALU.mult, [x2, yacc], [gy])
            pa = [kb.ps(f"pa{i}", [128, 512], F32) for i in range(2)]
            pbb = [kb.ps(f"pbb{i}", [128, 512], F32) for i in range(2)]
            sg = [kb.sb(f"sg{i}", [128, 512], F32) for i in range(2)]
            n = 0
            for c in range(4):
                for (t0, tn) in TCH:
                    A, B, SG = pa[n % 2], pbb[n % 2], sg[n % 2]
                    n += 1
                    for k in range(4):
                        kb.mm(A[:, :tn], Wg[:, k, c * 128:(c + 1) * 128], gy[:, k, t0:t0 + tn], k == 0, k == 3, [Wg, gy], [A])
                    for k in range(4):
                        kb.mm(B[:, :tn], Wg[:, k, 512 + c * 128:512 + (c + 1) * 128], gy[:, k, t0:t0 + tn], k == 0, k == 3, [Wg, gy], [B])
                    kb.act(SG[:, :tn], B[:, :tn], AF.Sigmoid, [B], [SG])
                    kb.tt("dve", Y5T[:, c, t0:t0 + tn], A[:, :tn], SG[:, :tn], ALU.mult, [A, SG], [Y5T])


def load_lnrep(g, L, names):
    kb, ins = g.kb, g.ins
    g.lnrep = {}
    for nm in names:
        t = kb.sb(nm + "rep", [128, D], F32)
        kb.dma("sp", t[:], ins[nm][L:L + 1, :].broadcast_to([128, D]), reads=[ins[nm]], writes=[t])
        g.lnrep[nm] = t


def post_norm_tile(g, pool, mix_ap, mixT, xt, grep, gname, bname, outT, out_ap):
    kb = g.kb
    t, st, mv, rs = pool["t"], pool["st"], pool["mv"], pool["rs"]
    kb.tt("dve", t[:], mix_ap, grep[:], ALU.mult, [mixT, grep], [t])
    kb.stt(t[:], xt[:], float(g.ALPHA), t[:], ALU.mult, ALU.add, [xt, t], [t])
    for hf in range(2):
        kb.op("dve", lambda e: e.bn_stats(out=st[:, hf, :], in_=t[:, hf * 512:(hf + 1) * 512]), reads=[t], writes=[st])
    kb.op("dve", lambda e: e.bn_aggr(out=mv[:], in_=st[:].rearrange("p a b -> p (a b)")), reads=[st], writes=[mv])
    kb.act(rs[:], mv[:, 1:2], AF.Sqrt, [mv], [rs], bias=g.epsc[:, 0:1])
    kb.op("dve", lambda e: e.reciprocal(out=rs[:], in_=rs[:]), reads=[rs], writes=[rs])
    kb.ts("dve", t[:], t[:], mv[:, 0:1], rs[:, 0:1], ALU.subtract, ALU.mult, [t, mv, rs], [t])
    kb.tt("pool", t[:], t[:], g.lnrep[gname][:], ALU.mult, [t, g.lnrep[gname]], [t])
    kb.tt("pool", t[:], t[:], g.lnrep[bname][:], ALU.add, [t, g.lnrep[bname]], [t])
    kb.dma("sp", out_ap, t[:], reads=[t], writes=[outT])


def stage_merge(g, L, b, hT, Ys, Xsrc):
    kb, NB = g.kb, g.NB
    ins = g.ins
    w_in = ins["w_in"]
    nctx = CTX // 128
    with kb.scope():
        zT = kb.sb("zT", [128, 8, NT], BF16)
        with kb.scope():
            Wg = [kb.sb(f"Wgm{i}", [128, 8, 4, 128], BF16) for i in range(2)]
            Wb = [kb.sb(f"Wbm{i}", [128, 4, 4, 128], BF16) for i in range(2)]
            zf = kb.sb("zf", [128, 512], F32)
            sg = [kb.sb(f"msg{i}", [128, 512], F32) for i in range(2)]
            pG = [kb.ps(f"pG{i}", [128, 512], F32) for i in range(2)]
            pM = [kb.ps(f"pM{i}", [128, 512], F32) for i in range(2)]
            n = 0
            for fc in range(8):
                WG, WB = Wg[fc % 2], Wb[fc % 2]
                for br in range(4):
                    c0 = 3600 + br * 1024 + fc * 128
                    kb.dma("pool", WG[:, :, br, :], w_in[L, :, c0:c0 + 128].rearrange("(k p) c -> p k c", p=128), reads=[w_in], writes=[WG])
                    wsrc = ins["w_branch"]
                    if br in (0, 2):
                        for i in range(4):
                            for j in range(2):
                                h = 4 * j + i
                                kb.dma("pool", WB[j * 64:(j + 1) * 64, br, i, :], wsrc[L, br, h * 64:(h + 1) * 64, fc * 128:(fc + 1) * 128],
                                       reads=[wsrc], writes=[WB])
                    else:
                        kb.dma("pool", WB[:, br, :, :], wsrc[L, br, :, fc * 128:(fc + 1) * 128].rearrange("(k p) c -> p k c", p=128),
                               reads=[wsrc], writes=[WB])
                for (t0, tn) in TCH:
                    for br in range(4):
                        G_, M_, S_ = pG[n % 2], pM[n % 2], sg[n % 2]
                        n += 1
                        for k in range(8):
                            kb.mm(G_[:, :tn], WG[:, k, br, :], hT[:, k, t0:t0 + tn], k == 0, k == 7, [WG, hT], [G_])
                        for k in range(4):
                            kb.mm(M_[:, :tn], WB[:, br, k, :], Ys[br][:, k, t0:t0 + tn], k == 0, k == 3, [WB, Ys[br]], [M_])
                        kb.act(S_[:, :tn], G_[:, :tn], AF.Sigmoid, [G_], [S_])
                        if br == 0:
                            kb.tt("dve", zf[:, :tn], M_[:, :tn], S_[:, :tn], ALU.mult, [M_, S_], [zf])
                        else:
                            kb.tt("dve", S_[:, :tn], M_[:, :tn], S_[:, :tn], ALU.mult, [M_, S_], [S_])
                            if br < 3:
                                kb.tt("pool", zf[:, :tn], zf[:, :tn], S_[:, :tn], ALU.add, [zf, S_], [zf])
                            else:
                                kb.tt("pool", zT[:, fc, t0:t0 + tn], zf[:, :tn], S_[:, :tn], ALU.add, [zf, S_], [zT])
        with kb.scope():
            load_lnrep(g, L, ("ln1_g", "ln1_b"))
            Wo = kb.sb("Wo", [128, 8, D], BF16)
            for hf in range(2):
                kb.dma("pool", Wo[:, :, hf * 512:(hf + 1) * 512], ins["w_out"][L, :, hf * 512:(hf + 1) * 512].rearrange("(k p) c -> p k c", p=128),
                       reads=[ins["w_out"]], writes=[Wo])
            pX = [kb.ps(f"pX{i}", [128, D], F32) for i in range(2)]
            xts = [kb.sb(f"mxt{i}", [128, D], F32) for i in range(2)]
            pool = {"t": kb.sb("pn_t", [128, D], F32), "st": kb.sb("pn_st", [128, 2, 6], F32),
                    "mv": kb.sb("pn_mv", [128, 2], F32), "rs": kb.sb("pn_rs", [128, 1], F32)}
            for i in range(NTILE):
                PX, xt = pX[i % 2], xts[i % 2]
                kb.dma("act", xt[:], Xsrc[b, i * 128:(i + 1) * 128, :], reads=[Xsrc], writes=[xt])
                for hf in range(2):
                    for k in range(8):
                        kb.mm(PX[:, hf * 512:(hf + 1) * 512], zT[:, k, i * 128:(i + 1) * 128], Wo[:, k, hf * 512:(hf + 1) * 512], k == 0, k == 7, [zT, Wo], [PX])
                grep = g.greps[NB if i < nctx else b][0]
                post_norm_tile(g, pool, PX[:], PX, xt, grep, "ln1_g", "ln1_b", g.X1, g.X1[b, i * 128:(i + 1) * 128, :])


def stage_moe(g, L, b, last):
    kb, NB = g.kb, g.NB
    ins = g.ins
    nctx = CTX // 128
    t_start = CTX if last else 0
    tiles = list(range(t_start // 128, NTILE))
    chunks = [(t0, min(512, NT - t0)) for t0 in range(t_start, NT, 512)]
    with kb.scope():
        h2T = kb.sb("h2T", [128, 8, NT], BF16)
        stage_ln(g, b, g.X1, 32, 24, h2T)
        Yacc = kb.sb("Yacc2", [128, NTILE, D], F32)
        Wt = kb.sb("Wt", [128, NTILE, 32], F32)
        with kb.scope():
            rw = kb.sb("rw", [128, 8, 32], BF16)
            kb.dma("pool", rw[:], ins["router_w"][:, :].rearrange("(k p) c -> p k c", p=128), reads=[ins["router_w"]], writes=[rw])
            rb = kb.sb("rb", [128, 32], F32)
            kb.dma("sp", rb[:], ins["router_bias"][:].rearrange("(o c) -> o c", o=1).broadcast_to([128, 32]), reads=[ins["router_bias"]], writes=[rb])
            pr_ = [kb.ps(f"prt{i}", [128, 512], F32) for i in range(2)]
            sc = kb.sb("sc", [128, 32], F32)
            sel = kb.sb("sel_", [128, 32], F32)
            eq = kb.sb("eq", [128, 32], F32)
            m1 = kb.sb("m1", [128, 8], F32)
            m2 = kb.sb("m2", [128, 8], F32)
            gs = kb.sb("gs", [128, 8], F32)
            gm = kb.sb("gm", [128, 1], F32)
            v3 = lambda t: t[:].rearrange("p (a b) -> p a b", a=8)
            bc = lambda t: t[:].unsqueeze(2).broadcast_to([128, 8, 4])
            for i in tiles:
                Pp = pr_[i % 2]
                for k in range(8):
                    kb.mm(Pp[:, 0:32], h2T[:, k, i * 128:(i + 1) * 128], rw[:, k, :], k == 0, k == 7, [h2T, rw], [Pp])
                kb.act(sc[:], Pp[:, 0:32], AF.Sigmoid, [Pp], [sc])
                kb.tt("dve", sel[:], sc[:], rb[:], ALU.add, [sc, rb], [sel])
                kb.op("dve", lambda e: e.tensor_reduce(out=m1[:], in_=v3(sel), axis=AX.X, op=ALU.max), reads=[sel], writes=[m1])
                kb.tt("dve", v3(eq), v3(sel), bc(m1), ALU.is_equal, [sel, m1], [eq])
                kb.stt(eq[:], eq[:], -1000.0, sel[:], ALU.mult, ALU.add, [eq, sel], [eq])
                kb.op("dve", lambda e: e.tensor_reduce(out=m2[:], in_=v3(eq), axis=AX.X, op=ALU.max), reads=[eq], writes=[m2])
                kb.tt("dve", gs[:], m1[:], m2[:], ALU.add, [m1, m2], [gs])
                kb.op("dve", lambda e: e.tensor_reduce(out=gm[:], in_=gs[:], axis=AX.X, op=ALU.max), reads=[gs], writes=[gm])
                kb.ts("dve", gs[:], gs[:], gm[:, 0:1], None, ALU.is_equal, None, [gs, gm], [gs])
                kb.tt("dve", v3(eq), v3(sel), bc(m2), ALU.is_ge, [sel, m2], [eq])
                kb.tt("dve", v3(eq), v3(eq), bc(gs), ALU.mult, [eq, gs], [eq])
                kb.tt("dve", eq[:], eq[:], sc[:], ALU.mult, [eq, sc], [eq])
                kb.op("dve", lambda e: e.tensor_reduce(out=gm[:], in_=eq[:], axis=AX.X, op=ALU.add), reads=[eq], writes=[gm])
                kb.op("dve", lambda e: e.reciprocal(out=gm[:], in_=gm[:]), reads=[gm], writes=[gm])
                kb.ts("dve", Wt[:, i, :], eq[:], gm[:, 0:1], None, ALU.mult, None, [eq, gm], [Wt])
        if g.debug and L == 0 and b == 0:
            dbg_out(g, "Wt", Wt, [128, NTILE, 32])
        with kb.scope():
            Wgu = [kb.sb(f"Wgu{i}", [128, 2, 8, 512], BF16) for i in range(2)]
            Wd = [kb.sb(f"Wd{i}", [128, 4, D], BF16) for i in range(2)]
            AT = [kb.sb(f"AT{i}", [128, 4, 512], BF16) for i in range(2)]
            sgm = [kb.sb(f"sgm{i}", [128, 512], BF16) for i in range(2)]
            pGm = [kb.ps(f"pGm{i}", [128, 512], F32) for i in range(2)]
            pUm = [kb.ps(f"pUm{i}", [128, 512], F32) for i in range(2)]
            pY = [kb.ps(f"pY{i}", [128, D], F32) for i in range(2)]
            n = 0
            ny = 0
            na = 0
            for e_ in range(32):
                WGU, WD = Wgu[e_ % 2], Wd[e_ % 2]
                kb.dma("pool", WGU[:, 0], ins["moe_w_gate"][L, e_, :, :].rearrange("(k p) c -> p k c", p=128), reads=[ins["moe_w_gate"]], writes=[WGU])
                kb.dma("pool", WGU[:, 1], ins["moe_w_up"][L, e_, :, :].rearrange("(k p) c -> p k c", p=128), reads=[ins["moe_w_up"]], writes=[WGU])
                for hf in range(2):
                    kb.dma("pool", WD[:, :, hf * 512:(hf + 1) * 512], ins["moe_w_down"][L, e_, :, hf * 512:(hf + 1) * 512].rearrange("(k p) c -> p k c", p=128),
                           reads=[ins["moe_w_down"]], writes=[WD])
                for (t0, tn) in chunks:
                    A_ = AT[na % 2]
                    na += 1
                    for fcx in range(4):
                        G_, U_, S_ = pGm[n % 2], pUm[n % 2], sgm[n % 2]
                        n += 1
                        for k in range(8):
                            kb.mm(G_[:, :tn], WGU[:, 0, k, fcx * 128:(fcx + 1) * 128], h2T[:, k, t0:t0 + tn], k == 0, k == 7, [WGU, h2T], [G_])
                        for k in range(8):
                            kb.mm(U_[:, :tn], WGU[:, 1, k, fcx * 128:(fcx + 1) * 128], h2T[:, k, t0:t0 + tn], k == 0, k == 7, [WGU, h2T], [U_])
                        kb.act(S_[:, :tn], G_[:, :tn], AF.Silu, [G_], [S_])
                        kb.tt("dve", A_[:, fcx, :tn], U_[:, :tn], S_[:, :tn], ALU.mult, [U_, S_], [A_])
                    for ti in range(tn // 128):
                        i = t0 // 128 + ti
                        Y_ = pY[ny % 2]
                        ny += 1
                        for hf in range(2):
                            for k in range(4):
                                kb.mm(Y_[:, hf * 512:(hf + 1) * 512], A_[:, k, ti * 128:(ti + 1) * 128], WD[:, k, hf * 512:(hf + 1) * 512], k == 0, k == 3, [A_, WD], [Y_])
                        if e_ == 0:
                            kb.ts("dve", Yacc[:, i, :], Y_[:], Wt[:, i, e_:e_ + 1], None, ALU.mult, None, [Y_, Wt], [Yacc])
                        else:
                            kb.stt(Yacc[:, i, :], Y_[:], Wt[:, i, e_:e_ + 1], Yacc[:, i, :], ALU.mult, ALU.add, [Y_, Wt, Yacc], [Yacc])
        if g.debug and L == 0 and b == 0:
            dbg_out(g, "ffn", Yacc, [128, NTILE, D])
        with kb.scope():
            load_lnrep(g, L, ("ln2_g", "ln2_b"))
            xts = [kb.sb(f"fxt{i}", [128, D], F32) for i in range(2)]
            pool = {"t": kb.sb("pn2_t", [128, D], F32), "st": kb.sb("pn2_st", [128, 2, 6], F32),
                    "mv": kb.sb("pn2_mv", [128, 2], F32), "rs": kb.sb("pn2_rs", [128, 1], F32)}
            for i in tiles:
                xt = xts[i % 2]
                kb.dma("act", xt[:], g.X1[b, i * 128:(i + 1) * 128, :], reads=[g.X1], writes=[xt])
                grep = g.greps[NB if i < nctx else b][1]
                if last:
                    oT, oap = g.out, g.out[b, (i - nctx) * 128:(i - nctx + 1) * 128, :]
                else:
                    oT, oap = g.X2, g.X2[b, i * 128:(i + 1) * 128, :]
                post_norm_tile(g, pool, Yacc[:, i, :], Yacc, xt, grep, "ln2_g", "ln2_b", oT, oap)


WEIGHT_NAMES = ["mod_w", "mod_b", "w_in", "wa_sink", "ga_q_norm", "ga_k_norm", "ssd_conv_w", "ssd_conv_b", "ssd_dt_bias",
                "ssd_a_log", "ssd_d", "ssd_norm_w", "s5_a_re", "s5_a_im", "s5_log_dt", "s5_b_re", "s5_b_im", "s5_c_re",
                "s5_c_im", "s5_d", "s5_w_glu", "w_branch", "w_out", "ln1_g", "ln1_b", "ln2_g", "ln2_b", "router_w",
                "router_bias", "moe_w_gate", "moe_w_up", "moe_w_down"]


def kernel(**inputs):
    NB = 2
    n_cores = 8
    nc, g = build(NB=NB, DEPTH=4, debug=False)
    consts = host_consts()
    x = np.asarray(inputs["x"], dtype=np.float32)
    ctx = np.asarray(inputs["ctx"], dtype=np.float32)
    c = np.asarray(inputs["c"], dtype=np.float32)
    c_ctx = np.asarray(inputs["c_ctx"], dtype=np.float32)
    weights = {k: np.ascontiguousarray(np.asarray(inputs[k], dtype=np.float32)) for k in WEIGHT_NAMES}
    in_maps = []
    for core in range(n_cores):
        bs = slice(core * NB, (core + 1) * NB)
        m = {}
        m["xin"] = np.ascontiguousarray(np.concatenate([ctx[bs], x[bs]], axis=1))
        m["cvec"] = np.ascontiguousarray(np.concatenate([c[bs], c_ctx[None, :]], axis=0))
        m.update(weights)
        m.update(consts)
        in_maps.append(m)
    res = run_bass_kernel_spmd(nc, in_maps, core_ids=list(range(n_cores)))
    out = np.concatenate([np.asarray(res.results[i]["out"]) for i in range(n_cores)], axis=0)
    return out.astype(np.float32)
```

```python
import contextlib
import math
import numpy as np
import ml_dtypes
import concourse.bass as bass
import concourse.mybir as mybir
from concourse.bass_utils import run_bass_kernel_spmd

F32 = mybir.dt.float32
BF16 = mybir.dt.bfloat16
I32 = mybir.dt.int32
AF = mybir.ActivationFunctionType
ALU = mybir.AluOpType
AX = mybir.AxisListType

SEM_LIM = 20000
ND = 10

D = 1024
CTX = 256
SEQ = 2048
NT = CTX + SEQ
NTILE = NT // 128
GRID_W = 64
EPS = 1e-6
D_IN = 7696
TCH = [(0, 512), (512, 512), (1024, 512), (1536, 512), (2048, 256)]
NEG = -30000.0


class TT:
    __slots__ = ("h", "name", "w", "r", "ps")

    def __init__(self, h, name, ps=False):
        self.h = h
        self.name = name
        self.w = None
        self.r = {}
        self.ps = ps

    def __getitem__(self, idx):
        return self.h[idx]


class KB:
    def __init__(self, nc):
        self.nc = nc
        self.es = contextlib.ExitStack()
        self.alloc_es = self.es
        self.engines = {"pe": nc.tensor, "dve": nc.vector, "act": nc.scalar,
                        "pool": nc.gpsimd, "sp": nc.sync}
        self.cur = {}
        self.known = {k: {} for k in self.engines}
        self.allsems = []
        self.dpool = {}
        self.dnext = {}
        self.nsem = 0
        self.uid = 0
        self.ninst = 0
        self.pe_sems = set()

    def _name(self, name):
        self.uid += 1
        return f"{name}_{self.uid}"

    def sb(self, name, shape, dtype=F32):
        n = self._name(name)
        h = self.alloc_es.enter_context(self.nc.sbuf_tensor(n, list(shape), dtype))
        return TT(h, n)

    def ps(self, name, shape, dtype=F32):
        n = self._name(name)
        h = self.alloc_es.enter_context(self.nc.psum_tensor(n, list(shape), dtype))
        return TT(h, n, ps=True)

    def dram(self, name, shape, dtype=F32, kind="Internal"):
        h = self.nc.dram_tensor(name, list(shape), dtype, kind=kind)
        return TT(h, name)

    @contextlib.contextmanager
    def scope(self):
        es = contextlib.ExitStack()
        old = self.alloc_es
        self.alloc_es = es
        try:
            yield
        finally:
            self.barrier()
            es.close()
            self.alloc_es = old

    def _newsem(self, name):
        self.nsem += 1
        s = self.es.enter_context(self.nc.semaphore(f"{name}_{self.nsem}"))
        rec = [s, 0]
        self.allsems.append(rec)
        return rec

    def _deps(self, reads, writes):
        d = {}

        def add(ev):
            rec, v = ev
            k = id(rec)
            if k not in d or d[k][1] < v:
                d[k] = (rec, v)
        for t in reads:
            if t.w is not None:
                add(t.w)
            if t.ps:
                for ev in t.r.values():
                    add(ev)
        for t in writes:
            if t.w is not None:
                add(t.w)
            for ev in t.r.values():
                add(ev)
        return d

    def _emit(self, e, fn, deps):
        eng = self.engines[e]
        kn = self.known[e]
        waits = []
        for k, (rec, v) in deps.items():
            if e == "pe" and k in self.pe_sems:
                continue
            if kn.get(k, 0) < v:
                waits.append((rec, v))
                kn[k] = v
        for rec, v in waits[:-1]:
            eng.wait_ge(rec[0], v)
            self.ninst += 1
        ins = fn(eng)
        self.ninst += 1
        if waits:
            rec, v = waits[-1]
            ins._wait_ge(rec[0], v)
        return ins

    def _record(self, ev, reads, writes):
        rec, v = ev
        for t in reads:
            t.r[id(rec)] = ev
        for t in writes:
            t.w = ev
            t.r = {}

    def op(self, e, fn, reads=(), writes=()):
        deps = self._deps(reads, writes)
        ins = self._emit(e, fn, deps)
        rec = self.cur.get(e)
        if rec is None or rec[1] >= SEM_LIM:
            rec = self.cur[e] = self._newsem("s" + e)
            if e == "pe":
                self.pe_sems.add(id(rec))
        rec[1] += 1
        ins.then_inc(rec[0], 1)
        self._record((rec, rec[1]), reads, writes)
        return ins

    def dma(self, q, out, in_, reads=(), writes=(), **kw):
        if q not in self.dpool:
            self.dpool[q] = [self._newsem("d" + q) for _ in range(ND)]
            self.dnext[q] = 0
        i = self.dnext[q]
        self.dnext[q] = i + 1
        rec = self.dpool[q][i % ND]
        if rec[1] >= SEM_LIM:
            rec = self.dpool[q][i % ND] = self._newsem("d" + q)
        deps = self._deps(reads, writes)
        if rec[1] > 0:
            k = id(rec)
            if k not in deps or deps[k][1] < rec[1]:
                deps[k] = (rec, rec[1])
        ins = self._emit(q, lambda eng: eng.dma_start(out=out, in_=in_, **kw), deps)
        rec[1] += 16
        ins.then_inc(rec[0], 16)
        self._record((rec, rec[1]), reads, writes)
        return ins

    def barrier(self):
        for e, eng in self.engines.items():
            kn = self.known[e]
            for rec in self.allsems:
                if rec[1] > 0 and kn.get(id(rec), 0) < rec[1]:
                    eng.wait_ge(rec[0], rec[1])
                    self.ninst += 1
                    kn[id(rec)] = rec[1]

    def close(self):
        self.barrier()
        self.es.close()

    def mm(self, out, lhsT, rhs, start, stop, R, W):
        return self.op("pe", lambda e: e.matmul(out, lhsT=lhsT, rhs=rhs, start=start, stop=stop), reads=R, writes=W)

    def mm32(self, out, lhsT, rhs, start, stop, R, W, junk, jt, identb):
        self.mm(out, lhsT, rhs, start, stop, R, W)
        if stop:
            self.op("pe", lambda e: e.matmul(junk, lhsT=identb[:, 0:32], rhs=identb[:, 0:1], start=True, stop=True),
                    reads=[], writes=list(W) + ([jt] if jt not in W else []))

    def tr(self, out, in_, ident, R, W):
        return self.op("pe", lambda e: e.transpose(out=out, in_=in_, identity=ident), reads=R, writes=W)

    def act(self, out, in_, func, R, W, **kw):
        return self.op("act", lambda e: e.activation(out=out, in_=in_, func=func, **kw), reads=R, writes=W)

    def ts(self, eng, out, in0, s1, s2, op0, op1, R, W):
        if op1 is None:
            return self.op(eng, lambda e: e.tensor_scalar(out=out, in0=in0, scalar1=s1, scalar2=None, op0=op0), reads=R, writes=W)
        return self.op(eng, lambda e: e.tensor_scalar(out=out, in0=in0, scalar1=s1, scalar2=s2, op0=op0, op1=op1), reads=R, writes=W)

    def tt(self, eng, out, in0, in1, op, R, W):
        return self.op(eng, lambda e: e.tensor_tensor(out=out, in0=in0, in1=in1, op=op), reads=R, writes=W)

    def stt(self, out, in0, scalar, in1, op0, op1, R, W):
        return self.op("dve", lambda e: e.scalar_tensor_tensor(out=out, in0=in0, scalar=scalar, in1=in1, op0=op0, op1=op1), reads=R, writes=W)

    def cp(self, eng, out, in_, R, W):
        if eng == "act":
            return self.act(out, in_, AF.Copy, R, W)
        return self.op(eng, lambda e: e.tensor_copy(out=out, in_=in_), reads=R, writes=W)


def host_consts():
    c = {}
    c["c_identb"] = np.eye(128).astype(ml_dtypes.bfloat16)
    c["c_identf"] = np.eye(128).astype(np.float32)
    rows = SEQ // GRID_W
    row = np.repeat(np.arange(rows, dtype=np.float32), GRID_W)
    col = np.tile(np.arange(GRID_W, dtype=np.float32), rows)
    axis_dim = 32
    inv_freq = (10000.0 ** (-np.arange(0, axis_dim, 2, dtype=np.float32) / axis_dim)).astype(np.float32)
    ang_r = row[:, None] * inv_freq
    ang_c = col[:, None] * inv_freq
    ang = np.concatenate([ang_r, ang_r, ang_c, ang_c], axis=-1)
    cos = np.cos(ang).T
    sin = np.sin(ang).T
    sign = np.where((np.arange(64) % 32) < 16, -1.0, 1.0)[:, None]
    tab = np.zeros((128, 2, NT), np.float32)
    tab[:, 0, :CTX] = 1.0
    for h in range(2):
        tab[h * 64:(h + 1) * 64, 0, CTX:] = cos
        tab[h * 64:(h + 1) * 64, 1, CTX:] = sin * sign
    c["c_rope"] = tab.astype(ml_dtypes.bfloat16)
    perm = np.zeros((128, 128), np.float32)
    for h in range(2):
        for m in range(64):
            src = m + 16 if (m % 32) < 16 else m - 16
            perm[h * 64 + src, h * 64 + m] = 1.0
    c["c_perm"] = perm.astype(ml_dtypes.bfloat16)
    on2 = np.zeros((128, 128), np.float32)
    on2[:64, :64] = 1.0 / 64
    on2[64:, 64:] = 1.0 / 64
    c["c_ones2"] = on2.astype(ml_dtypes.bfloat16)
    k = np.arange(128)[:, None]
    q = np.arange(128)[None, :]
    mL = np.where(k >= q, 0.0, NEG).astype(np.float32)
    mU = np.where(k <= q, 0.0, NEG).astype(np.float32)
    c["c_maskL"] = np.tile(mL, (1, 4)).astype(ml_dtypes.bfloat16)
    c["c_maskU"] = np.tile(mU, (1, 4)).astype(ml_dtypes.bfloat16)
    c["c_ones"] = np.ones((128, 128), ml_dtypes.bfloat16)
    c["c_triF"] = (k <= q).astype(np.float32)
    c["c_triB"] = (k >= q).astype(np.float32)
    c["c_nmF"] = np.where(k <= q, 0.0, NEG).astype(ml_dtypes.bfloat16)
    c["c_nmB"] = np.where(k >= q, 0.0, NEG).astype(ml_dtypes.bfloat16)
    return c


class G:
    pass


def build(NB=2, DEPTH=4, ALPHA=8 ** 0.25, debug=False, stages=None, use_moe=True):
    nc = bass.Bass("TRN2", target_bir_lowering=False)
    kb = KB(nc)
    g = G()
    g.kb, g.nc, g.NB, g.DEPTH, g.ALPHA, g.debug = kb, nc, NB, DEPTH, ALPHA, debug
    g.dbg = {}
    ins = {}

    def inp(name, shape, dt=F32):
        ins[name] = kb.dram(name, shape, dt, kind="ExternalInput")
        return ins[name]
    g.ins = ins
    inp("xin", [NB, NT, D])
    inp("cvec", [NB + 1, D])
    Ld = DEPTH
    inp("mod_w", [Ld, D, 6 * D]); inp("mod_b", [Ld, 6 * D]); inp("w_in", [Ld, D, D_IN])
    inp("wa_sink", [Ld, 8]); inp("ga_q_norm", [Ld, 64]); inp("ga_k_norm", [Ld, 64])
    inp("ssd_conv_w", [Ld, 3, 1024]); inp("ssd_conv_b", [Ld, 1024]); inp("ssd_dt_bias", [Ld, 16])
    inp("ssd_a_log", [Ld, 2, 8]); inp("ssd_d", [Ld, 8]); inp("ssd_norm_w", [Ld, 512])
    inp("s5_a_re", [Ld, 2, 32, 64]); inp("s5_a_im", [Ld, 2, 32, 64]); inp("s5_log_dt", [Ld, 2, 32])
    inp("s5_b_re", [Ld, 32, 64, 16]); inp("s5_b_im", [Ld, 32, 64, 16])
    inp("s5_c_re", [Ld, 32, 16, 64]); inp("s5_c_im", [Ld, 32, 16, 64])
    inp("s5_d", [Ld, 512]); inp("s5_w_glu", [Ld, 512, 1024])
    inp("w_branch", [Ld, 4, 512, D]); inp("w_out", [Ld, D, D])
    inp("ln1_g", [Ld, D]); inp("ln1_b", [Ld, D]); inp("ln2_g", [Ld, D]); inp("ln2_b", [Ld, D])
    inp("router_w", [D, 32]); inp("router_bias", [32])
    if use_moe:
        inp("moe_w_gate", [Ld, 32, D, 512]); inp("moe_w_up", [Ld, 32, D, 512]); inp("moe_w_down", [Ld, 32, 512, D])
    hc = host_consts()
    for k, v in hc.items():
        inp(k, list(v.shape), BF16 if v.dtype == ml_dtypes.bfloat16 else F32)
    g.out = kb.dram("out", [NB, SEQ, D], F32, kind="ExternalOutput")
    g.X1 = kb.dram("X1", [NB, NT, D], F32, kind="ExternalOutput" if debug else "Internal")
    g.X2 = kb.dram("X2", [NB, NT, D], F32, kind="ExternalOutput" if debug else "Internal")

    def cload(name, shape, dt):
        t = kb.sb(name, shape, dt)
        kb.dma("sp", t[:], ins[name][:], reads=[ins[name]], writes=[t])
        return t
    g.identb = cload("c_identb", [128, 128], BF16)
    g.identf = cload("c_identf", [128, 128], F32)
    g.perm = cload("c_perm", [128, 128], BF16)
    g.ones2 = cload("c_ones2", [128, 128], BF16)
    g.maskL = cload("c_maskL", [128, 512], BF16)
    g.maskU = cload("c_maskU", [128, 512], BF16)
    g.ones = cload("c_ones", [128, 128], BF16)
    g.triF = cload("c_triF", [128, 128], F32)
    g.triB = cload("c_triB", [128, 128], F32)
    g.nmF = cload("c_nmF", [128, 128], BF16)
    g.nmB = cload("c_nmB", [128, 128], BF16)

    run_all(g, stages)
    kb.close()
    return nc, g


def dbg_out(g, name, t, shape, dt=F32):
    if not g.debug:
        return
    kb = g.kb
    d = kb.dram("dbg_" + name, shape, dt, kind="ExternalOutput")
    kb.dma("sp", d[:], t[:], reads=[t], writes=[d])
    g.dbg[name] = d


def stage_mod(g, L):
    kb, NB = g.kb, g.NB
    ins = g.ins
    R = NB + 1
    modT = kb.sb("modT", [128, 48, R], F32)
    greps = [[kb.sb(f"grep{r}_{s}", [128, D], BF16) for s in range(2)] for r in range(R)]
    with kb.scope():
        condT = kb.sb("condT", [128, 8, R], F32)
        for r in range(R):
            kb.dma("sp", condT[:, :, r], ins["cvec"][r, :].rearrange("(k p) -> p k", p=128), reads=[ins["cvec"]], writes=[condT],
                   allow_slow_non_contiguous=True)
        kb.act(condT[:], condT[:], AF.Silu, [condT], [condT])
        mbT = kb.sb("mbT", [128, 48], F32)
        kb.dma("sp", mbT[:], ins["mod_b"][L, :].rearrange("(c p) -> p c", p=128), reads=[ins["mod_b"]], writes=[mbT],
               allow_slow_non_contiguous=True)
        mbrow = kb.sb("mbrow", [R, 6 * D], F32)
        kb.dma("sp", mbrow[:], ins["mod_b"][L:L + 1, :].broadcast_to([R, 6 * D]), reads=[ins["mod_b"]], writes=[mbrow])
        sel = kb.sb("sel", [R, R, 128], F32)
        kb.op("dve", lambda e: e.memset(sel[:], 0.0), writes=[sel])
        kb.op("dve", lambda e: e.tensor_copy(out=sel[:], in_=g.identf[0:R, 0:R].unsqueeze(2).broadcast_to([R, R, 128])),
              reads=[g.identf], writes=[sel])
        wt = [kb.sb(f"modw{i}", [128, 8, D], F32) for i in range(2)]
        pm = kb.ps("pm", [128, 512], F32)
        pr = kb.ps("pr", [128, 512], F32)
        pg = kb.ps("pg", [128, 512], F32)
        pj = kb.ps("pj", [128, 512], F32)
        rows = kb.sb("rows", [R, 512], F32)
        for s in range(6):
            w = wt[s % 2]
            kb.dma("sp" if s % 2 == 0 else "act", w[:], ins["mod_w"][L, :, s * D:(s + 1) * D].rearrange("(k p) c -> p k c", p=128),
                   reads=[ins["mod_w"]], writes=[w])
            for fc in range(8):
                for k in range(8):
                    kb.mm32(pm[:, fc * R:(fc + 1) * R], w[:, k, fc * 128:(fc + 1) * 128], condT[:, k, :], k == 0, k == 7, [w, condT], [pm],
                            pj[0:32, 0:1], pj, g.identb)
            for fc in range(8):
                ch = s * 8 + fc
                kb.ts("dve", modT[:, ch, :], pm[:, fc * R:(fc + 1) * R], mbT[:, ch:ch + 1], 1.0 if s in (1, 4) else 0.0,
                      ALU.add, ALU.add, [pm, mbT], [modT])
            if s in (2, 5):
                gi = 0 if s == 2 else 1
                for half in range(2):
                    for k in range(8):
                        kb.mm32(pr[0:R, :], condT[:, k, :], w[:, k, half * 512:(half + 1) * 512], k == 0, k == 7, [w, condT], [pr],
                                pj[0:32, 0:1], pj, g.identb)
                    kb.tt("dve", rows[:], pr[0:R, :], mbrow[:, s * D + half * 512: s * D + (half + 1) * 512], ALU.add, [pr, mbrow], [rows])
                    for r in range(R):
                        kb.mm32(pg[:], sel[:, r, :], rows[:], True, True, [sel, rows], [pg], pj[0:32, 0:1], pj, g.identb)
                        kb.cp("dve", greps[r][gi][:, half * 512:(half + 1) * 512], pg[:], [pg], [greps[r][gi]])
    g.modT = modT
    g.greps = greps


def stage_ln(g, b, Xsrc, chunkA, chunkB, hT, router=None):
    kb, NB = g.kb, g.NB
    with kb.scope():
        xts = [kb.sb(f"ln_x{i}", [128, D], F32) for i in range(2)]
        xns = [kb.sb(f"ln_xn{i}", [128, D], BF16) for i in range(2)]
        st = kb.sb("ln_st", [128, 2, 6], F32)
        mv = kb.sb("ln_mv", [128, 2], F32)
        rs = kb.sb("ln_rs", [128, 1], F32)
        tps = [kb.ps(f"ln_tp{i}", [128, D], BF16) for i in range(2)]
        for i in range(NTILE):
            col = NB if i < CTX // 128 else b
            xt, xn, tp = xts[i % 2], xns[i % 2], tps[i % 2]
            kb.dma("sp", xt[:], Xsrc[b, i * 128:(i + 1) * 128, :], reads=[Xsrc], writes=[xt])
            for hf in range(2):
                kb.op("dve", lambda e: e.bn_stats(out=st[:, hf, :], in_=xt[:, hf * 512:(hf + 1) * 512]), reads=[xt], writes=[st])
            kb.op("dve", lambda e: e.bn_aggr(out=mv[:], in_=st[:].rearrange("p a b -> p (a b)")), reads=[st], writes=[mv])
            kb.act(rs[:], mv[:, 1:2], AF.Sqrt, [mv], [rs], bias=g.epsc[:, 0:1])
            kb.op("dve", lambda e: e.reciprocal(out=rs[:], in_=rs[:]), reads=[rs], writes=[rs])
            kb.ts("dve", xn[:], xt[:], mv[:, 0:1], rs[:, 0:1], ALU.subtract, ALU.mult, [xt, mv, rs], [xn])
            for c in range(8):
                kb.tr(tp[:, c * 128:(c + 1) * 128], xn[:, c * 128:(c + 1) * 128], g.identb[:], [xn, g.identb], [tp])
            for c in range(8):
                o = hT[:, c, i * 128:(i + 1) * 128]
                a = g.modT[:, chunkA + c, col:col + 1]
                bb = g.modT[:, chunkB + c, col:col + 1]
                if c % 2 == 0:
                    kb.ts("dve", o, tp[:, c * 128:(c + 1) * 128], a, bb, ALU.mult, ALU.add, [tp, g.modT], [hT])
                else:
                    kb.act(o, tp[:, c * 128:(c + 1) * 128], AF.Identity, [tp, g.modT], [hT], scale=a, bias=bb)


def load_w(g, name, dst, src_ap, srcT, q="pool"):
    g.kb.dma(q, dst, src_ap, reads=[srcT], writes=[name])


def stage_attn(g, L, b, hT, YT, kind):
    kb, NB = g.kb, g.NB
    ins = g.ins
    w_in = g.wbd["w_in"]
    c0 = 0 if kind == "a" else 768
    with kb.scope():
        Wq = kb.sb("Wq", [128, 8, 512], BF16)
        for i in range(4):
            for j in range(2):
                h = 4 * j + i
                kb.dma("sp", Wq[:, :, i * 128 + j * 64: i * 128 + (j + 1) * 64],
                       w_in[:, c0 + h * 64: c0 + (h + 1) * 64].rearrange("(k p) c -> p k c", p=128), reads=[w_in], writes=[Wq])
        Wk = kb.sb("Wk", [128, 8, 128], BF16)
        kb.dma("sp", Wk[:], w_in[:, c0 + 512: c0 + 640].rearrange("(k p) c -> p k c", p=128), reads=[w_in], writes=[Wk])
        Wv = kb.sb("Wv", [128, 8, 128], BF16)
        kb.dma("sp", Wv[:], w_in[:, c0 + 640: c0 + 768].rearrange("(k p) c -> p k c", p=128), reads=[w_in], writes=[Wv])
        QT = kb.sb("QT", [128, 4, NT], BF16)
        KT = kb.sb("KT", [128, NT], BF16)
        g.rope = kb.sb("rope", [128, 2, NT], BF16)
        kb.dma("sp", g.rope[:], ins["c_rope"][:], reads=[ins["c_rope"]], writes=[g.rope])
        V = kb.sb("V", [128, NTILE, 128], BF16)
        if kind == "g":
            nw = kb.sb("nw", [128, 2], F32)
            for hf in range(2):
                kb.dma("sp", nw[hf * 64:(hf + 1) * 64, 0:1], ins["ga_q_norm"][L, :].rearrange("(p o) -> p o", o=1), reads=[ins["ga_q_norm"]], writes=[nw])
                kb.dma("sp", nw[hf * 64:(hf + 1) * 64, 1:2], ins["ga_k_norm"][L, :].rearrange("(p o) -> p o", o=1), reads=[ins["ga_k_norm"]], writes=[nw])
        else:
            esink = kb.sb("esink", [128, 4], F32)
            for j in range(2):
                kb.dma("sp", esink[j * 64:(j + 1) * 64, :], ins["wa_sink"][L:L + 1, 4 * j:4 * j + 4].broadcast_to([64, 4]),
                       reads=[ins["wa_sink"]], writes=[esink])
            kb.act(esink[:], esink[:], AF.Exp, [esink], [esink])
        with kb.scope():
            pA = [kb.ps(f"pA{i}", [128, 512], F32) for i in range(2)]
            pB = [kb.ps(f"pB{i}", [128, 512], F32) for i in range(2)]
            pV = kb.ps("pV", [128, 512], F32)
            qa = [kb.sb(f"qa{i}", [128, 512], BF16) for i in range(2)]
            sq = [kb.sb(f"sq{i}", [128, 512], BF16) for i in range(2)]
            rstd = [kb.sb(f"rstd{i}", [128, 512], F32) for i in range(2)]
            t1 = [kb.sb(f"t1{i}", [128, 512], F32) for i in range(2)]
            t2 = [kb.sb(f"t2{i}", [128, 512], F32) for i in range(2)]
            units = [(t0, tn, m) for (t0, tn) in TCH for m in range(5)]

            def bufs(u):
                return pA[u % 2], pB[u % 2], qa[u % 2], sq[u % 2], rstd[u % 2], t1[u % 2], t2[u % 2]

            def ph1(u):
                t0, tn, m = units[u]
                W = Wq if m < 4 else Wk
                wc = m * 128 if m < 4 else 0
                A, B, Q, S, RS, T1, T2 = bufs(u)
                for k in range(8):
                    kb.mm(A[:, :tn], W[:, k, wc:wc + 128], hT[:, k, t0:t0 + tn], k == 0, k == 7, [W, hT], [A])
                if kind == "g":
                    kb.act(S[:, :tn], A[:, :tn], AF.Square, [A], [S])
                else:
                    kb.cp("act", Q[:, :tn], A[:, :tn], [A], [Q])

            def ph2(u):
                if kind != "g":
                    return
                t0, tn, m = units[u]
                A, B, Q, S, RS, T1, T2 = bufs(u)
                kb.mm(B[:, :tn], g.ones2[:], S[:, :tn], True, True, [g.ones2, S], [B])
                kb.act(RS[:, :tn], B[:, :tn], AF.Sqrt, [B], [RS], bias=g.epsc[:, 0:1])
                kb.op("dve", lambda e: e.reciprocal(out=RS[:, :tn], in_=RS[:, :tn]), reads=[RS], writes=[RS])
                kb.stt(T1[:, :tn], A[:, :tn], nw[:, (0 if m < 4 else 1):(1 if m < 4 else 2)], RS[:, :tn], ALU.mult, ALU.mult, [A, nw, RS], [T1])
                kb.cp("act", Q[:, :tn], T1[:, :tn], [T1], [Q])

            def ph3(u):
                t0, tn, m = units[u]
                A, B, Q, S, RS, T1, T2 = bufs(u)
                dst = QT[:, m, t0:t0 + tn] if m < 4 else KT[:, t0:t0 + tn]
                dstT = QT if m < 4 else KT
                src = T1 if kind == "g" else A
                kb.mm(B[:, :tn], g.perm[:], Q[:, :tn], True, True, [g.perm, Q], [B])
                kb.tt("dve", T2[:, :tn], B[:, :tn], g.rope[:, 1, t0:t0 + tn], ALU.mult, [B, g.rope], [T2])
                kb.tt("pool" if src is T1 else "dve", T1[:, :tn], src[:, :tn], g.rope[:, 0, t0:t0 + tn], ALU.mult, [src, g.rope], [T1])
                kb.tt("pool", dst, T1[:, :tn], T2[:, :tn], ALU.add, [T1, T2], [dstT])

            for u in range(len(units) + 1):
                if u < len(units):
                    ph1(u)
                if u - 1 >= 0:
                    ph2(u - 1)
                    ph3(u - 1)
            for i in range(NTILE):
                for k in range(8):
                    kb.mm(pV[:, 0:128], hT[:, k, i * 128:(i + 1) * 128], Wv[:, k, :], k == 0, k == 7, [hT, Wv], [pV])
                kb.cp("act", V[:, i, :], pV[:, 0:128], [pV], [V])
            if g.debug and b == 0 and L == 0:
                dbg_out(g, f"QT{kind}", QT, [128, 4, NT], BF16)
                dbg_out(g, f"KT{kind}", KT, [128, NT], BF16)
                dbg_out(g, f"V{kind}", V, [128, NTILE, 128], BF16)
        pS = [kb.ps(f"pS{i}", [128, 512], F32) for i in range(4)]
        pO = kb.ps("pO", [128, 512], F32)
        pD = kb.ps("pD", [128, 512], F32)
        P = [kb.sb(f"P{i}", [128, 512], BF16) for i in range(4)]
        rec = kb.sb("rec", [128, 512], F32)
        nctx = CTX // 128
        items = []
        for qi in range(NTILE):
            if qi < nctx:
                keys = [(kt, None) for kt in range(nctx)]
            elif kind == "g":
                keys = [(kt, None) for kt in range(NTILE)]
            else:
                keys = [(kt, None) for kt in range(nctx)]
                if qi - 1 >= nctx:
                    keys.append((qi - 1, g.maskL))
                keys.append((qi, None))
                if qi + 1 < NTILE:
                    keys.append((qi + 1, g.maskU))
            for j in range(2):
                for ki, (kt, msk) in enumerate(keys):
                    items.append((qi, j, ki, len(keys), kt, msk))
        LA = 2

        def s_phase(t):
            qi, j, ki, nk, kt, msk = items[t]
            ps_ = slice(j * 64, (j + 1) * 64)
            S_, P_ = pS[t % 4], P[t % 4]
            kb.mm(S_[:], KT[ps_, kt * 128:(kt + 1) * 128], QT[ps_, :, qi * 128:(qi + 1) * 128], True, msk is None, [KT, QT], [S_])
            if msk is not None:
                kb.mm(S_[:], g.identb[:], msk[:], False, True, [g.identb, msk], [S_])
            kb.act(P_[:], S_[:], AF.Exp, [S_], [P_], scale=0.125)

        def pv_phase(t):
            qi, j, ki, nk, kt, msk = items[t]
            ps_ = slice(j * 64, (j + 1) * 64)
            P_ = P[t % 4]
            kb.mm(pO[ps_, :], V[:, kt, ps_], P_[:], ki == 0, ki == nk - 1, [V, P_], [pO])
            kb.mm(pD[ps_, :], g.ones[:, 0:64], P_[:], ki == 0, ki == nk - 1, [g.ones, P_], [pD])
            if j == 1 and ki == nk - 1:
                if kind == "a":
                    kb.tt("dve", rec[:].rearrange("p (i t) -> p i t", i=4), pD[:].rearrange("p (i t) -> p i t", i=4),
                          esink[:].unsqueeze(2).broadcast_to([128, 4, 128]), ALU.add, [pD, esink], [rec])
                    kb.op("dve", lambda e: e.reciprocal(out=rec[:], in_=rec[:]), reads=[rec], writes=[rec])
                else:
                    kb.op("dve", lambda e: e.reciprocal(out=rec[:], in_=pD[:]), reads=[pD], writes=[rec])
                kb.tt("dve", YT[:, :, qi * 128:(qi + 1) * 128], pO[:].rearrange("p (i t) -> p i t", i=4),
                      rec[:].rearrange("p (i t) -> p i t", i=4), ALU.mult, [pO, rec], [YT])

        for t in range(len(items) + LA):
            if t < len(items):
                s_phase(t)
            if t - LA >= 0:
                pv_phase(t - LA)


def stage_ssd(g, L, b, hT, YsT):
    kb, NB = g.kb, g.NB
    ins = g.ins
    w_in = g.wbd["w_in"]
    nctx = CTX // 128
    segs = [(0, CTX), (CTX, NT)]
    with kb.scope():
        bcT = kb.sb("bcT", [128, 4, NT], BF16)
        xbtok = kb.sb("xbtok", [128, NTILE, 768], BF16)
        zs = kb.sb("zs", [128, NTILE, 512], BF16)
        dt = kb.sb("dt", [128, NTILE, 16], F32)
        da = kb.sb("da", [128, NTILE, 16], F32)
        with kb.scope():
            Wc = kb.sb("Wc", [128, 8, 1024], BF16)
            kb.dma("sp", Wc[:, :, 0:512], w_in[:, 1536:2048].rearrange("(k p) c -> p k c", p=128), reads=[w_in], writes=[Wc])
            kb.dma("sp", Wc[:, :, 512:1024], w_in[:, 2560:3072].rearrange("(k p) c -> p k c", p=128), reads=[w_in], writes=[Wc])
            Wz = kb.sb("Wz", [128, 8, 512], BF16)
            kb.dma("sp", Wz[:], w_in[:, 2048:2560].rearrange("(k p) c -> p k c", p=128), reads=[w_in], writes=[Wz])
            Wdt = kb.sb("Wdt", [128, 8, 16], BF16)
            kb.dma("sp", Wdt[:], w_in[:, 3072:3088].rearrange("(k p) c -> p k c", p=128), reads=[w_in], writes=[Wdt])
            cw = kb.sb("cw", [128, 8, 3], F32)
            for k in range(3):
                kb.dma("sp", cw[:, :, k], ins["ssd_conv_w"][L, k, :].rearrange("(c p) -> p c", p=128), reads=[ins["ssd_conv_w"]], writes=[cw],
                       allow_slow_non_contiguous=True)
            cb = kb.sb("cb", [128, 8], F32)
            kb.dma("sp", cb[:], ins["ssd_conv_b"][L, :].rearrange("(c p) -> p c", p=128), reads=[ins["ssd_conv_b"]], writes=[cb],
                   allow_slow_non_contiguous=True)
            dtb = kb.sb("dtb", [128, 16], F32)
            kb.dma("sp", dtb[:], ins["ssd_dt_bias"][L:L + 1, :].broadcast_to([128, 16]), reads=[ins["ssd_dt_bias"]], writes=[dtb])
            negA = kb.sb("negA", [128, 16], F32)
            kb.dma("sp", negA[:], ins["ssd_a_log"][L:L + 1, :, :].rearrange("o a h -> o (a h)").broadcast_to([128, 16]),
                   reads=[ins["ssd_a_log"]], writes=[negA])
            kb.act(negA[:], negA[:], AF.Exp, [negA], [negA])
            kb.ts("dve", negA[:], negA[:], -1.0, None, ALU.mult, None, [negA], [negA])
            raw = kb.sb("raw", [128, NT], BF16)
            acc = kb.sb("acc", [128, NT], F32)
            xT = kb.sb("xT", [128, 4, NT], BF16)
            pp = [kb.ps(f"pp{i}", [128, 512], F32) for i in range(2)]
            pz = [kb.ps(f"pz{i}", [128, 512], F32) for i in range(2)]
            pt = [kb.ps(f"ptt{i}", [128, 768], BF16) for i in range(2)]
            n = 0
            for c in range(8):
                for (t0, tn) in TCH:
                    A = pp[n % 2]
                    n += 1
                    for k in range(8):
                        kb.mm(A[:, :tn], Wc[:, k, c * 128:(c + 1) * 128], hT[:, k, t0:t0 + tn], k == 0, k == 7, [Wc, hT], [A])
                    kb.cp("act", raw[:, t0:t0 + tn], A[:, :tn], [A], [raw])
                kb.ts("dve", acc[:], raw[:], cw[:, c, 1:2], cb[:, c:c + 1], ALU.mult, ALU.add, [raw, cw, cb], [acc])
                for (s0, s1) in segs:
                    kb.stt(acc[:, s0 + 1:s1], raw[:, s0:s1 - 1], cw[:, c, 0:1], acc[:, s0 + 1:s1], ALU.mult, ALU.add, [raw, cw, acc], [acc])
                    kb.stt(acc[:, s0:s1 - 1], raw[:, s0 + 1:s1], cw[:, c, 2:3], acc[:, s0:s1 - 1], ALU.mult, ALU.add, [raw, cw, acc], [acc])
                dst = xT[:, c, :] if c < 4 else bcT[:, c - 4, :]
                kb.act(dst, acc[:], AF.Silu, [acc], [xT if c < 4 else bcT])
            tmp16 = kb.sb("tmp16", [128, 16], F32)
            for i in range(NTILE):
                Z = pz[i % 2]
                tk = slice(i * 128, (i + 1) * 128)
                for k in range(8):
                    kb.mm(Z[:], hT[:, k, tk], Wz[:, k, :], k == 0, k == 7, [hT, Wz], [Z])
                kb.act(zs[:, i, :], Z[:], AF.Silu, [Z], [zs])
                Dp = pp[i % 2]
                for k in range(8):
                    kb.mm(Dp[:, 0:16], hT[:, k, tk], Wdt[:, k, :], k == 0, k == 7, [hT, Wdt], [Dp])
                kb.tt("dve", tmp16[:], Dp[:, 0:16], dtb[:], ALU.add, [Dp, dtb], [tmp16])
                kb.ts("dve", tmp16[:], tmp16[:], 30.0, None, ALU.min, None, [tmp16], [tmp16])
                kb.act(tmp16[:], tmp16[:], AF.Exp, [tmp16], [tmp16])
                kb.act(dt[:, i, :], tmp16[:], AF.Ln, [tmp16], [dt], bias=g.onec[:, 0:1])
                T = pt[i % 2]
                for c in range(6):
                    src = xT[:, c, tk] if c < 4 else bcT[:, c - 4, tk]
                    kb.tr(T[:, c * 128:(c + 1) * 128], src, g.identb[:], [xT if c < 4 else bcT, g.identb], [T])
                kb.cp("dve", xbtok[:, i, :], T[:], [T], [xbtok])
            kb.tt("dve", da[:], dt[:], negA[:].unsqueeze(1).broadcast_to([128, NTILE, 16]), ALU.mult, [dt, negA], [da])
        if g.debug and L == 0 and b == 0:
            dbg_out(g, "ssd_dt", dt, [128, NTILE, 16])
            dbg_out(g, "ssd_xbtok", xbtok, [128, NTILE, 768], BF16)
        import os
        PH = int(os.environ.get("SSD_PHASE", "3"))
        Yacc = kb.sb("Yacc", [128, NTILE, 512], F32)
        if PH < 2:
            return
        with kb.scope():
            selh = kb.sb("selh", [8, 8, 128], F32)
            kb.op("dve", lambda e: e.tensor_copy(out=selh[:], in_=g.identf[0:8, 0:8].unsqueeze(2).broadcast_to([8, 8, 128])),
                  reads=[g.identf], writes=[selh])
            pcs = kb.ps("pcs", [128, 512], F32)
            pR = kb.ps("pR", [128, 1024], F32)
            pCB = kb.ps("pCB", [128, 512], F32)
            pYd = kb.ps("pYd", [128, 512], F32)
            pYo = kb.ps("pYo", [128, 512], F32)
            pSn = kb.ps("pSn", [128, 512], F32)
            ncs = kb.sb("ncs", [128, 8], F32)
            csT = kb.sb("csT", [8, 128], F32)
            Lt = kb.sb("Lt", [128, 8, 128], F32)
            Gt = kb.sb("Gt", [128, 8, 128], BF16)
            ecol = kb.sb("ecol", [128, 8], F32)
            eend = kb.sb("eend", [128, 8], F32)
            wcol = kb.sb("wcol", [128, 8], F32)
            xd = kb.sb("xd", [128, 512], BF16)
            xdd = kb.sb("xdd", [128, 512], BF16)
            tmpy = kb.sb("tmpy", [128, 512], F32)
            S = kb.sb("S", [128, 512], F32)
            Sb = kb.sb("Sb", [128, 512], BF16)
            CUT = int(os.environ.get("SSD_CUT", "0"))
            for d in range(2):
                tri = g.triF if d == 0 else g.triB
                nmask = g.nmF if d == 0 else g.nmB
                e_idx = 127 if d == 0 else 0
                order = list(range(NTILE)) if d == 0 else ([1, 0] + list(range(NTILE - 1, nctx - 1, -1)))
                kb.op("dve", lambda e: e.memset(S[:], 0.0), writes=[S])
                kb.op("pool", lambda e: e.memset(Sb[:], 0.0), writes=[Sb])
                if CUT == 20 and d == 1:
                    return
                for ci in order:
                    if CUT == 21 and d == 1:
                        CUT = int(os.environ.get("SSD_CUT2", "13"))
                    tk = slice(ci * 128, (ci + 1) * 128)
                    dah = da[:, ci, d * 8:(d + 1) * 8]
                    dth = dt[:, ci, d * 8:(d + 1) * 8]
                    xs3 = xbtok[:, ci, 0:512].rearrange("p (h q) -> p h q", h=8)
                    kb.mm(pcs[:, 0:8], tri[:], dah, True, True, [tri, da], [pcs])
                    kb.mm32(pcs[0:8, 128:256], dah, tri[:], True, True, [tri, da], [pcs], pcs[0:32, 511:512], pcs, g.identb)
                    if CUT == 1:
                        dump = kb.sb("dump", [128, 256], F32)
                        kb.cp("dve", dump[:], pcs[:, 0:256], [pcs], [dump])
                        dbg_out(g, "pcs", dump, [128, 256])
                        return
                    kb.ts("dve", ncs[:], pcs[:, 0:8], -1.0, None, ALU.mult, None, [pcs], [ncs])
                    if CUT == 15:
                        return
                    kb.cp("dve", csT[:], pcs[0:8, 128:256], [pcs], [csT])
                    if CUT == 16:
                        return
                    kb.act(ecol[:], ncs[:], AF.Exp, [ncs], [ecol], scale=-1.0)
                    if CUT == 2:
                        return
                    for h in range(8):
                        kb.mm(pR[:, h * 128:(h + 1) * 128], selh[:, h, :], csT[:], True, False, [selh, csT], [pR])
                        kb.mm(pR[:, h * 128:(h + 1) * 128], g.identb[:], nmask[:], False, True, [g.identb, nmask], [pR])
                    if CUT == 3:
                        return
                    for gg in range(2):
                        kb.mm(pCB[:, gg * 128:(gg + 1) * 128], bcT[:, gg, tk], bcT[:, 2 + gg, tk], True, True, [bcT], [pCB])
                    if CUT == 4:
                        return
                    for h in range(8):
                        kb.act(Lt[:, h, :], pR[:, h * 128:(h + 1) * 128], AF.Exp, [pR, ncs], [Lt], bias=ncs[:, h:h + 1])
                    if CUT == 5:
                        return
                    kb.tt("dve", eend[:], Lt[:, :, e_idx], ecol[:], ALU.mult, [Lt, ecol], [eend])
                    if CUT == 6:
                        return
                    for gg in range(2):
                        kb.tt("dve", Gt[:, gg * 4:(gg + 1) * 4, :], Lt[:, gg * 4:(gg + 1) * 4, :],
                              pCB[:, gg * 128:(gg + 1) * 128].unsqueeze(1).broadcast_to([128, 4, 128]), ALU.mult, [Lt, pCB], [Gt])
                    if CUT == 7:
                        return
                    kb.tt("dve", wcol[:], dth, Lt[:, :, e_idx], ALU.mult, [dt, Lt], [wcol])
                    if CUT == 8:
                        return
                    kb.tt("dve", xd[:].rearrange("p (h q) -> p h q", h=8), xs3, dth.unsqueeze(2).broadcast_to([128, 8, 64]), ALU.mult, [xbtok, dt], [xd])
                    if CUT == 9:
                        return
                    kb.tt("pool", xdd[:].rearrange("p (h q) -> p h q", h=8), xs3, wcol[:].unsqueeze(2).broadcast_to([128, 8, 64]), ALU.mult, [xbtok, wcol], [xdd])
                    if CUT == 10:
                        return
                    for h in range(8):
                        kb.mm(pYd[:, h * 64:(h + 1) * 64], Gt[:, h, :], xd[:, h * 64:(h + 1) * 64], True, True, [Gt, xd], [pYd])
                    if CUT == 11:
                        return
                    for h in range(8):
                        kb.mm(pYo[:, h * 64:(h + 1) * 64], bcT[:, 2 + h // 4, tk], Sb[:, h * 64:(h + 1) * 64], True, True, [bcT, Sb], [pYo])
                    if CUT == 12:
                        return
                    for gg in range(2):
                        kb.mm(pSn[:, gg * 256:(gg + 1) * 256], xbtok[:, ci, 512 + gg * 128: 512 + (gg + 1) * 128], xdd[:, gg * 256:(gg + 1) * 256],
                              True, True, [xbtok, xdd], [pSn])
                    if CUT == 13:
                        return
                    kb.tt("dve", tmpy[:].rearrange("p (h q) -> p h q", h=8), pYo[:].rearrange("p (h q) -> p h q", h=8),
                          ecol[:].unsqueeze(2).broadcast_to([128, 8, 64]), ALU.mult, [pYo, ecol], [tmpy])
                    if d == 0:
                        kb.tt("dve", Yacc[:, ci, :], tmpy[:], pYd[:], ALU.add, [tmpy, pYd], [Yacc])
                    else:
                        kb.tt("dve", tmpy[:], tmpy[:], pYd[:], ALU.add, [tmpy, pYd], [tmpy])
                        kb.tt("pool", Yacc[:, ci, :], Yacc[:, ci, :], tmpy[:], ALU.add, [Yacc, tmpy], [Yacc])
                    if CUT == 14:
                        return
                    kb.tt("dve", S[:].rearrange("p (h q) -> p h q", h=8), S[:].rearrange("p (h q) -> p h q", h=8),
                          eend[:].unsqueeze(2).broadcast_to([128, 8, 64]), ALU.mult, [S, eend], [S])
                    kb.tt("dve", S[:], S[:], pSn[:], ALU.add, [S, pSn], [S])
                    kb.cp("act", Sb[:], S[:], [S], [Sb])
                    if CUT == 17:
                        return
                    if CUT == 18 and ci == order[2]:
                        return
                    if CUT == 19 and d == 1 and ci == order[0]:
                        return
        if PH < 3:
            return
        with kb.scope():
            Drep = kb.sb("Drep", [128, 8], F32)
            kb.dma("sp", Drep[:], ins["ssd_d"][L:L + 1, :].broadcast_to([128, 8]), reads=[ins["ssd_d"]], writes=[Drep])
            nwrep = kb.sb("nwrep", [128, 512], F32)
            kb.dma("sp", nwrep[:], ins["ssd_norm_w"][L:L + 1, :].broadcast_to([128, 512]), reads=[ins["ssd_norm_w"]], writes=[nwrep])
            ty = [kb.sb(f"ty{i}", [128, 512], F32) for i in range(2)]
            junk = kb.sb("junk", [128, 512], F32)
            ss = kb.sb("ss", [128, 1], F32)
            yb = [kb.sb(f"yb{i}", [128, 512], BF16) for i in range(2)]
            pt2 = [kb.ps(f"pt2{i}", [128, 512], BF16) for i in range(2)]
            for i in range(NTILE):
                Y, Yb, T = ty[i % 2], yb[i % 2], pt2[i % 2]
                kb.tt("dve", Y[:].rearrange("p (h q) -> p h q", h=8), xbtok[:, i, 0:512].rearrange("p (h q) -> p h q", h=8),
                      Drep[:].unsqueeze(2).broadcast_to([128, 8, 64]), ALU.mult, [xbtok, Drep], [Y])
                kb.tt("dve", Y[:], Y[:], Yacc[:, i, :], ALU.add, [Y, Yacc], [Y])
                kb.tt("dve", Y[:], Y[:], zs[:, i, :], ALU.mult, [Y, zs], [Y])
                kb.act(junk[:], Y[:], AF.Square, [Y], [junk, ss], accum_out=ss[:])
                kb.act(ss[:], ss[:], AF.Sqrt, [ss], [ss], scale=1.0 / 512, bias=g.epsc[:, 0:1])
                kb.op("dve", lambda e: e.reciprocal(out=ss[:], in_=ss[:]), reads=[ss], writes=[ss])
                kb.stt(Yb[:], Y[:], ss[:, 0:1], nwrep[:], ALU.mult, ALU.mult, [Y, ss, nwrep], [Yb])
                for c in range(4):
                    kb.tr(T[:, c * 128:(c + 1) * 128], Yb[:, c * 128:(c + 1) * 128], g.identb[:], [Yb, g.identb], [T])
                kb.cp("act", YsT[:, :, i * 128:(i + 1) * 128], T[:].rearrange("p (c t) -> p c t", c=4), [T], [YsT])


def precast_layer(g, L):
    kb, ins = g.kb, g.ins
    if not hasattr(g, "wbd"):
        g.wbd = {"w_in": kb.dram("wbf_w_in", [D, D_IN], BF16), "w_branch": kb.dram("wbf_w_branch", [4, 512, D], BF16),
                 "w_out": kb.dram("wbf_w_out", [D, D], BF16), "s5_w_glu": kb.dram("wbf_s5_w_glu", [512, 1024], BF16)}
    for c0 in range(0, D_IN, 2048):
        c1 = min(D_IN, c0 + 2048)
        kb.dma("pool", g.wbd["w_in"][:, c0:c1], ins["w_in"][L, :, c0:c1], reads=[ins["w_in"]], writes=[g.wbd["w_in"]])
    for br in range(4):
        kb.dma("pool", g.wbd["w_branch"][br, :, :], ins["w_branch"][L, br, :, :], reads=[ins["w_branch"]], writes=[g.wbd["w_branch"]])
    kb.dma("pool", g.wbd["w_out"][:, :], ins["w_out"][L, :, :], reads=[ins["w_out"]], writes=[g.wbd["w_out"]])
    kb.dma("pool", g.wbd["s5_w_glu"][:, :], ins["s5_w_glu"][L, :, :], reads=[ins["s5_w_glu"]], writes=[g.wbd["s5_w_glu"]])


def run_all(g, stages=None):
    kb, NB = g.kb, g.NB
    stages = stages or ("ssd", "s5", "a", "g", "merge", "moe")
    g.epsc = kb.sb("epsc", [128, 1], F32)
    kb.op("dve", lambda e: e.memset(g.epsc[:], EPS), writes=[g.epsc])
    g.onec = kb.sb("onec", [128, 1], F32)
    kb.op("dve", lambda e: e.memset(g.onec[:], 1.0), writes=[g.onec])
    Xin = g.ins["xin"]
    for L in range(g.DEPTH):
        with kb.scope():
            precast_layer(g, L)
            stage_mod(g, L)
            for b in range(NB):
                with kb.scope():
                    dbg = g.debug and L == 0 and b == 0
                    hT = kb.sb("hT", [128, 8, NT], BF16)
                    stage_ln(g, b, Xin if L == 0 else g.X2, 8, 0, hT)
                    YsT = kb.sb("YsT", [128, 4, NT], BF16)
                    if "ssd" in stages:
                        stage_ssd(g, L, b, hT, YsT)
                        if dbg:
                            dbg_out(g, "YsT", YsT, [128, 4, NT], BF16)
                    Y5T = kb.sb("Y5T", [128, 4, NT], BF16)
                    if "s5" in stages:
                        import os
                        (stage_s5 if os.environ.get("S5_OLD") else stage_s5b)(g, L, b, hT, Y5T)
                        if dbg:
                            dbg_out(g, "Y5T", Y5T, [128, 4, NT], BF16)
                    YTa = kb.sb("YTa", [128, 4, NT], BF16)
                    YTg = kb.sb("YTg", [128, 4, NT], BF16)
                    if "a" in stages:
                        stage_attn(g, L, b, hT, YTa, "a")
                    if "g" in stages:
                        stage_attn(g, L, b, hT, YTg, "g")
                    if dbg and "a" in stages and "g" in stages:
                        dbg_out(g, "YTa", YTa, [128, 4, NT], BF16)
                        dbg_out(g, "YTg", YTg, [128, 4, NT], BF16)
                    if "merge" in stages:
                        stage_merge(g, L, b, hT, [YTa, YsT, YTg, Y5T], Xin if L == 0 else g.X2)
                if "moe" in stages:
                    stage_moe(g, L, b, L == g.DEPTH - 1 and not g.debug)


def s5_params(g, L):
    kb = g.kb
    ins = g.ins
    P = {}
    TWO_PI = 2 * math.pi
    P["BxT"] = kb.sb("BxT", [128, 2, 2, 16, 128], BF16)
    P["Cx"] = kb.sb("Cx", [128, 2, 16, 128], BF16)
    P["rr"] = kb.sb("rr", [128, 2, 16], F32)
    P["c1"] = kb.sb("c1", [128, 2, 16], F32)
    P["s1"] = kb.sb("s1", [128, 2, 16], F32)
    P["th"] = kb.sb("th", [128, 2, 16], F32)
    P["Dc"] = kb.sb("Dc", [128, 4], F32)
    kb.dma("sp", P["Dc"][:], ins["s5_d"][L, :].rearrange("(c p) -> p c", p=128), reads=[ins["s5_d"]], writes=[P["Dc"]],
           allow_slow_non_contiguous=True)
    with kb.scope():
        Are = kb.sb("Are", [128, 2, 16], F32)
        Aim = kb.sb("Aim", [128, 2, 16], F32)
        Ldt = kb.sb("Ldt", [128, 2, 16], F32)
        Bre = kb.sb("Bre", [128, 16, 16], F32)
        Bim = kb.sb("Bim", [128, 16, 16], F32)
        Cf = kb.sb("Cf", [128, 2, 16, 128], F32)
        kb.op("pool", lambda e: e.memset(Cf[:], 0.0), writes=[Cf])
        for gl in range(2):
            ph = slice(gl * 64, (gl + 1) * 64)
            for d in range(2):
                kb.dma("sp", Are[ph, d, :], ins["s5_a_re"][L, d, gl::2, :].rearrange("j n -> n j"), reads=[ins["s5_a_re"]], writes=[Are], allow_slow_non_contiguous=True)
                kb.dma("sp", Aim[ph, d, :], ins["s5_a_im"][L, d, gl::2, :].rearrange("j n -> n j"), reads=[ins["s5_a_im"]], writes=[Aim], allow_slow_non_contiguous=True)
                kb.dma("sp", Ldt[ph, d, :], ins["s5_log_dt"][L, d:d + 1, gl::2].broadcast_to([64, 16]), reads=[ins["s5_log_dt"]], writes=[Ldt], allow_slow_non_contiguous=True)
            kb.dma("sp", Bre[ph, :, :], ins["s5_b_re"][L, gl::2, :, :].rearrange("j n i -> n j i"), reads=[ins["s5_b_re"]], writes=[Bre], allow_slow_non_contiguous=True)
            kb.dma("act", Bim[ph, :, :], ins["s5_b_im"][L, gl::2, :, :].rearrange("j n i -> n j i"), reads=[ins["s5_b_im"]], writes=[Bim], allow_slow_non_contiguous=True)
            for jr in range(4):
                off = 32 * jr + 16 * gl
                for ri, nm in enumerate(("s5_c_re", "s5_c_im")):
                    for jq in range(4):
                        j = jq * 4 + jr
                        kb.dma("sp" if ri == 0 else "act", Cf[ph, ri, j, off:off + 16],
                               ins[nm][L, 2 * j + gl, :, :].rearrange("o n -> n o"), reads=[ins[nm]], writes=[Cf], allow_slow_non_contiguous=True)
        kb.cp("act", P["Cx"][:, 0], Cf[:, 0], [Cf], [P["Cx"]])
        kb.ts("dve", P["Cx"][:, 1], Cf[:, 1], -1.0, None, ALU.mult, None, [Cf], [P["Cx"]])
        dtt = kb.sb("dtt", [128, 2, 16], F32)
        kb.act(dtt[:], Ldt[:], AF.Exp, [Ldt], [dtt])
        kb.ts("dve", Are[:], Are[:], -1e-4, None, ALU.min, None, [Are], [Are])
        xre = kb.sb("xre", [128, 2, 16], F32)
        kb.tt("dve", xre[:], Are[:], dtt[:], ALU.mult, [Are, dtt], [xre])
        kb.tt("dve", P["th"][:], Aim[:], dtt[:], ALU.mult, [Aim, dtt], [P["th"]])
        kb.act(P["rr"][:], xre[:], AF.Exp, [xre], [P["rr"]])
        sc = kb.sb("sc", [128, 2, 2, 16], F32)
        s5_sincos(g, P["th"][:].rearrange("p d j -> p (d j)"), P["s1"][:].rearrange("p d j -> p (d j)"),
                  P["c1"][:].rearrange("p d j -> p (d j)"), [P["th"]], [P["s1"], P["c1"]], 32)
        nr = kb.sb("nr", [128, 2, 16], F32)
        ni = kb.sb("ni", [128, 2, 16], F32)
        kb.tt("dve", nr[:], P["rr"][:], P["c1"][:], ALU.mult, [P["rr"], P["c1"]], [nr])
        kb.ts("dve", nr[:], nr[:], -1.0, None, ALU.add, None, [nr], [nr])
        kb.tt("dve", ni[:], P["rr"][:], P["s1"][:], ALU.mult, [P["rr"], P["s1"]], [ni])
        den = kb.sb("den", [128, 2, 16], F32)
        t_ = kb.sb("t_", [128, 2, 16], F32)
        kb.tt("dve", den[:], Are[:], Are[:], ALU.mult, [Are], [den])
        kb.tt("dve", t_[:], Aim[:], Aim[:], ALU.mult, [Aim], [t_])
        kb.tt("dve", den[:], den[:], t_[:], ALU.add, [den, t_], [den])
        kb.op("dve", lambda e: e.reciprocal(out=den[:], in_=den[:]), reads=[den], writes=[den])
        kre = kb.sb("kre", [128, 2, 16], F32)
        kim = kb.sb("kim", [128, 2, 16], F32)
        kb.tt("dve", kre[:], nr[:], Are[:], ALU.mult, [nr, Are], [kre])
        kb.tt("dve", t_[:], ni[:], Aim[:], ALU.mult, [ni, Aim], [t_])
        kb.tt("dve", kre[:], kre[:], t_[:], ALU.add, [kre, t_], [kre])
        kb.tt("dve", kre[:], kre[:], den[:], ALU.mult, [kre, den], [kre])
        kb.tt("dve", kim[:], ni[:], Are[:], ALU.mult, [ni, Are], [kim])
        kb.tt("dve", t_[:], nr[:], Aim[:], ALU.mult, [nr, Aim], [t_])
        kb.tt("dve", kim[:], kim[:], t_[:], ALU.subtract, [kim, t_], [kim])
        kb.tt("dve", kim[:], kim[:], den[:], ALU.mult, [kim, den], [kim])
        Bx = kb.sb("Bx", [128, 16, 128], F32)
        tb1 = kb.sb("tb1", [128, 16, 16], F32)
        tb2 = kb.sb("tb2", [128, 16, 16], F32)
        ptb = [kb.ps(f"ptb{i}", [128, 512], F32) for i in range(2)]
        n = 0
        for d in range(2):
            for ri in range(2):
                X1, X2 = (Bre, Bim) if ri == 0 else (Bim, Bre)
                kb.tt("dve", tb1[:], X1[:], kre[:, d, :].unsqueeze(2).broadcast_to([128, 16, 16]), ALU.mult, [X1, kre], [tb1])
                kb.tt("dve", tb2[:], X2[:], kim[:, d, :].unsqueeze(2).broadcast_to([128, 16, 16]), ALU.mult, [X2, kim], [tb2])
                kb.tt("dve", tb1[:], tb1[:], tb2[:], ALU.subtract if ri == 0 else ALU.add, [tb1, tb2], [tb1])
                kb.op("pool", lambda e: e.memset(Bx[:], 0.0), writes=[Bx])
                for gl in range(2):
                    ph = slice(gl * 64, (gl + 1) * 64)
                    for jr in range(4):
                        off = 32 * jr + 16 * gl
                        kb.cp("dve", Bx[ph, jr::4, off:off + 16], tb1[ph, jr::4, :], [tb1], [Bx])
                for jq in range(4):
                    T = ptb[n % 2]
                    n += 1
                    for jj in range(4):
                        j = jq * 4 + jj
                        kb.tr(T[:, jj * 128:(jj + 1) * 128], Bx[:, j, :], g.identf[:], [Bx, g.identf], [T])
                    kb.cp("dve", P["BxT"][:, d, ri, jq * 4:(jq + 1) * 4, :], T[:].rearrange("p (a b) -> p a b", a=4), [T], [P["BxT"]])
    g.s5 = P


def s5_sincos(g, ang, s_out, c_out, R, W, n):
    kb = g.kb
    TWO_PI = 2 * math.pi
    with kb.scope():
        kf = kb.sb("kf", [128, n], F32)
        ki = kb.sb("ki", [128, n], I32)
        r = kb.sb("r", [128, n], F32)
        m = kb.sb("m", [128, n], F32)
        kb.ts("dve", kf[:], ang, 1.0 / TWO_PI, None, ALU.mult, None, R, [kf])
        kb.cp("dve", ki[:], kf[:], [kf], [ki])
        kb.cp("dve", kf[:], ki[:], [ki], [kf])
        kb.stt(r[:], kf[:], -TWO_PI, ang, ALU.mult, ALU.add, [kf] + list(R), [r])
        for shift, out in ((0.0, s_out), (math.pi / 2, c_out)):
            src = r
            if shift:
                kb.ts("dve", m[:], r[:], shift, None, ALU.add, None, [r], [m])
                src = m
            for _ in range(2):
                kb.ts("dve", kf[:], src[:], math.pi, -TWO_PI, ALU.is_gt, ALU.mult, [src], [kf])
                kb.tt("dve", m[:], src[:], kf[:], ALU.add, [src, kf], [m])
                src = m
                kb.ts("dve", kf[:], m[:], -math.pi, TWO_PI, ALU.is_lt, ALU.mult, [m], [kf])
                kb.tt("dve", m[:], m[:], kf[:], ALU.add, [m, kf], [m])
            kb.act(out, m[:], AF.Sin, [m], W)


def stage_s5(g, L, b, hT, Y5T):
    kb, NB = g.kb, g.NB
    ins = g.ins
    w_in = g.wbd["w_in"]
    T = 256
    import os
    CUT = int(os.environ.get("S5_CUT", "0"))
    chunks = [(0, CTX)] + [(CTX + i * T, T) for i in range(SEQ // T)]
    with kb.scope():
        s5_params(g, L)
        P = g.s5
        uT = Y5T
        yacc = kb.sb("yacc", [128, 4, NT], F32)
        with kb.scope():
            Wu = kb.sb("Wu", [128, 8, 512], BF16)
            kb.dma("sp", Wu[:], w_in[:, 3088:3600].rearrange("(k p) c -> p k c", p=128), reads=[w_in], writes=[Wu])
            pu = [kb.ps(f"pu{i}", [128, 512], F32) for i in range(2)]
            n = 0
            for c in range(4):
                for (t0, tn) in TCH:
                    A = pu[n % 2]
                    n += 1
                    for k in range(8):
                        kb.mm(A[:, :tn], Wu[:, k, c * 128:(c + 1) * 128], hT[:, k, t0:t0 + tn], k == 0, k == 7, [Wu, hT], [A])
                    kb.cp("dve", uT[:, c, t0:t0 + tn], A[:, :tn], [A], [uT])
                    kb.ts("dve", yacc[:, c, t0:t0 + tn], A[:, :tn], P["Dc"][:, c:c + 1], None, ALU.mult, None, [A, P["Dc"]], [yacc])
        if CUT == 2:
            return
        with kb.scope():
            iot = kb.sb("iot", [128, T], F32)
            kb.op("pool", lambda e: e.iota(iot[:], pattern=[[1, T]], base=0, channel_multiplier=0, allow_small_or_imprecise_dtypes=True), writes=[iot])
            ang = kb.sb("ang", [128, T], F32)
            ctab = kb.sb("ctab", [128, T], F32)
            stab = kb.sb("stab", [128, T], F32)
            rrow = kb.sb("rrow", [128, T], F32)
            pb = [kb.ps(f"pbu{i}", [128, 512], F32) for i in range(4)]
            py = [kb.ps(f"py{i}", [128, 512], F32) for i in range(2)]
            v2 = [[kb.sb(f"v{i}_{q}", [128, T], F32) for i in range(2)] for q in range(2)]
            tmp2 = [[kb.sb(f"tmp{i}_{q}", [128, T], F32) for i in range(4)] for q in range(2)]
            sh2 = [[kb.sb(f"sh{i}_{q}", [128, T], F32) for i in range(2)] for q in range(2)]
            sf2 = [[kb.sb(f"sf{i}_{q}", [128, T], F32) for i in range(2)] for q in range(2)]
            ET = kb.sb("ET", [128, 2], F32)
            et = kb.sb("et", [128, 4], F32)
            sbf = [[kb.sb(f"sbf{i}_{k}", [128, T], BF16) for i in range(2)] for k in range(2)]
            init = kb.sb("init", [128, 2], F32)
            it = kb.sb("it", [128, 2], F32)
            n = 0
            for d in range(2):
                for j in range(16):
                    kc = j // 4
                    thc = P["th"][:, d, j:j + 1]
                    kb.ts("dve", ang[:], iot[:], thc, None, ALU.mult, None, [iot, P["th"]], [ang])
                    s5_sincos(g, ang[:], stab[:], ctab[:], [ang], [stab, ctab], T)
                    kb.ts("dve", rrow[:], iot[:], 0.0, P["rr"][:, d, j:j + 1], ALU.mult, ALU.add, [iot, P["rr"]], [rrow])
                    kb.op("dve", lambda e: e.memset(init[:], 0.0), writes=[init])
                    c1c, s1c = P["c1"][:, d, j:j + 1], P["s1"][:, d, j:j + 1]
                    kb.ts("dve", et[:, 0:1], ctab[:, T - 1:T], c1c, None, ALU.mult, None, [ctab, P["c1"]], [et])
                    kb.ts("dve", et[:, 1:2], stab[:, T - 1:T], s1c, None, ALU.mult, None, [stab, P["s1"]], [et])
                    kb.ts("dve", et[:, 2:3], stab[:, T - 1:T], c1c, None, ALU.mult, None, [stab, P["c1"]], [et])
                    kb.ts("dve", et[:, 3:4], ctab[:, T - 1:T], s1c, None, ALU.mult, None, [ctab, P["s1"]], [et])
                    kb.tt("dve", ET[:, 0:1], et[:, 0:1], et[:, 1:2], ALU.subtract, [et], [ET])
                    kb.tt("dve", ET[:, 1:2], et[:, 2:3], et[:, 3:4], ALU.add, [et], [ET])
                    if CUT == 3:
                        return
                    order = chunks if d == 0 else [chunks[0]] + chunks[:0:-1]
                    for ci, (t0, tn) in enumerate(order):
                        def rv(ap_full):
                            a = ap_full[:, 0:tn]
                            return a if d == 0 else a[:, ::-1]
                        assert tn == T
                        v, tmp, sh, sf = v2[n % 2], tmp2[n % 2], sh2[n % 2], sf2[n % 2]
                        Bre_p, Bim_p = pb[(2 * n) % 4], pb[(2 * n + 1) % 4]
                        Y = py[n % 2]
                        SB = sbf[n % 2]
                        n += 1
                        kb.mm(Bre_p[:, :tn], P["BxT"][:, d, 0, j, :], uT[:, kc, t0:t0 + tn], True, True, [P["BxT"], uT], [Bre_p])
                        kb.mm(Bim_p[:, :tn], P["BxT"][:, d, 1, j, :], uT[:, kc, t0:t0 + tn], True, True, [P["BxT"], uT], [Bim_p])
                        c_, s_ = ctab[:, :tn], stab[:, :tn]
                        kb.tt("dve", tmp[0][:, :tn], rv(Bre_p), c_, ALU.mult, [Bre_p, ctab], [tmp[0]])
                        kb.tt("dve", tmp[1][:, :tn], rv(Bim_p), s_, ALU.mult, [Bim_p, stab], [tmp[1]])
                        kb.tt("dve", tmp[2][:, :tn], rv(Bim_p), c_, ALU.mult, [Bim_p, ctab], [tmp[2]])
                        kb.tt("dve", tmp[3][:, :tn], rv(Bre_p), s_, ALU.mult, [Bre_p, stab], [tmp[3]])
                        kb.tt("pool", v[0][:, :tn], tmp[0][:, :tn], tmp[1][:, :tn], ALU.add, [tmp[0], tmp[1]], [v[0]])
                        kb.tt("pool", v[1][:, :tn], tmp[2][:, :tn], tmp[3][:, :tn], ALU.subtract, [tmp[2], tmp[3]], [v[1]])
                        for ri in range(2):
                            kb.op("dve", lambda e: e.tensor_tensor_scan(out=sh[ri][:, :tn], data0=rrow[:, :tn], data1=v[ri][:, :tn],
                                                                          initial=init[:, ri:ri + 1], op0=ALU.mult, op1=ALU.add),
                                  reads=[rrow, v[ri], init], writes=[sh[ri]])
                        kb.ts("dve", it[:, 0:1], sh[0][:, tn - 1:tn], ET[:, 0:1], None, ALU.mult, None, [sh[0], ET], [it])
                        kb.ts("dve", it[:, 1:2], sh[0][:, tn - 1:tn], ET[:, 1:2], None, ALU.mult, None, [sh[0], ET], [it])
                        kb.stt(init[:, 0:1], sh[1][:, tn - 1:tn], ET[:, 1:2], it[:, 0:1], ALU.mult, ALU.subtract, [sh[1], ET, it], [init])
                        kb.ts("dve", init[:, 0:1], init[:, 0:1], -1.0, None, ALU.mult, None, [init], [init])
                        kb.stt(init[:, 1:2], sh[1][:, tn - 1:tn], ET[:, 0:1], it[:, 1:2], ALU.mult, ALU.add, [sh[1], ET, it], [init])
                        kb.tt("pool", tmp[0][:, :tn], sh[0][:, :tn], c_, ALU.mult, [sh[0], ctab], [tmp[0]])
                        kb.tt("pool", tmp[1][:, :tn], sh[1][:, :tn], s_, ALU.mult, [sh[1], stab], [tmp[1]])
                        kb.tt("pool", tmp[2][:, :tn], sh[0][:, :tn], s_, ALU.mult, [sh[0], stab], [tmp[2]])
                        kb.tt("pool", tmp[3][:, :tn], sh[1][:, :tn], c_, ALU.mult, [sh[1], ctab], [tmp[3]])
                        kb.tt("dve", sf[0][:, :tn], tmp[0][:, :tn], tmp[1][:, :tn], ALU.subtract, [tmp[0], tmp[1]], [sf[0]])
                        kb.tt("dve", sf[1][:, :tn], tmp[2][:, :tn], tmp[3][:, :tn], ALU.add, [tmp[2], tmp[3]], [sf[1]])
                        for ri in range(2):
                            dstv = SB[ri][:, :tn] if d == 0 else SB[ri][:, :tn][:, ::-1]
                            kb.cp("act", dstv, sf[ri][:, :tn], [sf[ri]], [SB[ri]])
                        kb.mm(Y[:, :tn], P["Cx"][:, 0, j, :], SB[0][:, :tn], True, False, [P["Cx"], SB[0]], [Y])
                        kb.mm(Y[:, :tn], P["Cx"][:, 1, j, :], SB[1][:, :tn], False, True, [P["Cx"], SB[1]], [Y])
                        kb.tt("dve", yacc[:, kc, t0:t0 + tn], yacc[:, kc, t0:t0 + tn], Y[:, :tn], ALU.add, [yacc, Y], [yacc])
                        if CUT == 4:
                            return
                    if CUT == 5:
                        return
                if CUT == 6:
                    return
        if g.debug and L == 0 and b == 0:
            dbg_out(g, "s5_yacc", yacc, [128, 4, NT])
        if CUT == 7:
            return
        with kb.scope():
            Wg = kb.sb("Wg", [128, 4, 1024], BF16)
            kb.dma("sp", Wg[:], g.wbd["s5_w_glu"][:, :].rearrange("(k p) c -> p k c", p=128), reads=[g.wbd["s5_w_glu"]], writes=[Wg])
            gy = kb.sb("gy", [128, 4, NT], BF16)
            x2 = kb.sb("x2", [128, NT], F32)
            for c in range(4):
                x = yacc[:, c, :]
                kb.act(x2[:], x, AF.Square, [yacc], [x2])
                kb.ts("dve", x2[:], x2[:], 0.044715 * math.sqrt(2 / math.pi), math.sqrt(2 / math.pi), ALU.mult, ALU.add, [x2], [x2])
                kb.tt("dve", x2[:], x2[:], x, ALU.mult, [x2, yacc], [x2])
                kb.act(x2[:], x2[:], AF.Tanh, [x2], [x2])
                kb.ts("dve", x2[:], x2[:], 1.0, 0.5, ALU.add, ALU.mult, [x2], [x2])
                kb.tt("dve", gy[:, c, :], x2[:], x, ALU.mult, [x2, yacc], [gy])
            pa = [kb.ps(f"pa{i}", [128, 512], F32) for i in range(2)]
            pbb = [kb.ps(f"pbb{i}", [128, 512], F32) for i in range(2)]
            sg = [kb.sb(f"sg{i}", [128, 512], F32) for i in range(2)]
            n = 0
            for c in range(4):
                for (t0, tn) in TCH:
                    A, B, SG = pa[n % 2], pbb[n % 2], sg[n % 2]
                    n += 1
                    for k in range(4):
                        kb.mm(A[:, :tn], Wg[:, k, c * 128:(c + 1) * 128], gy[:, k, t0:t0 + tn], k == 0, k == 3, [Wg, gy], [A])
                    for k in range(4):
                        kb.mm(B[:, :tn], Wg[:, k, 512 + c * 128:512 + (c + 1) * 128], gy[:, k, t0:t0 + tn], k == 0, k == 3, [Wg, gy], [B])
                    kb.act(SG[:, :tn], B[:, :tn], AF.Sigmoid, [B], [SG])
                    kb.tt("dve", Y5T[:, c, t0:t0 + tn], A[:, :tn], SG[:, :tn], ALU.mult, [A, SG], [Y5T])


def load_lnrep(g, L, names):
    kb, ins = g.kb, g.ins
    g.lnrep = {}
    for nm in names:
        t = kb.sb(nm + "rep", [128, D], F32)
        kb.dma("sp", t[:], ins[nm][L:L + 1, :].broadcast_to([128, D]), reads=[ins[nm]], writes=[t])
        g.lnrep[nm] = t


def post_norm_tile(g, pool, mix_ap, mixT, xt, grep, gname, bname, outT, out_ap):
    kb = g.kb
    t, st, mv, rs = pool["t"], pool["st"], pool["mv"], pool["rs"]
    kb.tt("dve", t[:], mix_ap, grep[:], ALU.mult, [mixT, grep], [t])
    kb.stt(t[:], xt[:], float(g.ALPHA), t[:], ALU.mult, ALU.add, [xt, t], [t])
    for hf in range(2):
        kb.op("dve", lambda e: e.bn_stats(out=st[:, hf, :], in_=t[:, hf * 512:(hf + 1) * 512]), reads=[t], writes=[st])
    kb.op("dve", lambda e: e.bn_aggr(out=mv[:], in_=st[:].rearrange("p a b -> p (a b)")), reads=[st], writes=[mv])
    kb.act(rs[:], mv[:, 1:2], AF.Sqrt, [mv], [rs], bias=g.epsc[:, 0:1])
    kb.op("dve", lambda e: e.reciprocal(out=rs[:], in_=rs[:]), reads=[rs], writes=[rs])
    kb.ts("dve", t[:], t[:], mv[:, 0:1], rs[:, 0:1], ALU.subtract, ALU.mult, [t, mv, rs], [t])
    kb.tt("pool", t[:], t[:], g.lnrep[gname][:], ALU.mult, [t, g.lnrep[gname]], [t])
    kb.tt("pool", t[:], t[:], g.lnrep[bname][:], ALU.add, [t, g.lnrep[bname]], [t])
    kb.dma("sp", out_ap, t[:], reads=[t], writes=[outT])


def stage_merge(g, L, b, hT, Ys, Xsrc):
    kb, NB = g.kb, g.NB
    ins = g.ins
    w_in = g.wbd["w_in"]
    nctx = CTX // 128
    with kb.scope():
        zT = kb.sb("zT", [128, 8, NT], BF16)
        with kb.scope():
            Wg = [kb.sb(f"Wgm{i}", [128, 8, 4, 128], BF16) for i in range(2)]
            Wb = [kb.sb(f"Wbm{i}", [128, 4, 4, 128], BF16) for i in range(2)]
            zf = kb.sb("zf", [128, 512], F32)
            sg = [kb.sb(f"msg{i}", [128, 512], F32) for i in range(2)]
            pG = [kb.ps(f"pG{i}", [128, 512], F32) for i in range(2)]
            pM = [kb.ps(f"pM{i}", [128, 512], F32) for i in range(2)]
            n = 0
            for fc in range(8):
                WG, WB = Wg[fc % 2], Wb[fc % 2]
                for br in range(4):
                    c0 = 3600 + br * 1024 + fc * 128
                    kb.dma("sp", WG[:, :, br, :], w_in[:, c0:c0 + 128].rearrange("(k p) c -> p k c", p=128), reads=[w_in], writes=[WG])
                    wsrc = g.wbd["w_branch"]
                    if br in (0, 2):
                        for i in range(4):
                            for j in range(2):
                                h = 4 * j + i
                                kb.dma("sp", WB[j * 64:(j + 1) * 64, br, i, :], wsrc[br, h * 64:(h + 1) * 64, fc * 128:(fc + 1) * 128],
                                       reads=[wsrc], writes=[WB])
                    else:
                        kb.dma("sp", WB[:, br, :, :], wsrc[br, :, fc * 128:(fc + 1) * 128].rearrange("(k p) c -> p k c", p=128),
                               reads=[wsrc], writes=[WB])
                for (t0, tn) in TCH:
                    for br in range(4):
                        G_, M_, S_ = pG[n % 2], pM[n % 2], sg[n % 2]
                        n += 1
                        for k in range(8):
                            kb.mm(G_[:, :tn], WG[:, k, br, :], hT[:, k, t0:t0 + tn], k == 0, k == 7, [WG, hT], [G_])
                        for k in range(4):
                            kb.mm(M_[:, :tn], WB[:, br, k, :], Ys[br][:, k, t0:t0 + tn], k == 0, k == 3, [WB, Ys[br]], [M_])
                        kb.act(S_[:, :tn], G_[:, :tn], AF.Sigmoid, [G_], [S_])
                        if br == 0:
                            kb.tt("dve", zf[:, :tn], M_[:, :tn], S_[:, :tn], ALU.mult, [M_, S_], [zf])
                        else:
                            kb.tt("dve", S_[:, :tn], M_[:, :tn], S_[:, :tn], ALU.mult, [M_, S_], [S_])
                            if br < 3:
                                kb.tt("pool", zf[:, :tn], zf[:, :tn], S_[:, :tn], ALU.add, [zf, S_], [zf])
                            else:
                                kb.tt("pool", zT[:, fc, t0:t0 + tn], zf[:, :tn], S_[:, :tn], ALU.add, [zf, S_], [zT])
        with kb.scope():
            load_lnrep(g, L, ("ln1_g", "ln1_b"))
            Wo = kb.sb("Wo", [128, 8, D], BF16)
            for hf in range(2):
                kb.dma("sp", Wo[:, :, hf * 512:(hf + 1) * 512], g.wbd["w_out"][:, hf * 512:(hf + 1) * 512].rearrange("(k p) c -> p k c", p=128),
                       reads=[g.wbd["w_out"]], writes=[Wo])
            pX = [kb.ps(f"pX{i}", [128, D], F32) for i in range(2)]
            xts = [kb.sb(f"mxt{i}", [128, D], F32) for i in range(2)]
            pool = {"t": kb.sb("pn_t", [128, D], F32), "st": kb.sb("pn_st", [128, 2, 6], F32),
                    "mv": kb.sb("pn_mv", [128, 2], F32), "rs": kb.sb("pn_rs", [128, 1], F32)}
            for i in range(NTILE):
                PX, xt = pX[i % 2], xts[i % 2]
                kb.dma("act", xt[:], Xsrc[b, i * 128:(i + 1) * 128, :], reads=[Xsrc], writes=[xt])
                for hf in range(2):
                    for k in range(8):
                        kb.mm(PX[:, hf * 512:(hf + 1) * 512], zT[:, k, i * 128:(i + 1) * 128], Wo[:, k, hf * 512:(hf + 1) * 512], k == 0, k == 7, [zT, Wo], [PX])
                grep = g.greps[NB if i < nctx else b][0]
                post_norm_tile(g, pool, PX[:], PX, xt, grep, "ln1_g", "ln1_b", g.X1, g.X1[b, i * 128:(i + 1) * 128, :])


def stage_moe(g, L, b, last):
    kb, NB = g.kb, g.NB
    ins = g.ins
    nctx = CTX // 128
    t_start = CTX if last else 0
    tiles = list(range(t_start // 128, NTILE))
    chunks = [(t0, min(512, NT - t0)) for t0 in range(t_start, NT, 512)]
    with kb.scope():
        h2T = kb.sb("h2T", [128, 8, NT], BF16)
        stage_ln(g, b, g.X1, 32, 24, h2T)
        Yacc = kb.sb("Yacc2", [128, NTILE, D], F32)
        Wt = kb.sb("Wt", [128, NTILE, 32], F32)
        with kb.scope():
            rw = kb.sb("rw", [128, 8, 32], BF16)
            kb.dma("pool", rw[:], ins["router_w"][:, :].rearrange("(k p) c -> p k c", p=128), reads=[ins["router_w"]], writes=[rw])
            rb = kb.sb("rb", [128, 32], F32)
            kb.dma("sp", rb[:], ins["router_bias"][:].rearrange("(o c) -> o c", o=1).broadcast_to([128, 32]), reads=[ins["router_bias"]], writes=[rb])
            pr_ = [kb.ps(f"prt{i}", [128, 512], F32) for i in range(2)]
            sc = kb.sb("sc", [128, 32], F32)
            sel = kb.sb("sel_", [128, 32], F32)
            eq = kb.sb("eq", [128, 32], F32)
            m1 = kb.sb("m1", [128, 8], F32)
            m2 = kb.sb("m2", [128, 8], F32)
            gs = kb.sb("gs", [128, 8], F32)
            gm = kb.sb("gm", [128, 1], F32)
            v3 = lambda t: t[:].rearrange("p (a b) -> p a b", a=8)
            bc = lambda t: t[:].unsqueeze(2).broadcast_to([128, 8, 4])
            for i in tiles:
                Pp = pr_[i % 2]
                for k in range(8):
                    kb.mm(Pp[:, 0:32], h2T[:, k, i * 128:(i + 1) * 128], rw[:, k, :], k == 0, k == 7, [h2T, rw], [Pp])
                kb.act(sc[:], Pp[:, 0:32], AF.Sigmoid, [Pp], [sc])
                kb.tt("dve", sel[:], sc[:], rb[:], ALU.add, [sc, rb], [sel])
                kb.op("dve", lambda e: e.tensor_reduce(out=m1[:], in_=v3(sel), axis=AX.X, op=ALU.max), reads=[sel], writes=[m1])
                kb.tt("dve", v3(eq), v3(sel), bc(m1), ALU.is_equal, [sel, m1], [eq])
                kb.stt(eq[:], eq[:], -1000.0, sel[:], ALU.mult, ALU.add, [eq, sel], [eq])
                kb.op("dve", lambda e: e.tensor_reduce(out=m2[:], in_=v3(eq), axis=AX.X, op=ALU.max), reads=[eq], writes=[m2])
                kb.tt("dve", gs[:], m1[:], m2[:], ALU.add, [m1, m2], [gs])
                kb.op("dve", lambda e: e.tensor_reduce(out=gm[:], in_=gs[:], axis=AX.X, op=ALU.max), reads=[gs], writes=[gm])
                kb.ts("dve", gs[:], gs[:], gm[:, 0:1], None, ALU.is_equal, None, [gs, gm], [gs])
                kb.tt("dve", v3(eq), v3(sel), bc(m2), ALU.is_ge, [sel, m2], [eq])
                kb.tt("dve", v3(eq), v3(eq), bc(gs), ALU.mult, [eq, gs], [eq])
                kb.tt("dve", eq[:], eq[:], sc[:], ALU.mult, [eq, sc], [eq])
                kb.op("dve", lambda e: e.tensor_reduce(out=gm[:], in_=eq[:], axis=AX.X, op=ALU.add), reads=[eq], writes=[gm])
                kb.op("dve", lambda e: e.reciprocal(out=gm[:], in_=gm[:]), reads=[gm], writes=[gm])
                kb.ts("dve", Wt[:, i, :], eq[:], gm[:, 0:1], None, ALU.mult, None, [eq, gm], [Wt])
        if g.debug and L == 0 and b == 0:
            dbg_out(g, "Wt", Wt, [128, NTILE, 32])
        with kb.scope():
            Wgu = [kb.sb(f"Wgu{i}", [128, 2, 8, 512], BF16) for i in range(2)]
            Wd = [kb.sb(f"Wd{i}", [128, 4, D], BF16) for i in range(2)]
            AT = [kb.sb(f"AT{i}", [128, 4, 512], BF16) for i in range(2)]
            sgm = [kb.sb(f"sgm{i}", [128, 512], BF16) for i in range(2)]
            pGm = [kb.ps(f"pGm{i}", [128, 512], F32) for i in range(2)]
            pUm = [kb.ps(f"pUm{i}", [128, 512], F32) for i in range(2)]
            pY = [kb.ps(f"pY{i}", [128, D], F32) for i in range(2)]
            n = 0
            ny = 0
            na = 0
            for e_ in range(32):
                WGU, WD = Wgu[e_ % 2], Wd[e_ % 2]
                kb.dma("pool", WGU[:, 0], ins["moe_w_gate"][L, e_, :, :].rearrange("(k p) c -> p k c", p=128), reads=[ins["moe_w_gate"]], writes=[WGU])
                kb.dma("pool", WGU[:, 1], ins["moe_w_up"][L, e_, :, :].rearrange("(k p) c -> p k c", p=128), reads=[ins["moe_w_up"]], writes=[WGU])
                for hf in range(2):
                    kb.dma("pool", WD[:, :, hf * 512:(hf + 1) * 512], ins["moe_w_down"][L, e_, :, hf * 512:(hf + 1) * 512].rearrange("(k p) c -> p k c", p=128),
                           reads=[ins["moe_w_down"]], writes=[WD])
                for (t0, tn) in chunks:
                    A_ = AT[na % 2]
                    na += 1
                    for fcx in range(4):
                        G_, U_, S_ = pGm[n % 2], pUm[n % 2], sgm[n % 2]
                        n += 1
                        for k in range(8):
                            kb.mm(G_[:, :tn], WGU[:, 0, k, fcx * 128:(fcx + 1) * 128], h2T[:, k, t0:t0 + tn], k == 0, k == 7, [WGU, h2T], [G_])
                        for k in range(8):
                            kb.mm(U_[:, :tn], WGU[:, 1, k, fcx * 128:(fcx + 1) * 128], h2T[:, k, t0:t0 + tn], k == 0, k == 7, [WGU, h2T], [U_])
                        kb.act(S_[:, :tn], G_[:, :tn], AF.Silu, [G_], [S_])
                        kb.tt("dve", A_[:, fcx, :tn], U_[:, :tn], S_[:, :tn], ALU.mult, [U_, S_], [A_])
                    for ti in range(tn // 128):
                        i = t0 // 128 + ti
                        Y_ = pY[ny % 2]
                        ny += 1
                        for hf in range(2):
                            for k in range(4):
                                kb.mm(Y_[:, hf * 512:(hf + 1) * 512], A_[:, k, ti * 128:(ti + 1) * 128], WD[:, k, hf * 512:(hf + 1) * 512], k == 0, k == 3, [A_, WD], [Y_])
                        if e_ == 0:
                            kb.ts("dve", Yacc[:, i, :], Y_[:], Wt[:, i, e_:e_ + 1], None, ALU.mult, None, [Y_, Wt], [Yacc])
                        else:
                            kb.stt(Yacc[:, i, :], Y_[:], Wt[:, i, e_:e_ + 1], Yacc[:, i, :], ALU.mult, ALU.add, [Y_, Wt, Yacc], [Yacc])
        if g.debug and L == 0 and b == 0:
            dbg_out(g, "ffn", Yacc, [128, NTILE, D])
        with kb.scope():
            load_lnrep(g, L, ("ln2_g", "ln2_b"))
            xts = [kb.sb(f"fxt{i}", [128, D], F32) for i in range(2)]
            pool = {"t": kb.sb("pn2_t", [128, D], F32), "st": kb.sb("pn2_st", [128, 2, 6], F32),
                    "mv": kb.sb("pn2_mv", [128, 2], F32), "rs": kb.sb("pn2_rs", [128, 1], F32)}
            for i in tiles:
                xt = xts[i % 2]
                kb.dma("act", xt[:], g.X1[b, i * 128:(i + 1) * 128, :], reads=[g.X1], writes=[xt])
                grep = g.greps[NB if i < nctx else b][1]
                if last:
                    oT, oap = g.out, g.out[b, (i - nctx) * 128:(i - nctx + 1) * 128, :]
                else:
                    oT, oap = g.X2, g.X2[b, i * 128:(i + 1) * 128, :]
                post_norm_tile(g, pool, Yacc[:, i, :], Yacc, xt, grep, "ln2_g", "ln2_b", oT, oap)


def stage_s5b(g, L, b, hT, Y5T):
    kb, NB = g.kb, g.NB
    ins = g.ins
    w_in = g.wbd["w_in"]
    TS = 4
    TB = 256
    segs = [(0, CTX // TS)] + [(CTX + c * TB * TS, TB) for c in range(SEQ // (TB * TS))]
    with kb.scope():
        uT = Y5T
        yacc = kb.sb("yacc", [128, 4, NT], F32)
        kb.op("pool", lambda e: e.memset(yacc[:], 0.0), writes=[yacc])
        with kb.scope():
            Wu = kb.sb("Wu", [128, 8, 512], BF16)
            kb.dma("sp", Wu[:], w_in[:, 3088:3600].rearrange("(k p) c -> p k c", p=128), reads=[w_in], writes=[Wu])
            pu = [kb.ps(f"pu{i}", [128, 512], F32) for i in range(2)]
            n = 0
            for c in range(4):
                for (t0, tn) in TCH:
                    A = pu[n % 2]
                    n += 1
                    for k in range(8):
                        kb.mm(A[:, :tn], Wu[:, k, c * 128:(c + 1) * 128], hT[:, k, t0:t0 + tn], k == 0, k == 7, [Wu, hT], [A])
                    kb.cp("act" if n % 2 else "dve", uT[:, c, t0:t0 + tn], A[:, :tn], [A], [uT])
        Bre = kb.sb("Bre", [128, 16, 16], F32)
        Bim = kb.sb("Bim", [128, 16, 16], F32)
        Cf = kb.sb("Cf", [128, 2, 16, 128], BF16)
        Dc = kb.sb("Dc", [128, 4], F32)
        apr = kb.sb("apr", [128, 2, 16, 5], F32)
        api = kb.sb("api", [128, 2, 16, 5], F32)
        ck5 = kb.sb("ck5", [128, 2, 16, 5], F32)
        sk5 = kb.sb("sk5", [128, 2, 16, 5], F32)
        rk5 = kb.sb("rk5", [128, 2, 16, 5], F32)
        ang5 = kb.sb("ang5", [128, 2, 16, 5], F32)
        qre = kb.sb("qre", [128, 2, 16, 4], F32)
        qim = kb.sb("qim", [128, 2, 16, 4], F32)
        Kacc = kb.sb("Kacc", [128, 4, 7, 128], F32)
        kb.dma("sp", Dc[:], ins["s5_d"][L, :].rearrange("(c p) -> p c", p=128), reads=[ins["s5_d"]], writes=[Dc], allow_slow_non_contiguous=True)
        fl = lambda t: t[:].rearrange("p d j k -> p (d j k)")
        with kb.scope():
            Are = kb.sb("Are", [128, 2, 16], F32)
            Aim = kb.sb("Aim", [128, 2, 16], F32)
            Ldt = kb.sb("Ldt", [128, 2, 16], F32)
            Cff = kb.sb("Cff", [128, 2, 16, 128], F32)
            kb.op("pool", lambda e: e.memset(Cff[:], 0.0), writes=[Cff])
            for gl in range(2):
                ph = slice(gl * 64, (gl + 1) * 64)
                for d in range(2):
                    kb.dma("sp", Are[ph, d, :], ins["s5_a_re"][L, d, gl::2, :].rearrange("j n -> n j"), reads=[ins["s5_a_re"]], writes=[Are], allow_slow_non_contiguous=True)
                    kb.dma("sp", Aim[ph, d, :], ins["s5_a_im"][L, d, gl::2, :].rearrange("j n -> n j"), reads=[ins["s5_a_im"]], writes=[Aim], allow_slow_non_contiguous=True)
                    kb.dma("sp", Ldt[ph, d, :], ins["s5_log_dt"][L, d:d + 1, gl::2].broadcast_to([64, 16]), reads=[ins["s5_log_dt"]], writes=[Ldt], allow_slow_non_contiguous=True)
                kb.dma("sp", Bre[ph, :, :], ins["s5_b_re"][L, gl::2, :, :].rearrange("j n i -> n j i"), reads=[ins["s5_b_re"]], writes=[Bre], allow_slow_non_contiguous=True)
                kb.dma("act", Bim[ph, :, :], ins["s5_b_im"][L, gl::2, :, :].rearrange("j n i -> n j i"), reads=[ins["s5_b_im"]], writes=[Bim], allow_slow_non_contiguous=True)
                for jr in range(4):
                    off = 32 * jr + 16 * gl
                    for ri, nm in enumerate(("s5_c_re", "s5_c_im")):
                        for jq in range(4):
                            j = jq * 4 + jr
                            kb.dma("sp" if ri == 0 else "act", Cff[ph, ri, j, off:off + 16],
                                   ins[nm][L, 2 * j + gl, :, :].rearrange("o n -> n o"), reads=[ins[nm]], writes=[Cff], allow_slow_non_contiguous=True)
            kb.cp("act", Cf[:], Cff[:], [Cff], [Cf])
            dtt = kb.sb("dtt", [128, 2, 16], F32)
            kb.act(dtt[:], Ldt[:], AF.Exp, [Ldt], [dtt])
            kb.ts("dve", Are[:], Are[:], -1e-4, None, ALU.min, None, [Are], [Are])
            xre = kb.sb("xre", [128, 2, 16], F32)
            th = kb.sb("th", [128, 2, 16], F32)
            kb.tt("dve", xre[:], Are[:], dtt[:], ALU.mult, [Are, dtt], [xre])
            kb.tt("dve", th[:], Aim[:], dtt[:], ALU.mult, [Aim, dtt], [th])
            for k in range(5):
                kb.ts("dve", ang5[:, :, :, k], th[:], float(k), None, ALU.mult, None, [th], [ang5])
                kb.act(rk5[:, :, :, k], xre[:], AF.Exp, [xre], [rk5], scale=float(k))
            s5_sincos(g, fl(ang5), fl(sk5), fl(ck5), [ang5], [sk5, ck5], 160)
            kb.tt("dve", apr[:], rk5[:], ck5[:], ALU.mult, [rk5, ck5], [apr])
            kb.tt("dve", api[:], rk5[:], sk5[:], ALU.mult, [rk5, sk5], [api])
            nr = kb.sb("nr", [128, 2, 16], F32)
            ni = kb.sb("ni", [128, 2, 16], F32)
            den = kb.sb("den", [128, 2, 16], F32)
            t_ = kb.sb("t_", [128, 2, 16], F32)
            kre = kb.sb("kre", [128, 2, 16], F32)
            kim = kb.sb("kim", [128, 2, 16], F32)
            kb.ts("dve", nr[:], apr[:, :, :, 1], -1.0, None, ALU.add, None, [apr], [nr])
            kb.cp("dve", ni[:], api[:, :, :, 1], [api], [ni])
            kb.tt("dve", den[:], Are[:], Are[:], ALU.mult, [Are], [den])
            kb.tt("dve", t_[:], Aim[:], Aim[:], ALU.mult, [Aim], [t_])
            kb.tt("dve", den[:], den[:], t_[:], ALU.add, [den, t_], [den])
            kb.op("dve", lambda e: e.reciprocal(out=den[:], in_=den[:]), reads=[den], writes=[den])
            kb.tt("dve", kre[:], nr[:], Are[:], ALU.mult, [nr, Are], [kre])
            kb.tt("dve", t_[:], ni[:], Aim[:], ALU.mult, [ni, Aim], [t_])
            kb.tt("dve", kre[:], kre[:], t_[:], ALU.add, [kre, t_], [kre])
            kb.tt("dve", kre[:], kre[:], den[:], ALU.mult, [kre, den], [kre])
            kb.tt("dve", kim[:], ni[:], Are[:], ALU.mult, [ni, Are], [kim])
            kb.tt("dve", t_[:], nr[:], Aim[:], ALU.mult, [nr, Aim], [t_])
            kb.tt("dve", kim[:], kim[:], t_[:], ALU.subtract, [kim, t_], [kim])
            kb.tt("dve", kim[:], kim[:], den[:], ALU.mult, [kim, den], [kim])
            t4 = kb.sb("t4", [128, 2, 16, 4], F32)
            kb4 = lambda t: t[:].unsqueeze(3).broadcast_to([128, 2, 16, 4])
            kb.tt("dve", qre[:], apr[:, :, :, 0:4], kb4(kre), ALU.mult, [apr, kre], [qre])
            kb.tt("dve", t4[:], api[:, :, :, 0:4], kb4(kim), ALU.mult, [api, kim], [t4])
            kb.tt("dve", qre[:], qre[:], t4[:], ALU.subtract, [qre, t4], [qre])
            kb.tt("dve", qim[:], apr[:, :, :, 0:4], kb4(kim), ALU.mult, [apr, kim], [qim])
            kb.tt("dve", t4[:], api[:, :, :, 0:4], kb4(kre), ALU.mult, [api, kre], [t4])
            kb.tt("dve", qim[:], qim[:], t4[:], ALU.add, [qim, t4], [qim])
        with kb.scope():
            iot = kb.sb("iot", [128, TB], F32)
            kb.op("pool", lambda e: e.iota(iot[:], pattern=[[1, TB]], base=0, channel_multiplier=0, allow_small_or_imprecise_dtypes=True), writes=[iot])
            ang = kb.sb("ang", [128, TB], F32)
            ctab = kb.sb("ctab", [128, TB], F32)
            stab = kb.sb("stab", [128, TB], F32)
            rrow = kb.sb("rrow", [128, TB], F32)
            pb = [kb.ps(f"pbu{i}", [128, 512], F32) for i in range(4)]
            py = kb.ps("py", [128, 512], F32)
            pw = kb.ps("pw", [128, 1024], F32)
            pk = kb.ps("pk", [128, 512], F32)
            v2 = [[kb.sb(f"v{i}_{q}", [128, TB], F32) for i in range(2)] for q in range(2)]
            tmp2 = [[kb.sb(f"tmp{i}_{q}", [128, TB], F32) for i in range(4)] for q in range(2)]
            sh2 = [[kb.sb(f"sh{i}_{q}", [128, TB], F32) for i in range(2)] for q in range(2)]
            sf = [kb.sb(f"sf{i}", [128, TB], F32) for i in range(2)]
            ET = kb.sb("ET", [128, 2, 2], F32)
            et = kb.sb("et", [128, 4], F32)
            init = kb.sb("init", [128, 2], F32)
            it = kb.sb("it", [128, 2], F32)
            Wn = kb.sb("Wn", [128, 2, 4, 16], F32)
            wt = [kb.sb(f"wt{i}", [128, 4, 16], F32) for i in range(2)]
            Wpad = kb.sb("Wpad", [128, 4, 2, 128], F32)
            WinT = [kb.sb(f"WinT{i}", [128, 4, 2, 128], BF16) for i in range(2)]
            CA = kb.sb("CA", [128, 5, 2, 128], F32)
            cm = [kb.sb(f"cm{i}", [128, 5, 128], F32) for i in range(2)]
            Cxl = [kb.sb(f"Cxl{i}", [128, 4, 2, 128], BF16) for i in range(2)]
            SBc = kb.sb("SBc", [128, 2, CTX // TS + 1], BF16)
            SBl = kb.sb("SBl", [128, 2, SEQ // TS + 1], BF16)
            kset = set()
            n = 0
            u = 0
            for d in range(2):
                for j in range(16):
                    kc = j // 4
                    par = u % 2
                    u += 1
                    qr = qre[:, d, j, :] if d == 1 else qre[:, d, j, ::-1]
                    qi_ = qim[:, d, j, :] if d == 1 else qim[:, d, j, ::-1]
                    q3 = lambda q: q.unsqueeze(2).broadcast_to([128, 4, 16])
                    b3 = lambda B_: B_[:, j, :].unsqueeze(1).broadcast_to([128, 4, 16])
                    kb.tt("dve", wt[0][:], b3(Bre), q3(qr), ALU.mult, [Bre, qre], [wt[0]])
                    kb.tt("dve", wt[1][:], b3(Bim), q3(qi_), ALU.mult, [Bim, qim], [wt[1]])
                    kb.tt("dve", Wn[:, 0], wt[0][:], wt[1][:], ALU.subtract, [wt[0], wt[1]], [Wn])
                    kb.tt("dve", wt[0][:], b3(Bim), q3(qr), ALU.mult, [Bim, qre], [wt[0]])
                    kb.tt("dve", wt[1][:], b3(Bre), q3(qi_), ALU.mult, [Bre, qim], [wt[1]])
                    kb.tt("dve", Wn[:, 1], wt[0][:], wt[1][:], ALU.add, [wt[0], wt[1]], [Wn])
                    kb.op("pool", lambda e: e.memset(Wpad[:], 0.0), writes=[Wpad])
                    for gl in range(2):
                        ph = slice(gl * 64, (gl + 1) * 64)
                        off = 32 * (j % 4) + 16 * gl
                        for ri in range(2):
                            kb.cp("pool", Wpad[ph, :, ri, off:off + 16], Wn[ph, ri, :, :], [Wn], [Wpad])
                    for sp in range(4):
                        for ri in range(2):
                            kb.tr(pw[:, (sp * 2 + ri) * 128:(sp * 2 + ri + 1) * 128], Wpad[:, sp, ri, :], g.identf[:], [Wpad, g.identf], [pw])
                    W_ = WinT[par]
                    kb.cp("dve", W_[:].rearrange("p a b c -> p (a b c)"), pw[:], [pw], [W_])
                    c3 = lambda ri: Cf[:, ri, j, :].unsqueeze(1).broadcast_to([128, 5, 128])
                    p3 = lambda t: t[:, d, j, :].unsqueeze(2).broadcast_to([128, 5, 128])
                    kb.tt("dve", cm[0][:], c3(0), p3(apr), ALU.mult, [Cf, apr], [cm[0]])
                    kb.tt("pool", cm[1][:], c3(1), p3(api), ALU.mult, [Cf, api], [cm[1]])
                    kb.tt("dve", CA[:, :, 0, :], cm[0][:], cm[1][:], ALU.subtract, [cm[0], cm[1]], [CA])
                    kb.tt("dve", cm[0][:], c3(0), p3(api), ALU.mult, [Cf, api], [cm[0]])
                    kb.tt("pool", cm[1][:], c3(1), p3(apr), ALU.mult, [Cf, apr], [cm[1]])
                    kb.stt(CA[:, :, 1, :], cm[0][:], -1.0, cm[1][:], ALU.mult, ALU.subtract, [cm[0], cm[1]], [CA])
                    CX = Cxl[par]
                    kb.cp("act", CX[:], CA[:, 1:5] if d == 0 else CA[:, 1:5][:, ::-1], [CA], [CX])
                    sp0 = 3 if d == 0 else 0
                    for tau in range(4):
                        kb.mm(pk[:, tau * 128:(tau + 1) * 128], Wpad[:, sp0, 0, :], CA[:, tau, 0, :], True, False, [Wpad, CA], [pk])
                        kb.mm32(pk[:, tau * 128:(tau + 1) * 128], Wpad[:, sp0, 1, :], CA[:, tau, 1, :], False, True, [Wpad, CA], [pk], pw[0:32, 0:1], pw, g.identb)
                    for tau in range(4):
                        slot = 3 + tau if d == 0 else 3 - tau
                        if (kc, slot) in kset:
                            kb.tt("dve", Kacc[:, kc, slot, :], Kacc[:, kc, slot, :], pk[:, tau * 128:(tau + 1) * 128], ALU.add, [Kacc, pk], [Kacc])
                        else:
                            kset.add((kc, slot))
                            kb.cp("dve", Kacc[:, kc, slot, :], pk[:, tau * 128:(tau + 1) * 128], [pk], [Kacc])
                    kb.ts("dve", ang[:], iot[:], ang5[:, d, j, 4:5], None, ALU.mult, None, [iot, ang5], [ang])
                    s5_sincos(g, ang[:], stab[:], ctab[:], [ang], [stab, ctab], TB)
                    kb.ts("dve", rrow[:], iot[:], 0.0, rk5[:, d, j, 4:5], ALU.mult, ALU.add, [iot, rk5], [rrow])
                    c4c, s4c = ck5[:, d, j, 4:5], sk5[:, d, j, 4:5]
                    for wi, Tc in enumerate((CTX // TS, TB)):
                        kb.ts("dve", et[:, 0:1], ctab[:, Tc - 1:Tc], c4c, None, ALU.mult, None, [ctab, ck5], [et])
                        kb.ts("dve", et[:, 1:2], stab[:, Tc - 1:Tc], s4c, None, ALU.mult, None, [stab, sk5], [et])
                        kb.ts("dve", et[:, 2:3], stab[:, Tc - 1:Tc], c4c, None, ALU.mult, None, [stab, ck5], [et])
                        kb.ts("dve", et[:, 3:4], ctab[:, Tc - 1:Tc], s4c, None, ALU.mult, None, [ctab, sk5], [et])
                        kb.tt("dve", ET[:, wi, 0:1], et[:, 0:1], et[:, 1:2], ALU.subtract, [et], [ET])
                        kb.tt("dve", ET[:, wi, 1:2], et[:, 2:3], et[:, 3:4], ALU.add, [et], [ET])
                    kb.op("dve", lambda e: e.memset(init[:], 0.0), writes=[init])
                    kb.op("pool", lambda e: e.memset(SBc[:, :, 0:1] if d == 0 else SBc[:, :, CTX // TS:CTX // TS + 1], 0.0), writes=[SBc])
                    order = segs if d == 0 else [segs[0]] + segs[:0:-1]
                    m_lat = 0
                    for si, (tok0, nb) in enumerate(order):
                        is_ctx = tok0 == 0
                        SBs = SBc if is_ctx else SBl
                        if is_ctx:
                            m0 = 0
                        else:
                            m0 = (tok0 - CTX) // TS
                        rvp = (lambda a: a[:, 0:nb]) if d == 0 else (lambda a: a[:, 0:nb][:, ::-1])
                        v, tmp, sh = v2[n % 2], tmp2[n % 2], sh2[n % 2]
                        Bre_p, Bim_p = pb[(2 * n) % 4], pb[(2 * n + 1) % 4]
                        n += 1
                        for ri, Pp in enumerate((Bre_p, Bim_p)):
                            for sp in range(4):
                                kb.mm(Pp[:, :nb], W_[:, sp, ri, :], uT[:, kc, tok0 + sp: tok0 + nb * TS: TS], sp == 0, sp == 3, [W_, uT], [Pp])
                        c_, s_ = ctab[:, :nb], stab[:, :nb]
                        kb.tt("dve", tmp[0][:, :nb], rvp(Bre_p), c_, ALU.mult, [Bre_p, ctab], [tmp[0]])
                        kb.tt("dve", tmp[1][:, :nb], rvp(Bim_p), s_, ALU.mult, [Bim_p, stab], [tmp[1]])
                        kb.tt("dve", tmp[2][:, :nb], rvp(Bim_p), c_, ALU.mult, [Bim_p, ctab], [tmp[2]])
                        kb.tt("dve", tmp[3][:, :nb], rvp(Bre_p), s_, ALU.mult, [Bre_p, stab], [tmp[3]])
                        kb.tt("pool", v[0][:, :nb], tmp[0][:, :nb], tmp[1][:, :nb], ALU.add, [tmp[0], tmp[1]], [v[0]])
                        kb.tt("pool", v[1][:, :nb], tmp[2][:, :nb], tmp[3][:, :nb], ALU.subtract, [tmp[2], tmp[3]], [v[1]])
                        for ri in range(2):
                            kb.op("dve", lambda e: e.tensor_tensor_scan(out=sh[ri][:, :nb], data0=rrow[:, :nb], data1=v[ri][:, :nb],
                                                                          initial=init[:, ri:ri + 1], op0=ALU.mult, op1=ALU.add),
                                  reads=[rrow, v[ri], init], writes=[sh[ri]])
                        wi = 0 if is_ctx else 1
                        kb.ts("dve", it[:, 0:1], sh[0][:, nb - 1:nb], ET[:, wi, 0:1], None, ALU.mult, None, [sh[0], ET], [it])
                        kb.ts("dve", it[:, 1:2], sh[0][:, nb - 1:nb], ET[:, wi, 1:2], None, ALU.mult, None, [sh[0], ET], [it])
                        kb.stt(init[:, 0:1], sh[1][:, nb - 1:nb], ET[:, wi, 1:2], it[:, 0:1], ALU.mult, ALU.subtract, [sh[1], ET, it], [init])
                        kb.ts("dve", init[:, 0:1], init[:, 0:1], -1.0, None, ALU.mult, None, [init], [init])
                        kb.stt(init[:, 1:2], sh[1][:, nb - 1:nb], ET[:, wi, 0:1], it[:, 1:2], ALU.mult, ALU.add, [sh[1], ET, it], [init])
                        kb.tt("pool", tmp[0][:, :nb], sh[0][:, :nb], c_, ALU.mult, [sh[0], ctab], [tmp[0]])
                        kb.tt("pool", tmp[1][:, :nb], sh[1][:, :nb], s_, ALU.mult, [sh[1], stab], [tmp[1]])
                        kb.tt("pool", tmp[2][:, :nb], sh[0][:, :nb], s_, ALU.mult, [sh[0], stab], [tmp[2]])
                        kb.tt("pool", tmp[3][:, :nb], sh[1][:, :nb], c_, ALU.mult, [sh[1], ctab], [tmp[3]])
                        kb.tt("dve", sf[0][:, :nb], tmp[0][:, :nb], tmp[1][:, :nb], ALU.subtract, [tmp[0], tmp[1]], [sf[0]])
                        kb.tt("dve", sf[1][:, :nb], tmp[2][:, :nb], tmp[3][:, :nb], ALU.add, [tmp[2], tmp[3]], [sf[1]])
                        for ri in range(2):
                            if d == 0:
                                dstv = SBs[:, ri, m0 + 1:m0 + 1 + nb]
                            else:
                                dstv = SBs[:, ri, m0:m0 + nb][:, ::-1]
                            kb.cp("act", dstv, sf[ri][:, :nb], [sf[ri]], [SBs])
                        if is_ctx:
                            if d == 0:
                                kb.cp("act", SBl[:, :, 0:1], SBc[:, :, CTX // TS:CTX // TS + 1], [SBc], [SBl])
                            else:
                                kb.cp("act", SBl[:, :, SEQ // TS:SEQ // TS + 1], SBc[:, :, 0:1], [SBc], [SBl])
                        po = m0 if d == 0 else m0 + 1
                        for lp in range(4):
                            kb.mm(py[:, lp * 128: lp * 128 + nb] if nb <= 128 else py[:, 0:nb], CX[:, lp, 0, :], SBs[:, 0, po:po + nb], True, False, [CX, SBs], [py])
                            kb.mm(py[:, lp * 128: lp * 128 + nb] if nb <= 128 else py[:, 0:nb], CX[:, lp, 1, :], SBs[:, 1, po:po + nb], False, True, [CX, SBs], [py])
                            ysl = yacc[:, kc, tok0 + lp: tok0 + nb * TS: TS]
                            kb.tt("dve", ysl, ysl, py[:, lp * 128: lp * 128 + nb] if nb <= 128 else py[:, 0:nb], ALU.add, [yacc, py], [yacc])
        with kb.scope():
            for kc in range(4):
                kb.stt(Kacc[:, kc, 3, :], g.identf[:], Dc[:, kc:kc + 1], Kacc[:, kc, 3, :], ALU.mult, ALU.add, [g.identf, Dc, Kacc], [Kacc])
            Kb = kb.sb("Kb", [128, 4, 7, 128], BF16)
            kb.cp("act", Kb[:], Kacc[:], [Kacc], [Kb])
            pl = [kb.ps(f"pl{i}", [128, 512], F32) for i in range(2)]
            n = 0
            for kc in range(4):
                for (tok0, nb) in segs:
                    for lp in range(4):
                        Pp = pl[n % 2]
                        n += 1
                        for sp in range(4):
                            kb.mm(Pp[:, :nb], Kb[:, kc, 3 + lp - sp, :], uT[:, kc, tok0 + sp: tok0 + nb * TS: TS], sp == 0, sp == 3, [Kb, uT], [Pp])
                        ysl = yacc[:, kc, tok0 + lp: tok0 + nb * TS: TS]
                        kb.tt("dve", ysl, ysl, Pp[:, :nb], ALU.add, [yacc, Pp], [yacc])
        if g.debug and L == 0 and b == 0:
            dbg_out(g, "s5_yacc", yacc, [128, 4, NT])
        with kb.scope():
            Wg = kb.sb("Wg", [128, 4, 1024], BF16)
            kb.dma("sp", Wg[:], g.wbd["s5_w_glu"][:, :].rearrange("(k p) c -> p k c", p=128), reads=[g.wbd["s5_w_glu"]], writes=[Wg])
            gy = kb.sb("gy", [128, 4, NT], BF16)
            x2 = kb.sb("x2", [128, NT], F32)
            for c in range(4):
                x = yacc[:, c, :]
                kb.act(x2[:], x, AF.Square, [yacc], [x2])
                kb.ts("dve", x2[:], x2[:], 0.044715 * math.sqrt(2 / math.pi), math.sqrt(2 / math.pi), ALU.mult, ALU.add, [x2], [x2])
                kb.tt("dve", x2[:], x2[:], x, ALU.mult, [x2, yacc], [x2])
                kb.act(x2[:], x2[:], AF.Tanh, [x2], [x2])
                kb.ts("dve", x2[:], x2[:], 1.0, 0.5, ALU.add, ALU.mult, [x2], [x2])
                kb.tt("dve", gy[:, c, :], x2[:], x, ALU.mult, [x2, yacc], [gy])
            pa = [kb.ps(f"pa{i}", [128, 512], F32) for i in range(2)]
            pbb = [kb.ps(f"pbb{i}", [128, 512], F32) for i in range(2)]
            sg = [kb.sb(f"sg{i}", [128, 512], F32) for i in range(2)]
            n = 0
            for c in range(4):
                for (t0, tn) in TCH:
                    A, B, SG = pa[n % 2], pbb[n % 2], sg[n % 2]
                    n += 1
                    for k in range(4):
                        kb.mm(A[:, :tn], Wg[:, k, c * 128:(c + 1) * 128], gy[:, k, t0:t0 + tn], k == 0, k == 3, [Wg, gy], [A])
                    for k in range(4):
                        kb.mm(B[:, :tn], Wg[:, k, 512 + c * 128:512 + (c + 1) * 128], gy[:, k, t0:t0 + tn], k == 0, k == 3, [Wg, gy], [B])
                    kb.act(SG[:, :tn], B[:, :tn], AF.Sigmoid, [B], [SG])
                    kb.tt("dve", Y5T[:, c, t0:t0 + tn], A[:, :tn], SG[:, :tn], ALU.mult, [A, SG], [Y5T])


WEIGHT_NAMES = ["mod_w", "mod_b", "w_in", "wa_sink", "ga_q_norm", "ga_k_norm", "ssd_conv_w", "ssd_conv_b", "ssd_dt_bias",
                "ssd_a_log", "ssd_d", "ssd_norm_w", "s5_a_re", "s5_a_im", "s5_log_dt", "s5_b_re", "s5_b_im", "s5_c_re",
                "s5_c_im", "s5_d", "s5_w_glu", "w_branch", "w_out", "ln1_g", "ln1_b", "ln2_g", "ln2_b", "router_w",
                "router_bias", "moe_w_gate", "moe_w_up", "moe_w_down"]


def kernel(**inputs):
    NB = 2
    n_cores = 8
    nc, g = build(NB=NB, DEPTH=4, debug=False)
    consts = host_consts()
    x = np.asarray(inputs["x"], dtype=np.float32)
    ctx = np.asarray(inputs["ctx"], dtype=np.float32)
    c = np.asarray(inputs["c"], dtype=np.float32)
    c_ctx = np.asarray(inputs["c_ctx"], dtype=np.float32)
    weights = {k: np.ascontiguousarray(np.asarray(inputs[k], dtype=np.float32)) for k in WEIGHT_NAMES}
    in_maps = []
    for core in range(n_cores):
        bs = slice(core * NB, (core + 1) * NB)
        m = {}
        m["xin"] = np.ascontiguousarray(np.concatenate([ctx[bs], x[bs]], axis=1))
        m["cvec"] = np.ascontiguousarray(np.concatenate([c[bs], c_ctx[None, :]], axis=0))
        m.update(weights)
        m.update(consts)
        in_maps.append(m)
    res = run_bass_kernel_spmd(nc, in_maps, core_ids=list(range(n_cores)))
    out = np.concatenate([np.asarray(res.results[i]["out"]) for i in range(n_cores)], axis=0)
    return out.astype(np.float32)
```
